# Optimizing a Trainium2 kernel written in Bass

```python
import math
import jax, jax.numpy as jnp
from jax import lax
import numpy as np

D_MODEL = 2048
BATCH = 4
SEQ = 2048
DEPTH = 4
DEC_BATCH = 128
DEC_SEQ = 1
PAST_LEN = 16384
PAGE_SIZE = 128

RET_HEADS = 8
RET_DK = 128
RET_DV = 128
RET_QK = RET_HEADS * RET_DK
RET_W = RET_HEADS * RET_DV
RET_CHUNK = 128
ROPE_BASE = 10000.0
S5_GROUPS = 64
S5_GROUP_CH = 16
S5_W = S5_GROUPS * S5_GROUP_CH
S5_STATE = 64
S5_DT_MIN = 0.001
S5_DT_MAX = 0.1
D_FF = 5632
IN_W = 2 * RET_QK + 2 * RET_W + S5_W + 2 * D_MODEL
NORM_EPS = 1e-6
HEAD_NORM_EPS = 1e-5

kernel_name = 'hybrid_retention_s5_macaron_step'


def rms_norm(x, g):
    xf = x.astype(jnp.float32)
    xf = xf * lax.rsqrt(jnp.mean(xf * xf, axis=-1, keepdims=True) + NORM_EPS)
    return (xf * g.astype(jnp.float32)).astype(x.dtype)


def swiglu(x, w1, w3, w2):
    return (jax.nn.silu(x @ w1) * (x @ w3)) @ w2


def rotary(x, pos):
    half = x.shape[-1] // 2
    inv = ROPE_BASE ** (-jnp.arange(half, dtype=jnp.float32) / half)
    ang = pos.astype(jnp.float32)[:, None] * inv[None, :]
    cos = jnp.cos(ang)[None, :, None, :]
    sin = jnp.sin(ang)[None, :, None, :]
    x1, x2 = x[..., :half], x[..., half:]
    return jnp.concatenate([x1 * cos - x2 * sin, x1 * sin + x2 * cos], axis=-1)


def retention(q, k, v, s0):
    bsz, L, H, _ = q.shape
    C = RET_CHUNK if L % RET_CHUNK == 0 else L
    nc = L // C
    log_g = jnp.log1p(-jnp.exp2(-5.0 - jnp.arange(H, dtype=jnp.float32)))
    i = jnp.arange(C, dtype=jnp.float32)
    diff = i[:, None] - i[None, :]
    intra = jnp.where(diff[None] >= 0,
                      jnp.exp(jnp.maximum(diff, 0.0)[None] * log_g[:, None, None]), 0.0)
    q_dec = jnp.exp((i + 1.0)[None, :] * log_g[:, None])
    k_dec = jnp.exp((C - 1.0 - i)[None, :] * log_g[:, None])
    c_dec = jnp.exp(C * log_g)

    def chunks(t):
        return t.reshape(bsz, nc, C, H, t.shape[-1]).transpose(1, 0, 3, 2, 4)

    def step(s, qkv):
        qc, kc, vc = qkv
        sc = jnp.einsum('bhid,bhjd->bhij', qc, kc) * intra[None]
        o = (jnp.einsum('bhij,bhje->bhie', sc, vc)
             + jnp.einsum('bhid,bhde->bhie', qc * q_dec[None, :, :, None], s))
        s = (s * c_dec[None, :, None, None]
             + jnp.einsum('bhjd,bhje->bhde', kc * k_dec[None, :, :, None], vc))
        return s, o

    s, o = lax.scan(step, s0, (chunks(q), chunks(k), chunks(v)))
    o = o.transpose(1, 0, 3, 2, 4).reshape(bsz, L, H, v.shape[-1])
    return o, s


def head_norm(o, g):
    mu = jnp.mean(o, axis=-1, keepdims=True)
    var = jnp.mean(jnp.square(o - mu), axis=-1, keepdims=True)
    o = (o - mu) * lax.rsqrt(var + HEAD_NORM_EPS)
    bsz, L = o.shape[:2]
    return o.reshape(bsz, L, -1) * g.astype(jnp.float32)


def s5_scan(u, h0_re, h0_im, lam_re, lam_im, b_re, b_im, c_re, c_im, d, log_step):
    f32 = jnp.float32
    uf = u.astype(f32)
    L = u.shape[1]
    lam = lax.complex(lam_re.astype(f32), lam_im.astype(f32))
    dt = jnp.exp(log_step.astype(f32))[:, None]
    lam_dt = lam * dt
    lbar = jnp.exp(lam_dt)
    bbar = ((lbar - 1.0) / lam)[..., None] * lax.complex(b_re.astype(f32), b_im.astype(f32))
    cmat = lax.complex(c_re.astype(f32), c_im.astype(f32))
    bu = jnp.einsum('gpc,blgc->blgp', bbar, uf.astype(jnp.complex64))

    def combine(e1, e2):
        a1, x1 = e1
        a2, x2 = e2
        return a1 * a2, a2 * x1 + x2

    _, h = lax.associative_scan(combine, (jnp.broadcast_to(lbar, bu.shape), bu), axis=1)
    h0 = lax.complex(h0_re.astype(f32), h0_im.astype(f32))
    t = jnp.arange(1, L + 1, dtype=f32)
    h = h + jnp.exp(lam_dt[None] * t[:, None, None])[None] * h0[:, None]
    y = jnp.einsum('gcp,blgp->blgc', cmat, h).real + d.astype(f32) * uf
    h_last = h[:, -1]
    return y, h_last.real, h_last.imag


def mixer(u, pos, s_ret0, h0_re, h0_im, p):
    bsz, L, _ = u.shape
    f32 = jnp.float32
    proj = u @ p['w_in']
    splits = [RET_QK, 2 * RET_QK, 2 * RET_QK + RET_W, 2 * RET_QK + 2 * RET_W,
              2 * RET_QK + 2 * RET_W + S5_W, 2 * RET_QK + 2 * RET_W + S5_W + D_MODEL]
    q, k, v, rg, su, gr, gs = jnp.split(proj, splits, axis=-1)
    q = rotary(q.reshape(bsz, L, RET_HEADS, RET_DK).astype(f32), pos) * (RET_DK ** -0.5)
    k = rotary(k.reshape(bsz, L, RET_HEADS, RET_DK).astype(f32), pos)
    v = v.reshape(bsz, L, RET_HEADS, RET_DV).astype(f32)
    o, s_ret = retention(q, k, v, s_ret0.astype(f32))
    o = head_norm(o, p['ret_gn']) * jax.nn.silu(rg.astype(f32))
    b_ret = o.astype(u.dtype) @ p['ret_proj']
    y, h_re, h_im = s5_scan(su.reshape(bsz, L, S5_GROUPS, S5_GROUP_CH), h0_re, h0_im,
                            p['s5_lam_re'], p['s5_lam_im'], p['s5_b_re'], p['s5_b_im'],
                            p['s5_c_re'], p['s5_c_im'], p['s5_d'], p['s5_log_step'])
    z = jax.nn.gelu(y.reshape(bsz, L, S5_W)).astype(u.dtype)
    z = z * jax.nn.sigmoid(z @ p['glu_w'] + p['glu_b'])
    b_s5 = z @ p['s5_proj']
    m = jax.nn.sigmoid(gr) * b_ret + jax.nn.sigmoid(gs) * b_s5
    return m @ p['w_out'], s_ret, h_re, h_im


def layer_forward(x, pos, s_ret0, h0_re, h0_im, p):
    x = x + 0.5 * swiglu(rms_norm(x, p['ffn1_norm']), p['ffn1_w1'], p['ffn1_w3'], p['ffn1_w2'])
    mix, s_ret, h_re, h_im = mixer(rms_norm(x, p['mix_norm']), pos, s_ret0, h0_re, h0_im, p)
    x = x + mix
    x = x + 0.5 * swiglu(rms_norm(x, p['ffn2_norm']), p['ffn2_w1'], p['ffn2_w3'], p['ffn2_w2'])
    return x, s_ret, h_re, h_im


def setup_inputs(seed: int = 0) -> dict:
    key = jax.random.key(seed)
    ks = iter(jax.random.split(key, 40))
    f32 = jnp.float32

    def nrm(shape, scale):
        return jax.random.normal(next(ks), shape, f32) * scale

    def gain(shape):
        return 1.0 + nrm(shape, 0.01)

    d = D_MODEL
    inp = {}
    inp['x_prompt'] = nrm((BATCH, SEQ, d), 1.0)
    inp['x_sample'] = nrm((DEC_BATCH, DEC_SEQ, d), 1.0)
    inp['state_ret'] = nrm((DEPTH, DEC_BATCH, RET_HEADS, RET_DK, RET_DV), 2.0)
    inp['state_s5_re'] = nrm((DEPTH, DEC_BATCH, S5_GROUPS, S5_STATE), 0.5)
    inp['state_s5_im'] = nrm((DEPTH, DEC_BATCH, S5_GROUPS, S5_STATE), 0.5)
    inp['ffn1_norm'] = gain((DEPTH, d))
    inp['ffn1_w1'] = nrm((DEPTH, d, D_FF), d ** -0.5)
    inp['ffn1_w3'] = nrm((DEPTH, d, D_FF), d ** -0.5)
    inp['ffn1_w2'] = nrm((DEPTH, D_FF, d), D_FF ** -0.5)
    inp['mix_norm'] = gain((DEPTH, d))
    inp['w_in'] = nrm((DEPTH, d, IN_W), d ** -0.5)
    inp['ret_gn'] = gain((DEPTH, RET_W))
    inp['ret_proj'] = nrm((DEPTH, RET_W, d), RET_W ** -0.5)
    inp['s5_lam_re'] = -0.5 + nrm((DEPTH, S5_GROUPS, S5_STATE), 0.01)
    inp['s5_lam_im'] = (math.pi * jnp.arange(S5_STATE, dtype=f32))[None, None, :] + nrm((DEPTH, S5_GROUPS, S5_STATE), 0.01)
    inp['s5_b_re'] = nrm((DEPTH, S5_GROUPS, S5_STATE, S5_GROUP_CH), (2 * S5_GROUP_CH) ** -0.5)
    inp['s5_b_im'] = nrm((DEPTH, S5_GROUPS, S5_STATE, S5_GROUP_CH), (2 * S5_GROUP_CH) ** -0.5)
    inp['s5_c_re'] = nrm((DEPTH, S5_GROUPS, S5_GROUP_CH, S5_STATE), (2 * S5_STATE) ** -0.5)
    inp['s5_c_im'] = nrm((DEPTH, S5_GROUPS, S5_GROUP_CH, S5_STATE), (2 * S5_STATE) ** -0.5)
    inp['s5_d'] = nrm((DEPTH, S5_GROUPS, S5_GROUP_CH), 1.0)
    inp['s5_log_step'] = jax.random.uniform(next(ks), (DEPTH, S5_GROUPS), f32,
                                            math.log(S5_DT_MIN), math.log(S5_DT_MAX))
    inp['glu_w'] = nrm((DEPTH, S5_W, S5_W), S5_W ** -0.5)
    inp['glu_b'] = nrm((DEPTH, S5_W), 0.01)
    inp['s5_proj'] = nrm((DEPTH, S5_W, d), S5_W ** -0.5)
    inp['w_out'] = nrm((DEPTH, d, d), d ** -0.5)
    inp['ffn2_norm'] = gain((DEPTH, d))
    inp['ffn2_w1'] = nrm((DEPTH, d, D_FF), d ** -0.5)
    inp['ffn2_w3'] = nrm((DEPTH, d, D_FF), d ** -0.5)
    inp['ffn2_w2'] = nrm((DEPTH, D_FF, d), D_FF ** -0.5)
    inp['final_norm'] = gain((d,))
    return inp


def reference(x_prompt, x_sample, state_ret, state_s5_re, state_s5_im,
              ffn1_norm, ffn1_w1, ffn1_w3, ffn1_w2, mix_norm, w_in, ret_gn, ret_proj,
              s5_lam_re, s5_lam_im, s5_b_re, s5_b_im, s5_c_re, s5_c_im, s5_d, s5_log_step,
              glu_w, glu_b, s5_proj, w_out, ffn2_norm, ffn2_w1, ffn2_w3, ffn2_w2, final_norm):
    f32 = jnp.float32
    bp, lp = x_prompt.shape[:2]
    ls = x_sample.shape[1]
    pos_p = jnp.arange(lp, dtype=jnp.int32)
    pos_s = PAST_LEN + jnp.arange(ls, dtype=jnp.int32)
    zero_ret = jnp.zeros((bp, RET_HEADS, RET_DK, RET_DV), f32)
    zero_s5 = jnp.zeros((bp, S5_GROUPS, S5_STATE), f32)
    yp, ys = x_prompt, x_sample
    rp, pre, pim, rs, sre, sim = [], [], [], [], [], []
    for l in range(DEPTH):
        p = dict(ffn1_norm=ffn1_norm[l], ffn1_w1=ffn1_w1[l], ffn1_w3=ffn1_w3[l], ffn1_w2=ffn1_w2[l],
                 mix_norm=mix_norm[l], w_in=w_in[l], ret_gn=ret_gn[l], ret_proj=ret_proj[l],
                 s5_lam_re=s5_lam_re[l], s5_lam_im=s5_lam_im[l], s5_b_re=s5_b_re[l], s5_b_im=s5_b_im[l],
                 s5_c_re=s5_c_re[l], s5_c_im=s5_c_im[l], s5_d=s5_d[l], s5_log_step=s5_log_step[l],
                 glu_w=glu_w[l], glu_b=glu_b[l], s5_proj=s5_proj[l], w_out=w_out[l],
                 ffn2_norm=ffn2_norm[l], ffn2_w1=ffn2_w1[l], ffn2_w3=ffn2_w3[l], ffn2_w2=ffn2_w2[l])
        yp, s_r, h_r, h_i = layer_forward(yp, pos_p, zero_ret, zero_s5, zero_s5, p)
        rp.append(s_r); pre.append(h_r); pim.append(h_i)
        ys, s_r, h_r, h_i = layer_forward(ys, pos_s, state_ret[l], state_s5_re[l], state_s5_im[l], p)
        rs.append(s_r); sre.append(h_r); sim.append(h_i)
    yp = rms_norm(yp, final_norm)
    ys = rms_norm(ys, final_norm)
    return (yp, ys, jnp.stack(rp), jnp.stack(pre), jnp.stack(pim),
            jnp.stack(rs), jnp.stack(sre), jnp.stack(sim))
```

```python
import math
from contextlib import ExitStack
import numpy as np
import concourse.bass as bass
import concourse.mybir as mybir
from concourse.bass_utils import run_bass_kernel_spmd

F32 = mybir.dt.float32
F32R = mybir.dt.float32r
BF16 = mybir.dt.bfloat16
I32 = mybir.dt.int32
AF = mybir.ActivationFunctionType
ALU = mybir.AluOpType

D = 2048
DFF = 5632
DEPTH = 4
NCH = D // 128
NJ = DFF // 128
H = 8
NT = 512
NS = 16
SEQ = 2048
NTILE = SEQ // NT
PAST = 16384
INW = 9216
SEM_LIMIT = 30000
NWS = 4
NDMASEM = 6
SLOTW = 128
ARENA_W = 38 * 1024
ARENA_R_W = 7 * 1024


class SemObj:
    __slots__ = ("sem", "count")

    def __init__(self, sem):
        self.sem = sem
        self.count = 0


class Eng:
    def __init__(self, name, obj, T):
        self.name = name
        self.obj = obj
        self.T = T
        self.cur = None
        self.seen = {}

    def mark(self):
        if self.cur is None or self.cur.count >= SEM_LIMIT:
            self.cur = self.T.new_sem(self.name)
        self.cur.count += 1
        return (self.cur, self.cur.count)


class Buf:
    __slots__ = ("ap", "slots", "name")

    def __init__(self, ap, slots, name=""):
        self.ap = ap
        self.slots = slots
        self.name = name


class Tracker:
    def __init__(self, nc, es):
        self.nc = nc
        self.es = es
        self.dry = False
        self.nsem = 0
        self.engs = {}
        for name, obj in (("pe", nc.tensor), ("act", nc.scalar), ("dve", nc.vector),
                          ("pool", nc.gpsimd), ("sp", nc.sync)):
            self.engs[name] = Eng(name, obj, self)
        self.dma_sems = {}
        self.dma_rr = {}
        self.state = {}
        self.n_inst = 0

    def new_sem(self, name):
        self.nsem += 1
        return SemObj(self.es.enter_context(self.nc.semaphore(f"s_{name}_{self.nsem}")))

    def _deps(self, reads, writes):
        deps = {}
        st = self.state
        for b in reads:
            for s in b.slots:
                e = st.get(s)
                if e is not None and e[0] is not None:
                    so, v = e[0]
                    if deps.get(so, 0) < v:
                        deps[so] = v
        for b in writes:
            for s in b.slots:
                e = st.get(s)
                if e is not None:
                    if e[0] is not None:
                        so, v = e[0]
                        if deps.get(so, 0) < v:
                            deps[so] = v
                    for so, v in e[1].items():
                        if deps.get(so, 0) < v:
                            deps[so] = v
        return deps

    def _update(self, mark, reads, writes):
        st = self.state
        so, v = mark
        for b in reads:
            for s in b.slots:
                e = st.get(s)
                if e is None:
                    st[s] = [None, {so: v}]
                else:
                    e[1][so] = v
        for b in writes:
            for s in b.slots:
                st[s] = [mark, {}]

    def _wait(self, E, deps, skip_self=None):
        for so, v in deps.items():
            if skip_self is not None and so is skip_self:
                continue
            if E.seen.get(so, 0) < v:
                E.obj.wait_ge(so.sem, v)
                E.seen[so] = v

    def issue(self, eng, fn, reads=(), writes=()):
        if self.dry:
            return
        E = self.engs[eng]
        deps = self._deps(reads, writes)
        self._wait(E, deps, skip_self=(E.cur if eng == "pe" else None))
        inst = fn(E.obj)
        mark = E.mark()
        inst.then_inc(mark[0].sem, 1)
        self._update(mark, reads, writes)
        self.n_inst += 1

    def dma(self, queue, out, in_, reads=(), writes=()):
        if self.dry:
            return
        E = self.engs[queue]
        lst = self.dma_sems.setdefault(queue, [])
        rr = self.dma_rr.get(queue, 0)
        self.dma_rr[queue] = rr + 1
        if len(lst) < NDMASEM:
            lst.append(self.new_sem("dma" + queue))
        so = lst[rr % NDMASEM]
        deps = self._deps(reads, writes)
        if so.count > 0:
            deps[so] = max(deps.get(so, 0), so.count)
        self._wait(E, deps)
        inst = E.obj.dma_start(out=out, in_=in_)
        so.count += 16
        inst.then_inc(so.sem, 16)
        self._update((so, so.count), reads, writes)
        self.n_inst += 1

    def final_wait(self, eng="sp"):
        if self.dry:
            return
        E = self.engs[eng]
        for q, lst in self.dma_sems.items():
            for so in lst:
                if so.count > 0 and E.seen.get(so, 0) < so.count:
                    E.obj.wait_ge(so.sem, so.count)
                    E.seen[so] = so.count
        for name, e2 in self.engs.items():
            if e2.cur is not None and e2.cur.count > 0 and name != eng:
                E.obj.wait_ge(e2.cur.sem, e2.cur.count)


class Arena:
    def __init__(self, nc, es, T, name="arena", width=None, space="sb"):
        self.W = width
        self.space = space
        self.t = es.enter_context(nc.sbuf_tensor(name, [128, width], F32))
        self.top = 0
        self.T = T

    def alloc(self, words, name=""):
        words = (words + SLOTW - 1) // SLOTW * SLOTW
        o = self.top
        self.top += words
        assert self.top <= self.W, f"arena overflow at {name}: {self.top}"
        return self.view(o, words, name)

    def view(self, o, words, name=""):
        ap = self.t[:, o:o + words]
        slots = [(self.space, i) for i in range(o // SLOTW, (o + words) // SLOTW)]
        return Buf(ap, slots, name)

    def mark(self):
        return self.top

    def release(self, m):
        self.top = m


def sub(buf, o, words):
    s0 = buf.slots[0][1] + o // SLOTW
    s1 = buf.slots[0][1] + (o + words + SLOTW - 1) // SLOTW
    return Buf(buf.ap[:, o:o + words], [(buf.slots[0][0], i) for i in range(s0, s1)], buf.name)


def bf(buf):
    return buf.ap.bitcast(BF16)


def fr(buf):
    return buf.ap.bitcast(F32R)


def build(debug_stage=99):
    nc = bass.Bass("TRN2", target_bir_lowering=False)
    es = ExitStack()
    T = Tracker(nc, es)
    A = Arena(nc, es, T, "arena", ARENA_W, "sb")
    AR = Arena(nc, es, T, "arena_r", ARENA_R_W, "sr")

    def din(name, shape, dt=F32):
        return nc.dram_tensor(name, list(shape), dt, kind="ExternalInput").ap()

    def dout(name, shape):
        return nc.dram_tensor(name, list(shape), F32, kind="ExternalOutput").ap()

    xp = din("xp", [SEQ, D])
    xs = din("xs", [NS, D])
    st_ret = din("st_ret", [DEPTH, NS, H, 128, 128])
    st_re = din("st_re", [DEPTH, NS, 4096])
    st_im = din("st_im", [DEPTH, NS, 4096])
    Wd = {}
    for nm, shp in (("ffn1_w1", [DEPTH, D, DFF]), ("ffn1_w3", [DEPTH, D, DFF]), ("ffn1_w2", [DEPTH, DFF, D]),
                    ("w_in", [DEPTH, D, INW]), ("ret_proj", [DEPTH, 1024, D]), ("glu_w", [DEPTH, 1024, 1024]),
                    ("s5_proj", [DEPTH, 1024, D]), ("w_out", [DEPTH, D, D]),
                    ("ffn2_w1", [DEPTH, D, DFF]), ("ffn2_w3", [DEPTH, D, DFF]), ("ffn2_w2", [DEPTH, DFF, D])):
        Wd[nm] = din(nm, shp)
    gains = din("gains", [13, 16, 128])
    vec8 = din("vec8", [12, 8, 128])
    lam_re = din("s5_lam_re", [DEPTH, 64, 64])
    lam_im = din("s5_lam_im", [DEPTH, 64, 64])
    logstep = din("s5_log_step", [DEPTH, 64, 1])
    b_re = din("s5_b_re", [DEPTH, 64, 64, 16])
    b_im = din("s5_b_im", [DEPTH, 64, 64, 16])
    c_re = din("s5_c_re", [DEPTH, 1024, 64])
    c_im = din("s5_c_im", [DEPTH, 1024, 64])
    consts = din("consts", [128, CONST_W])
    constsr = din("constsr", [128, CONSTR_W])
    rope = din("rope", [NTILE + 1, 2, 128, NT])

    yp = dout("yp", [SEQ, D])
    ys = dout("ys", [NS, D])
    o_ret_p = dout("o_ret_p", [DEPTH, H, 128, 128])
    o_re_p = dout("o_re_p", [DEPTH, 32, 128])
    o_im_p = dout("o_im_p", [DEPTH, 32, 128])
    o_ret_s = dout("o_ret_s", [DEPTH, NS, H, 128, 128])
    o_re_s = dout("o_re_s", [DEPTH, NS * 32, 128])
    o_im_s = dout("o_im_s", [DEPTH, NS * 32, 128])
    scr_ret = nc.dram_tensor("scr_ret", [DEPTH, H, 128, 128], F32, kind="Internal").ap()
    scr_buf = [[Buf(None, [("dram", l, h)]) for h in range(H)] for l in range(DEPTH)]

    PS = []
    for i in range(8):
        t = es.enter_context(nc.psum_tensor(f"ps{i}", [128, 512], F32))
        PS.append(Buf(t[:], [("ps", i)], f"ps{i}"))
    ps_rr = [0]

    ps_free = list(range(8))

    def psum():
        b = PS[ps_free[ps_rr[0] % len(ps_free)]]
        ps_rr[0] += 1
        return b

    def psum_pin():
        b = psum()
        ps_free.remove(b.slots[0][1])
        return b

    def psum_unpin(b):
        ps_free.append(b.slots[0][1])
        ps_free.sort()

    CONST = A.alloc(CONST_W, "const")
    CONSTR = AR.alloc(CONSTR_W, "constr")
    X = A.alloc(NCH * NT, "X")
    XN = A.alloc(NCH * NT // 2, "XN")
    WS = [A.alloc(2048, f"ws{i}") for i in range(NWS)]
    GAIN = A.alloc(13 * 16, "gain")
    VEC8 = A.alloc(12 * 8, "vec8")
    ROPE_H = [None]
    S5ST = A.alloc(DEPTH * 64, "s5st")

    cv = CONST.ap
    ident = cv[:, C_ID:C_ID + 128]
    cr = fr(CONSTR)
    perm_r = cr[:, R_PERM:R_PERM + 128]
    ones_r = cr[:, R_ONES:R_ONES + 128]

    def cbuf():
        return CONST

    class WStream:
        def __init__(self):
            self.sched = []
            self.pos = 0
            self.issued = 0

        def reset(self):
            self.pos = 0
            self.issued = 0

        def get(self, mk):
            if T.dry:
                self.sched.append(mk)
                return WS[(len(self.sched) - 1) % NWS]
            idx = self.pos
            self.pos += 1
            while self.issued < min(len(self.sched), idx + NWS - 1):
                slot = WS[self.issued % NWS]
                for (o_ap, i_ap) in self.sched[self.issued](slot):
                    T.dma("pool", o_ap, i_ap, writes=[slot])
                self.issued += 1
            return WS[idx % NWS]

    WSR = WStream()

    def slabK(name, l, c0, ncol, kch):
        def mk(slot):
            o = bf(slot)[:, 0:kch * ncol].rearrange("p (k n) -> p k n", k=kch)
            i = Wd[name][l, :, c0:c0 + ncol].rearrange("(k p) n -> p k n", p=128)
            return [(o, i)]
        return mk

    def slabR(name, l, r0, nr):
        def mk(slot):
            o = bf(slot)[:, 0:nr * 2048].rearrange("p (j n) -> p j n", j=nr)
            i = Wd[name][l, r0 * 128:(r0 + nr) * 128, :].rearrange("(j p) n -> p j n", p=128)
            return [(o, i)]
        return mk

    def mm(out_buf, out_ap, lhsT, rhs, start, stop, reads):
        T.issue("pe", lambda e: e.matmul(out_ap, lhsT=lhsT, rhs=rhs, start=start, stop=stop),
                reads=reads, writes=[out_buf])

    def transpose(out_buf, out_ap, in_ap, idn, reads):
        T.issue("pe", lambda e: e.transpose(out_ap, in_ap, idn), reads=reads, writes=[out_buf])

    def act(out_ap, in_ap, func, reads, writes, scale=1.0, bias=0.0):
        T.issue("act", lambda e: e.activation(out=out_ap, in_=in_ap, func=func, bias=bias, scale=scale),
                reads=reads, writes=writes)

    def tt(out_ap, a, b, op, reads, writes, eng="dve"):
        T.issue(eng, lambda e: e.tensor_tensor(out=out_ap, in0=a, in1=b, op=op), reads=reads, writes=writes)

    def ts(out_ap, a, s1, s2, op0, op1, reads, writes, eng="dve"):
        T.issue(eng, lambda e: e.tensor_scalar(out=out_ap, in0=a, scalar1=s1, scalar2=s2, op0=op0, op1=op1),
                reads=reads, writes=writes)

    def stt(out_ap, a, s, b, op0, op1, reads, writes):
        T.issue("dve", lambda e: e.scalar_tensor_tensor(out=out_ap, in0=a, scalar=s, in1=b, op0=op0, op1=op1),
                reads=reads, writes=writes)

    def cp(out_ap, in_ap, reads, writes, eng="dve"):
        T.issue(eng, lambda e: e.tensor_copy(out=out_ap, in_=in_ap), reads=reads, writes=writes)

    def xv(nt):
        return X.ap[:, 0:NCH * nt].rearrange("p (c n) -> p c n", c=NCH)

    def xnv(nt):
        return bf(XN)[:, 0:NCH * nt].rearrange("p (c n) -> p c n", c=NCH)

    def load_x(src, nt):
        m = A.mark()
        nb = (nt + 127) // 128
        rows = min(nt, 128)
        for b in range(nb):
            stg = A.alloc(D, "xstage")
            T.dma("sp", stg.ap[0:rows, :], src[b * 128:b * 128 + rows, :], writes=[stg])
            for c4 in range(4):
                p = psum()
                for q in range(4):
                    c = c4 * 4 + q
                    transpose(p, p.ap[:, q * 128:q * 128 + rows], stg.ap[0:rows, c * 128:(c + 1) * 128],
                              ident[0:rows, 0:rows], [stg, CONST])
                o = xv(nt)[:, c4 * 4:(c4 + 1) * 4, b * 128:b * 128 + rows]
                i = p.ap.rearrange("p (q n) -> p q n", q=4)[:, :, 0:rows]
                (cp if (c4 % 2 == 0) else (lambda o_, i_, r_, w_: act(o_, i_, AF.Copy, r_, w_)))(o, i, [p], [X])
            if b % 2 == 1:
                A.release(m)
        A.release(m)

    def rmsnorm(nt, grow, out_kind):
        m = A.mark()
        mr = AR.mark()
        sq = [AR.alloc(nt, "sq0"), AR.alloc(nt, "sq1")]
        rstd = A.alloc(nt, "rstd")
        p = psum()
        for c in range(NCH):
            s = sq[c % 2]
            act(fr(s)[:, 0:nt], xv(nt)[:, c, :], AF.Square, [X], [s])
            mm(p, p.ap[:, 0:nt], ones_r, fr(s)[:, 0:nt], c == 0, c == NCH - 1, [s, CONSTR])
        act(rstd.ap[:, 0:nt], p.ap[:, 0:nt], AF.Sqrt, [p, CONST], [rstd], scale=1.0 / D,
            bias=cv[:, C_EPS6:C_EPS6 + 1])
        T.issue("dve", lambda e: e.reciprocal(out=rstd.ap[:, 0:nt], in_=rstd.ap[:, 0:nt]), reads=[rstd], writes=[rstd])
        g = GAIN.ap[:, grow * 16:(grow + 1) * 16]
        for c in range(NCH):
            if out_kind == "xn":
                stt(xnv(nt)[:, c, :], xv(nt)[:, c, :], g[:, c:c + 1], rstd.ap[:, 0:nt], ALU.mult, ALU.mult,
                    [X, GAIN, rstd], [XN])
            else:
                stt(xv(nt)[:, c, :], xv(nt)[:, c, :], g[:, c:c + 1], rstd.ap[:, 0:nt], ALU.mult, ALU.mult,
                    [X, GAIN, rstd], [X])
        A.release(m)
        AR.release(mr)

    def ffn(nt, l, pref):
        m = A.mark()
        GJ = 8
        Hb = A.alloc(GJ * nt // 2, "H")
        hv = bf(Hb)[:, 0:GJ * nt].rearrange("p (j n) -> p j n", j=GJ)
        sl = [A.alloc(nt, "silu0"), A.alloc(nt, "silu1")]
        xn = xnv(nt)
        j = 0
        cnt = 0
        while j < NJ:
            gj = min(GJ, NJ - j)
            for jj in range(0, gj, 2):
                w1 = WSR.get(slabK(pref + "_w1", l, (j + jj) * 128, 256, 16))
                w3 = WSR.get(slabK(pref + "_w3", l, (j + jj) * 128, 256, 16))
                w1v = bf(w1)[:, 0:4096].rearrange("p (k n) -> p k n", k=16)
                w3v = bf(w3)[:, 0:4096].rearrange("p (k n) -> p k n", k=16)
                for q in range(2):
                    p1 = psum()
                    p3 = psum()
                    for k in range(16):
                        mm(p1, p1.ap[:, 0:nt], w1v[:, k, q * 128:(q + 1) * 128], xn[:, k, :], k == 0, k == 15, [w1, XN])
                    for k in range(16):
                        mm(p3, p3.ap[:, 0:nt], w3v[:, k, q * 128:(q + 1) * 128], xn[:, k, :], k == 0, k == 15, [w3, XN])
                    s = sl[cnt % 2]
                    cnt += 1
                    act(s.ap[:, 0:nt], p1.ap[:, 0:nt], AF.Silu, [p1], [s])
                    tt(hv[:, jj + q, :], s.ap[:, 0:nt], p3.ap[:, 0:nt], ALU.mult, [s, p3], [Hb])
            for c2 in range(8):
                def mk(slot, c2=c2, j=j, gj=gj):
                    o = bf(slot)[:, 0:gj * 256].rearrange("p (r n) -> p r n", r=gj)
                    i = Wd[pref + "_w2"][l, j * 128:(j + gj) * 128, c2 * 256:(c2 + 1) * 256] \
                        .rearrange("(r p) n -> p r n", p=128)
                    return [(o, i)]
                w2 = WSR.get(mk)
                wv = bf(w2)[:, 0:gj * 256].rearrange("p (r n) -> p r n", r=gj)
                for q in range(2):
                    p = psum()
                    for ji in range(gj):
                        mm(p, p.ap[:, 0:nt], wv[:, ji, q * 128:(q + 1) * 128], hv[:, ji, :], ji == 0, ji == gj - 1,
                           [w2, Hb])
                    cc = c2 * 2 + q
                    stt(xv(nt)[:, cc, :], p.ap[:, 0:nt], 0.5, xv(nt)[:, cc, :], ALU.mult, ALU.add, [p, X], [X])
            j += gj
        A.release(m)

    def store_y(dst, nt):
        m = A.mark()
        nb = (nt + 127) // 128
        rows = min(nt, 128)
        for b in range(nb):
            stg = A.alloc(D, "ystage")
            for c4 in range(4):
                p = psum()
                for q in range(4):
                    c = c4 * 4 + q
                    transpose(p, p.ap[0:rows, q * 128:(q + 1) * 128], xv(nt)[:, c, b * 128:b * 128 + rows], ident, [X, CONST])
                o = stg.ap[0:rows, c4 * 512:(c4 + 1) * 512]
                if c4 % 2 == 0:
                    cp(o, p.ap[0:rows, :], [p], [stg])
                else:
                    act(o, p.ap[0:rows, :], AF.Copy, [p], [stg])
            T.dma("sp", dst[b * 128:b * 128 + rows, :], stg.ap[0:rows, :], reads=[stg])
            if b % 2 == 1:
                A.release(m)
        A.release(m)


    GH = [1.0 - 2.0 ** (-5.0 - h) for h in range(H)]
    GC = [g ** 128 for g in GH]
    maskT = cv[:, C_MASK:C_MASK + 128]
    ident_r = cr[:, R_ID:R_ID + 128]
    onesdiv_r = cr[:, R_ODIV:R_ODIV + 128]

    def head_norm(po_ap, n, l, head, srg_ap, srg_buf, out_ap, out_buf, pbuf):
        m = A.mark()
        mr = AR.mark()
        osb = AR.alloc(n, "osb")
        o2 = AR.alloc(n, "o2")
        mean = A.alloc(n, "mean")
        var = A.alloc(n, "var")
        t1 = A.alloc(n, "hn_t1")
        act(fr(osb)[:, 0:n], po_ap, AF.Copy, [pbuf], [osb])
        act(fr(o2)[:, 0:n], po_ap, AF.Square, [pbuf], [o2])
        pm = psum()
        mm(pm, pm.ap[:, 0:n], onesdiv_r, fr(osb)[:, 0:n], True, True, [CONSTR, osb])
        pv = psum()
        mm(pv, pv.ap[:, 0:n], onesdiv_r, fr(o2)[:, 0:n], True, True, [CONSTR, o2])
        act(mean.ap[:, 0:n], pm.ap[:, 0:n], AF.Copy, [pm], [mean])
        tt(var.ap[:, 0:n], mean.ap[:, 0:n], mean.ap[:, 0:n], ALU.mult, [mean], [var])
        tt(var.ap[:, 0:n], pv.ap[:, 0:n], var.ap[:, 0:n], ALU.subtract, [pv, var], [var])
        act(var.ap[:, 0:n], var.ap[:, 0:n], AF.Sqrt, [var, CONST], [var], bias=cv[:, C_EPS5:C_EPS5 + 1])
        T.issue("dve", lambda e: e.reciprocal(out=var.ap[:, 0:n], in_=var.ap[:, 0:n]), reads=[var], writes=[var])
        tt(t1.ap[:, 0:n], osb.ap[:, 0:n], mean.ap[:, 0:n], ALU.subtract, [osb, mean], [t1])
        tt(t1.ap[:, 0:n], t1.ap[:, 0:n], var.ap[:, 0:n], ALU.mult, [t1, var], [t1])
        gn = VEC8.ap[:, (l * 3 + 0) * 8 + head:(l * 3 + 0) * 8 + head + 1]
        stt(out_ap, t1.ap[:, 0:n], gn, srg_ap, ALU.mult, ALU.mult, [t1, VEC8, srg_buf], [out_buf])
        A.release(m)
        AR.release(mr)

    def proj16(slab, q, n, ncol=256):
        wv = bf(slab)[:, 0:16 * ncol].rearrange("p (k n) -> p k n", k=16)
        p = psum()
        for k in range(16):
            mm(p, p.ap[:, 0:n], wv[:, k, q * 128:(q + 1) * 128], xnv(n)[:, k, :], k == 0, k == 15, [slab, XN])
        return p

    def rotary(p, n, scale, tab_mul, name, res=None):
        if res is None:
            res = AR.alloc(n, name + "rot")
        m_ = A.mark()
        mr_ = AR.mark()
        raw = AR.alloc(n, name + "raw")
        t1 = A.alloc(n, name + "t1")
        act(fr(raw)[:, 0:n], p.ap[:, 0:n], AF.Copy, [p], [raw], scale=scale)
        pr = psum()
        mm(pr, pr.ap[:, 0:n], perm_r, fr(raw)[:, 0:n], True, True, [CONSTR, raw])
        tt(t1.ap[:, 0:n], raw.ap[:, 0:n], ROPE_H[0].ap[:, 0:n], ALU.mult, [raw, ROPE_H[0]], [t1])
        if tab_mul is None:
            tt(fr(res)[:, 0:n], pr.ap[:, 0:n], ROPE_H[0].ap[:, NT:NT + n], ALU.mult, [pr, ROPE_H[0]], [res])
            tt(fr(res)[:, 0:n], res.ap[:, 0:n], t1.ap[:, 0:n], ALU.add, [res, t1], [res])
        else:
            t2 = A.alloc(n, name + "t2")
            tt(t2.ap[:, 0:n], pr.ap[:, 0:n], ROPE_H[0].ap[:, NT:NT + n], ALU.mult, [pr, ROPE_H[0]], [t2])
            tt(t1.ap[:, 0:n], t1.ap[:, 0:n], t2.ap[:, 0:n], ALU.add, [t1, t2], [t1])
            tt(fr(res)[:, 0:n].rearrange("p (c i) -> p c i", i=128),
               t1.ap[:, 0:n].rearrange("p (c i) -> p c i", i=128),
               tab_mul.unsqueeze(1).broadcast_to([128, n // 128, 128]), ALU.mult, [t1, CONST], [res])
        A.release(m_)
        AR.release(mr_)
        return res

    def retention_prompt(ti, l, OALL):
        oall = bf(OALL)[:, 0:H * NT].rearrange("p (h n) -> p h n", h=H)
        n = NT
        ncx = n // 128
        for hp in range(4):
            m = A.mark()
            mr = AR.mark()
            wv_s = WSR.get(slabK("w_in", l, 2048 + hp * 256, 256, 16))
            wvv = bf(wv_s)[:, 0:4096].rearrange("p (k n) -> p k n", k=16)
            vtok = AR.alloc(ncx * 256, "vtok")
            vtv = fr(vtok)[:, 0:ncx * 256].rearrange("p (c e) -> p c e", c=ncx)
            for c2 in range(ncx // 2):
                pv = psum()
                for cc in range(2):
                    c = c2 * 2 + cc
                    for k in range(16):
                        mm(pv, pv.ap[:, cc * 256:(cc + 1) * 256], xnv(n)[:, k, c * 128:(c + 1) * 128], wvv[:, k, :],
                           k == 0, k == 15, [XN, wv_s])
                act(fr(vtok)[:, c2 * 512:(c2 + 1) * 512], pv.ap, AF.Copy, [pv], [vtok])
            qd = [AR.alloc(n, "qd0"), AR.alloc(n, "qd1")]
            kd = [AR.alloc(n, "kd0"), AR.alloc(n, "kd1")]
            wq_s = WSR.get(slabK("w_in", l, hp * 256, 256, 16))
            for hh in range(2):
                pq = proj16(wq_s, hh, n)
                rotary(pq, n, 128.0 ** -0.5, cv[:, C_QDEC + (2 * hp + hh) * 128:C_QDEC + (2 * hp + hh + 1) * 128], f"q{hh}", qd[hh])
            wk_s = WSR.get(slabK("w_in", l, 1024 + hp * 256, 256, 16))
            for hh in range(2):
                pk = proj16(wk_s, hh, n)
                rotary(pk, n, 1.0, cv[:, C_KDI + (2 * hp + hh) * 128:C_KDI + (2 * hp + hh + 1) * 128], f"k{hh}", kd[hh])
            wr_s = WSR.get(slabK("w_in", l, 3072 + hp * 256, 256, 16))
            srg = []
            for hh in range(2):
                pg = proj16(wr_s, hh, n)
                sb = A.alloc(n, f"srg{hh}")
                act(sb.ap[:, 0:n], pg.ap[:, 0:n], AF.Silu, [pg], [sb])
                srg.append(sb)
            for hh in range(2):
                head = 2 * hp + hh
                m2 = A.mark()
                mr2 = AR.mark()
                qv = fr(qd[hh])
                kv = fr(kd[hh])
                ps_ = psum()
                for c in range(ncx):
                    mm(ps_, ps_.ap[:, c * 128:(c + 1) * 128], kv[:, c * 128:(c + 1) * 128], qv[:, c * 128:(c + 1) * 128],
                       True, True, [kd[hh], qd[hh]])
                scT = AR.alloc(n, "scT")
                tt(fr(scT)[:, 0:n].rearrange("p (c i) -> p c i", i=128), ps_.ap[:, 0:n].rearrange("p (c i) -> p c i", i=128),
                   maskT.unsqueeze(1).broadcast_to([128, ncx, 128]), ALU.mult, [ps_, CONST], [scT])
                pt = psum()
                for c in range(ncx):
                    transpose(pt, pt.ap.bitcast(F32R)[:, c * 128:(c + 1) * 128], kv[:, c * 128:(c + 1) * 128], ident_r,
                              [kd[hh], CONSTR])
                kdt = AR.alloc(n, "kdt")
                act(fr(kdt)[:, 0:n], pt.ap[:, 0:n], AF.Copy, [pt], [kdt])
                pu = psum()
                for c in range(ncx):
                    mm(pu, pu.ap[:, c * 128:(c + 1) * 128], fr(kdt)[:, c * 128:(c + 1) * 128],
                       vtv[:, c, hh * 128:(hh + 1) * 128], True, True, [kdt, vtok])
                Sall = AR.alloc((ncx + 1) * 128, "Sall")
                Sv = fr(Sall)[:, 0:(ncx + 1) * 128].rearrange("p (c e) -> p c e", e=128)
                Svf = Sall.ap[:, 0:(ncx + 1) * 128].rearrange("p (c e) -> p c e", e=128)
                if ti == 0:
                    ts(Sv[:, 0, :], pu.ap[:, 0:128], 0.0, None, ALU.mult, ALU.bypass, [pu], [Sall])
                else:
                    T.dma("pool", Sv[:, 0, :], scr_ret[l, head], reads=[scr_buf[l][head]], writes=[Sall])
                tmp = A.alloc(128, "stmp")
                for c in range(ncx):
                    tt(tmp.ap[:, 0:128], pu.ap[:, c * 128:(c + 1) * 128], Svf[:, c, :], ALU.add, [pu, Sall], [tmp])
                    act(Sv[:, c + 1, :], tmp.ap[:, 0:128], AF.Copy, [tmp], [Sall], scale=GC[head])
                dst = o_ret_p[l, head] if ti == NTILE - 1 else scr_ret[l, head]
                T.dma("sp", dst, Svf[:, ncx, :], reads=[Sall], writes=([] if ti == NTILE - 1 else [scr_buf[l][head]]))
                po = psum()
                for c in range(ncx):
                    mm(po, po.ap[:, c * 128:(c + 1) * 128], vtv[:, c, hh * 128:(hh + 1) * 128], fr(scT)[:, c * 128:(c + 1) * 128],
                       True, False, [vtok, scT])
                    mm(po, po.ap[:, c * 128:(c + 1) * 128], Sv[:, c, :], qv[:, c * 128:(c + 1) * 128],
                       False, True, [Sall, qd[hh]])
                head_norm(po.ap[:, 0:n], n, l, head, srg[hh].ap[:, 0:n], srg[hh], oall[:, head, :], OALL, po)
                A.release(m2)
                AR.release(mr2)
            A.release(m)
            AR.release(mr)

    def retention_sample(l, OALL):
        n = NS
        oall = bf(OALL)[:, 0:H * n].rearrange("p (h n) -> p h n", h=H)
        m = A.mark()
        mr = AR.mark()
        QS = A.alloc(H * n, "QS")
        KS = A.alloc(H * n, "KS")
        SRG = A.alloc(H * n, "SRG")
        VT = A.alloc(1024, "VT")
        SEL = A.alloc(n * 128, "SEL")
        cp(SEL.ap[0:n, 0:n * 128].rearrange("p (b d) -> p b d", b=n),
           ident[0:n, 0:n].unsqueeze(2).broadcast_to([n, n, 128]), [CONST], [SEL])
        for hp in range(4):
            m2 = A.mark()
            mr2 = AR.mark()
            wv_s = WSR.get(slabK("w_in", l, 2048 + hp * 256, 256, 16))
            wvv = bf(wv_s)[:, 0:4096].rearrange("p (k n) -> p k n", k=16)
            pv = psum()
            for k in range(16):
                mm(pv, pv.ap[0:n, 0:256], xnv(n)[:, k, :], wvv[:, k, :], k == 0, k == 15, [XN, wv_s])
            cp(VT.ap[0:n, hp * 256:(hp + 1) * 256], pv.ap[0:n, 0:256], [pv], [VT])
            wq_s = WSR.get(slabK("w_in", l, hp * 256, 256, 16))
            for hh in range(2):
                pq = proj16(wq_s, hh, n)
                r = rotary(pq, n, 128.0 ** -0.5, None, "sq")
                cp(QS.ap[:, (2 * hp + hh) * n:(2 * hp + hh + 1) * n], r.ap[:, 0:n], [r], [QS])
            wk_s = WSR.get(slabK("w_in", l, 1024 + hp * 256, 256, 16))
            for hh in range(2):
                pk = proj16(wk_s, hh, n)
                r = rotary(pk, n, 1.0, None, "sk")
                cp(KS.ap[:, (2 * hp + hh) * n:(2 * hp + hh + 1) * n], r.ap[:, 0:n], [r], [KS])
            wr_s = WSR.get(slabK("w_in", l, 3072 + hp * 256, 256, 16))
            for hh in range(2):
                pg = proj16(wr_s, hh, n)
                act(SRG.ap[:, (2 * hp + hh) * n:(2 * hp + hh + 1) * n], pg.ap[:, 0:n], AF.Silu, [pg], [SRG])
            A.release(m2)
            AR.release(mr2)
        po = psum_pin()
        Sb = [A.alloc(1024, "Sb0"), A.alloc(1024, "Sb1")]
        for b in range(n):
            S = Sb[b % 2]
            Sv = S.ap[:, 0:1024].rearrange("p (h e) -> p h e", h=H)
            T.dma("sp", Sv, st_ret[l, b].rearrange("h d e -> d h e"), writes=[S])
            pvb = [psum(), psum()]
            for hf in range(2):
                mm(pvb[hf], pvb[hf].ap[:, 0:512], SEL.ap[0:n, b * 128:(b + 1) * 128], VT.ap[0:n, hf * 512:(hf + 1) * 512],
                   True, True, [SEL, VT])
            for h in range(H):
                act(Sv[:, h, :], Sv[:, h, :], AF.Copy, [S], [S], scale=GH[h])
                stt(Sv[:, h, :], pvb[h // 4].ap[:, (h % 4) * 128:(h % 4 + 1) * 128], KS.ap[:, h * n + b:h * n + b + 1], Sv[:, h, :],
                    ALU.mult, ALU.add, [pvb[h // 4], KS, S], [S])
            for h in range(H):
                mm(po, po.ap[:, h * n + b:h * n + b + 1], Sv[:, h, :], QS.ap[:, h * n + b:h * n + b + 1], True, True, [S, QS])
            T.dma("sp", o_ret_s[l, b].rearrange("h d e -> d h e"), Sv, reads=[S])
        for h in range(H):
            head_norm(po.ap[:, h * n:(h + 1) * n], n, l, h, SRG.ap[:, h * n:(h + 1) * n], SRG, oall[:, h, :], OALL, po)
        psum_unpin(po)
        A.release(m)
        AR.release(mr)

    def merge_and_out(nt, l, OALL, Z, use_s5):
        m = A.mark()
        M = A.alloc(NCH * nt // 2, "M")
        mv = bf(M)[:, 0:NCH * nt].rearrange("p (c n) -> p c n", c=NCH)
        oall = bf(OALL)[:, 0:H * nt].rearrange("p (h n) -> p h n", h=H)
        zv = bf(Z)[:, 0:8 * nt].rearrange("p (h n) -> p h n", h=8) if Z is not None else None
        sg = [A.alloc(nt, f"sg{i}") for i in range(4)]
        for cp_ in range(8):
            wgr = WSR.get(slabK("w_in", l, 5120 + cp_ * 256, 256, 16))
            for q in range(2):
                pg = proj16(wgr, q, nt)
                act(sg[q].ap[:, 0:nt], pg.ap[:, 0:nt], AF.Sigmoid, [pg], [sg[q]])
            wgs = WSR.get(slabK("w_in", l, 7168 + cp_ * 256, 256, 16))
            for q in range(2):
                pg = proj16(wgs, q, nt)
                act(sg[2 + q].ap[:, 0:nt], pg.ap[:, 0:nt], AF.Sigmoid, [pg], [sg[2 + q]])
            wrp = WSR.get(slabK("ret_proj", l, cp_ * 256, 256, 8))
            wrv = bf(wrp)[:, 0:2048].rearrange("p (k n) -> p k n", k=8)
            for q in range(2):
                pb = psum()
                for h in range(H):
                    mm(pb, pb.ap[:, 0:nt], wrv[:, h, q * 128:(q + 1) * 128], oall[:, h, :], h == 0, h == H - 1, [wrp, OALL])
                tt(sg[q].ap[:, 0:nt], sg[q].ap[:, 0:nt], pb.ap[:, 0:nt], ALU.mult, [sg[q], pb], [sg[q]])
            if use_s5:
                wsp = WSR.get(slabK("s5_proj", l, cp_ * 256, 256, 8))
                wsv = bf(wsp)[:, 0:2048].rearrange("p (k n) -> p k n", k=8)
                for q in range(2):
                    pz = psum()
                    for h in range(8):
                        mm(pz, pz.ap[:, 0:nt], wsv[:, h, q * 128:(q + 1) * 128], zv[:, h, :], h == 0, h == 7, [wsp, Z])
                    tt(sg[2 + q].ap[:, 0:nt], sg[2 + q].ap[:, 0:nt], pz.ap[:, 0:nt], ALU.mult, [sg[2 + q], pz], [sg[2 + q]])
                    tt(mv[:, cp_ * 2 + q, :], sg[q].ap[:, 0:nt], sg[2 + q].ap[:, 0:nt], ALU.add, [sg[q], sg[2 + q]], [M])
            else:
                for q in range(2):
                    cp(mv[:, cp_ * 2 + q, :], sg[q].ap[:, 0:nt], [sg[q]], [M])
        for c2 in range(8):
            wo = WSR.get(slabK("w_out", l, c2 * 256, 256, 16))
            wov = bf(wo)[:, 0:4096].rearrange("p (k n) -> p k n", k=16)
            for q in range(2):
                p = psum()
                for k in range(16):
                    mm(p, p.ap[:, 0:nt], wov[:, k, q * 128:(q + 1) * 128], mv[:, k, :], k == 0, k == 15, [wo, M])
                cc = c2 * 2 + q
                tt(xv(nt)[:, cc, :], xv(nt)[:, cc, :], p.ap[:, 0:nt], ALU.add, [X, p], [X])
        A.release(m)


    TWO_PI = 6.28318
    MASKQ = [cv[:, C_MQ + q * 128:C_MQ + (q + 1) * 128] for q in range(4)]

    def s5_branch(ti, nt, l, Z):
        is_s = ti >= NTILE
        m0 = A.mark()
        mr0 = AR.mark()
        Z0 = A.alloc(8 * nt // 2, "Z0")
        z0v = bf(Z0)[:, 0:8 * nt].rearrange("p (c n) -> p c n", c=8)
        zv = bf(Z)[:, 0:8 * nt].rearrange("p (c n) -> p c n", c=8)
        SU = A.alloc(8 * nt // 2, "SU")
        suv = bf(SU)[:, 0:8 * nt].rearrange("p (c n) -> p c n", c=8)
        for cp_ in range(4):
            ws = WSR.get(slabK("w_in", l, 4096 + cp_ * 256, 256, 16))
            for q in range(2):
                p = proj16(ws, q, nt)
                act(suv[:, cp_ * 2 + q, :], p.ap[:, 0:nt], AF.Copy, [p], [SU])
        PL = A.alloc(4 * 32, "s5pl")
        mG = A.mark()
        G = A.alloc(64 * 12, "s5g")
        gt = lambda i: G.ap[0:64, i * 64:(i + 1) * 64]
        LRE, LIM, RHO, TH, SN, CS, XI, T0, T1, SRE, SIM, DTB = range(12)
        T.dma("sp", gt(LRE), lam_re[l], writes=[G])
        T.dma("sp", gt(LIM), lam_im[l], writes=[G])
        T.dma("sp", gt(DTB)[:, 0:1], logstep[l], writes=[G])
        GB = [G]
        act(gt(DTB)[:, 1:2], gt(DTB)[:, 0:1], AF.Exp, GB, GB)
        dtc = gt(DTB)[:, 1:2]
        act(gt(RHO), gt(LRE), AF.Exp, GB, GB, scale=dtc)
        ts(gt(TH), gt(LIM), dtc, 1.0 / (2 * math.pi), ALU.mult, ALU.mult, GB, GB)
        xi = G.ap.bitcast(I32)[0:64, XI * 64:(XI + 1) * 64]
        cp(xi, gt(TH), GB, GB)
        tt(gt(T0), gt(TH), xi, ALU.subtract, GB, GB)
        act(gt(SN), gt(T0), AF.Sin, GB, GB, scale=TWO_PI)
        ts(gt(T1), gt(TH), 0.25, None, ALU.add, ALU.bypass, GB, GB)
        cp(xi, gt(T1), GB, GB)
        tt(gt(T0), gt(T1), xi, ALU.subtract, GB, GB)
        act(gt(CS), gt(T0), AF.Sin, GB, GB, scale=TWO_PI)
        tt(gt(CS), gt(CS), gt(RHO), ALU.mult, GB, GB)
        tt(gt(SN), gt(SN), gt(RHO), ALU.mult, GB, GB)
        ts(gt(CS), gt(CS), -1.0, None, ALU.add, ALU.bypass, GB, GB)
        tt(gt(T0), gt(LRE), gt(LRE), ALU.mult, GB, GB)
        tt(gt(T1), gt(LIM), gt(LIM), ALU.mult, GB, GB)
        tt(gt(T0), gt(T0), gt(T1), ALU.add, GB, GB)
        T.issue("dve", lambda e: e.reciprocal(out=gt(T0), in_=gt(T0)), reads=GB, writes=GB)
        tt(gt(SRE), gt(CS), gt(LRE), ALU.mult, GB, GB)
        tt(gt(T1), gt(SN), gt(LIM), ALU.mult, GB, GB)
        tt(gt(SRE), gt(SRE), gt(T1), ALU.add, GB, GB)
        tt(gt(SRE), gt(SRE), gt(T0), ALU.mult, GB, GB)
        tt(gt(SIM), gt(SN), gt(LRE), ALU.mult, GB, GB)
        tt(gt(T1), gt(CS), gt(LIM), ALU.mult, GB, GB)
        tt(gt(SIM), gt(SIM), gt(T1), ALU.subtract, GB, GB)
        tt(gt(SIM), gt(SIM), gt(T0), ALU.mult, GB, GB)
        Lm = A.alloc(128, "s5L")
        for i, src in enumerate((RHO, TH, SRE, SIM)):
            ts(Lm.ap[0:64, 0:64], gt(src), cv[0:64, C_EVEN:C_EVEN + 1], None, ALU.mult, ALU.bypass, [G, CONST], [Lm])
            ts(Lm.ap[0:64, 64:128], gt(src), cv[0:64, C_ODD:C_ODD + 1], None, ALU.mult, ALU.bypass, [G, CONST], [Lm])
            p = psum()
            mm(p, p.ap[:, 0:32], Lm.ap[0:64, 0:128], cv[0:64, C_SEL:C_SEL + 32], True, True, [Lm, CONST])
            cp(PL.ap[:, i * 32:(i + 1) * 32], p.ap[:, 0:32], [p], [PL])
        A.release(mG)
        pl = lambda i, pair: PL.ap[:, i * 32 + pair:i * 32 + pair + 1]
        if is_s:
            HIN = A.alloc(2 * 512, "hin")
            HOUT = A.alloc(2 * 512, "hout")
            for ri, src in enumerate((st_re, st_im)):
                stg = A.alloc(512, "hstg")
                T.dma("sp", stg.ap[:, 0:512].rearrange("r (k m) -> r k m", k=4),
                      src[l].rearrange("b (pr m) -> (b pr) m", m=128).rearrange("(k r) m -> r k m", r=128), writes=[stg])
                p = psum()
                for k in range(4):
                    transpose(p, p.ap[:, k * 128:(k + 1) * 128], stg.ap[:, k * 128:(k + 1) * 128], ident, [stg, CONST])
                cp(HIN.ap[:, ri * 512:(ri + 1) * 512], p.ap, [p], [HIN])
        tv = cv[:, C_ONE16:C_ONE16 + nt] if is_s else cv[:, C_TV:C_TV + nt]
        wm = A.mark()
        wmr = AR.mark()
        for ch in range(8):
            A.release(wm)
            AR.release(wmr)
            BR = A.alloc(4 * 64, "s5br")
            brv = lambda i: BR.ap[:, i * 64:(i + 1) * 64].rearrange("p (a c) -> p a c", a=4)
            T.dma("sp", brv(0), b_re[l, 8 * ch:8 * ch + 8].rearrange("(a gl) p c -> (gl p) a c", gl=2), writes=[BR])
            T.dma("sp", brv(1), b_im[l, 8 * ch:8 * ch + 8].rearrange("(a gl) p c -> (gl p) a c", gl=2), writes=[BR])
            sre = PL.ap[:, 2 * 32 + 4 * ch:2 * 32 + 4 * ch + 4].unsqueeze(2).broadcast_to([128, 4, 16])
            sim = PL.ap[:, 3 * 32 + 4 * ch:3 * 32 + 4 * ch + 4].unsqueeze(2).broadcast_to([128, 4, 16])
            BB = A.alloc(4 * 64, "s5bb")
            bbv = lambda i: BB.ap[:, i * 64:(i + 1) * 64].rearrange("p (a c) -> p a c", a=4)
            tt(bbv(0), brv(0), sre, ALU.mult, [BR, PL], [BB])
            tt(bbv(2), brv(1), sim, ALU.mult, [BR, PL], [BB])
            tt(bbv(0), bbv(0), bbv(2), ALU.subtract, [BB], [BB])
            tt(bbv(1), brv(1), sre, ALU.mult, [BR, PL], [BB])
            tt(bbv(2), brv(0), sim, ALU.mult, [BR, PL], [BB])
            tt(bbv(1), bbv(1), bbv(2), ALU.add, [BB], [BB])
            BL = A.alloc(8 * 64, "s5bl")
            blv = lambda a, ri: bf(BL)[:, (a * 2 + ri) * 128:(a * 2 + ri + 1) * 128]
            XP = [A.alloc(128, "s5xp0"), A.alloc(128, "s5xp1")]
            for a in range(4):
                for ri in range(2):
                    xp_ = XP[(a * 2 + ri) % 2]
                    tt(xp_.ap[:, 0:128].rearrange("p (g c) -> p g c", g=8),
                       bbv(ri)[:, a:a + 1, :].broadcast_to([128, 8, 16]) if False else
                       BB.ap[:, ri * 64 + a * 16:ri * 64 + (a + 1) * 16].unsqueeze(1).broadcast_to([128, 8, 16]),
                       MASKQ[a].rearrange("p (g c) -> p g c", g=8), ALU.mult, [BB, CONST], [xp_])
                    p = psum()
                    transpose(p, p.ap[:, 0:128], xp_.ap[:, 0:128], ident, [xp_, CONST])
                    act(blv(a, ri), p.ap[:, 0:128], AF.Copy, [p], [BL])
            CC = A.alloc(256, "s5cc")
            for ri, src in enumerate((c_re, c_im)):
                for hf in range(2):
                    T.dma("sp", CC.ap[:, ri * 128 + hf * 64:ri * 128 + (hf + 1) * 64], src[l, ch * 128:(ch + 1) * 128, :], writes=[CC])
            CL = AR.alloc(8 * 128, "s5cl")
            clv = lambda a, ri: fr(CL)[:, (a * 2 + ri) * 128:(a * 2 + ri + 1) * 128]
            for ri in range(2):
                p = psum()
                transpose(p, p.ap[:, 0:128], CC.ap[:, ri * 128:(ri + 1) * 128], ident, [CC, CONST])
                for a in range(4):
                    stt(clv(a, ri), p.ap[:, 0:128], (1.0 if ri == 0 else -1.0), MASKQ[a], ALU.mult, ALU.mult, [p, CONST], [CL])
            py = psum_pin()
            for a in range(4):
                pair = 4 * ch + a
                mw = A.mark()
                mrw = AR.mark()
                W_ = [A.alloc(nt, f"s5w{i}") for i in range(7)]
                COS, SIN, XA, XB, XI_, TA, TB = W_
                w = lambda b_: b_.ap[:, 0:nt]
                xiv = XI_.ap.bitcast(I32)[:, 0:nt]
                ts(w(XA), tv, pl(1, pair), None, ALU.mult, ALU.bypass, [CONST, PL], [XA])
                cp(xiv, w(XA), [XA], [XI_])
                tt(w(XB), w(XA), xiv, ALU.subtract, [XA, XI_], [XB])
                act(w(SIN), w(XB), AF.Sin, [XB], [SIN], scale=TWO_PI)
                ts(w(XA), w(XA), 0.25, None, ALU.add, ALU.bypass, [XA], [XA])
                cp(xiv, w(XA), [XA], [XI_])
                tt(w(XB), w(XA), xiv, ALU.subtract, [XA, XI_], [XB])
                act(w(COS), w(XB), AF.Sin, [XB], [COS], scale=TWO_PI)
                pre = psum()
                pim = psum()
                mm(pre, pre.ap[:, 0:nt], blv(a, 0), suv[:, ch, :], True, True, [BL, SU])
                mm(pim, pim.ap[:, 0:nt], blv(a, 1), suv[:, ch, :], True, True, [BL, SU])
                tt(w(XA), pre.ap[:, 0:nt], w(COS), ALU.mult, [pre, COS], [XA])
                tt(w(TA), pim.ap[:, 0:nt], w(SIN), ALU.mult, [pim, SIN], [TA])
                tt(w(XA), w(XA), w(TA), ALU.add, [XA, TA], [XA])
                tt(w(XB), pim.ap[:, 0:nt], w(COS), ALU.mult, [pim, COS], [XB])
                tt(w(TA), pre.ap[:, 0:nt], w(SIN), ALU.mult, [pre, SIN], [TA])
                tt(w(XB), w(XB), w(TA), ALU.subtract, [XB, TA], [XB])
                rho = pl(0, pair)
                if is_s:
                    hre = HIN.ap[:, pair:512:32]
                    him = HIN.ap[:, 512 + pair:1024:32]
                    stt(w(TA), hre, rho, w(XA), ALU.mult, ALU.add, [HIN, PL, XA], [TA])
                    stt(w(TB), him, rho, w(XB), ALU.mult, ALU.add, [HIN, PL, XB], [TB])
                else:
                    sre0 = S5ST.ap[:, l * 64 + pair:l * 64 + pair + 1]
                    sim0 = S5ST.ap[:, l * 64 + 32 + pair:l * 64 + 32 + pair + 1]
                    T.issue("dve", lambda e: e.tensor_tensor_scan(out=w(TA), data0=rho.broadcast_to([128, nt]), data1=w(XA),
                                                                 initial=sre0, op0=ALU.mult, op1=ALU.add),
                            reads=[PL, XA, S5ST], writes=[TA])
                    T.issue("dve", lambda e: e.tensor_tensor_scan(out=w(TB), data0=rho.broadcast_to([128, nt]), data1=w(XB),
                                                                 initial=sim0, op0=ALU.mult, op1=ALU.add),
                            reads=[PL, XB, S5ST], writes=[TB])
                P_ = [AR.alloc(nt, f"s5p{i}") for i in range(4)]
                pw = lambda i: fr(P_[i])[:, 0:nt]
                tt(pw(0), w(COS), w(TA), ALU.mult, [COS, TA], [P_[0]])
                stt(pw(1), w(SIN), -1.0, w(TB), ALU.mult, ALU.mult, [SIN, TB], [P_[1]])
                tt(pw(2), w(SIN), w(TA), ALU.mult, [SIN, TA], [P_[2]])
                tt(pw(3), w(COS), w(TB), ALU.mult, [COS, TB], [P_[3]])
                for i in range(4):
                    mm(py, py.ap[:, 0:nt], clv(a, 0 if i < 2 else 1), pw(i), (a == 0 and i == 0), (a == 3 and i == 3), [CL, P_[i]])
                if is_s:
                    tt(HOUT.ap[:, pair:512:32], P_[0].ap[:, 0:nt], P_[1].ap[:, 0:nt], ALU.add, [P_[0], P_[1]], [HOUT])
                    tt(HOUT.ap[:, 512 + pair:1024:32], P_[2].ap[:, 0:nt], P_[3].ap[:, 0:nt], ALU.add, [P_[2], P_[3]], [HOUT])
                else:
                    tt(S5ST.ap[:, l * 64 + pair:l * 64 + pair + 1], P_[0].ap[:, nt - 1:nt], P_[1].ap[:, nt - 1:nt], ALU.add,
                       [P_[0], P_[1]], [S5ST])
                    tt(S5ST.ap[:, l * 64 + 32 + pair:l * 64 + 32 + pair + 1], P_[2].ap[:, nt - 1:nt], P_[3].ap[:, nt - 1:nt], ALU.add,
                       [P_[2], P_[3]], [S5ST])
                A.release(mw)
                AR.release(mrw)
            YF = A.alloc(nt, "s5yf")
            YT = A.alloc(nt, "s5yt")
            dcol = VEC8.ap[:, (l * 3 + 2) * 8 + ch:(l * 3 + 2) * 8 + ch + 1]
            stt(YF.ap[:, 0:nt], suv[:, ch, :], dcol, py.ap[:, 0:nt], ALU.mult, ALU.add, [SU, VEC8, py], [YF])
            psum_unpin(py)
            act(YT.ap[:, 0:nt], YF.ap[:, 0:nt], AF.Square, [YF], [YT])
            ts(YT.ap[:, 0:nt], YT.ap[:, 0:nt], 0.044715, 1.0, ALU.mult, ALU.add, [YT], [YT])
            tt(YT.ap[:, 0:nt], YT.ap[:, 0:nt], YF.ap[:, 0:nt], ALU.mult, [YT, YF], [YT])
            act(YT.ap[:, 0:nt], YT.ap[:, 0:nt], AF.Sigmoid, [YT], [YT], scale=2.0 * math.sqrt(2.0 / math.pi))
            tt(z0v[:, ch, :], YT.ap[:, 0:nt], YF.ap[:, 0:nt], ALU.mult, [YT, YF], [Z0])
        if is_s:
            for ri, dstt in enumerate((o_re_s, o_im_s)):
                stg = A.alloc(512, "hostg")
                p = psum()
                for k in range(4):
                    transpose(p, p.ap[:, k * 128:(k + 1) * 128], HOUT.ap[:, ri * 512 + k * 128:ri * 512 + (k + 1) * 128], ident, [HOUT, CONST])
                cp(stg.ap[:, 0:512], p.ap, [p], [stg])
                T.dma("sp", dstt[l].rearrange("(k r) m -> r k m", r=128), stg.ap[:, 0:512].rearrange("r (k m) -> r k m", k=4), reads=[stg])
        elif ti == NTILE - 1:
            for ri, dstt in enumerate((o_re_p, o_im_p)):
                stg = A.alloc(128, "hostg")
                p = psum()
                transpose(p, p.ap[0:32, 0:128], S5ST.ap[:, l * 64 + ri * 32:l * 64 + (ri + 1) * 32], ident, [S5ST, CONST])
                cp(stg.ap[0:32, 0:128], p.ap[0:32, 0:128], [p], [stg])
                T.dma("sp", dstt[l], stg.ap[0:32, 0:128], reads=[stg])
        A.release(wm)
        gb = A.alloc(nt, "glusig")
        for cp_ in range(4):
            wg = WSR.get(slabK("glu_w", l, cp_ * 256, 256, 8))
            wgv = bf(wg)[:, 0:2048].rearrange("p (k n) -> p k n", k=8)
            for q in range(2):
                c = cp_ * 2 + q
                p = psum()
                for k in range(8):
                    mm(p, p.ap[:, 0:nt], wgv[:, k, q * 128:(q + 1) * 128], z0v[:, k, :], k == 0, k == 7, [wg, Z0])
                act(gb.ap[:, 0:nt], p.ap[:, 0:nt], AF.Sigmoid, [p, VEC8], [gb],
                    bias=VEC8.ap[:, (l * 3 + 1) * 8 + c:(l * 3 + 1) * 8 + c + 1])
                tt(zv[:, c, :], z0v[:, c, :], gb.ap[:, 0:nt], ALU.mult, [Z0, gb], [Z])
        A.release(m0)
        AR.release(mr0)

    def mixer(ti, nt, l):
        m = A.mark()
        rmsnorm(nt, l * 3 + 1, "xn")
        OALL = A.alloc(H * nt // 2, "OALL")
        mrope = A.mark()
        ROPE_H[0] = A.alloc(2 * NT, "rope")
        T.dma("sp", ROPE_H[0].ap[:, 0:2 * NT].rearrange("p (a n) -> p a n", a=2), rope[ti].rearrange("a p n -> p a n"),
              writes=[ROPE_H[0]])
        if ti < NTILE:
            retention_prompt(ti, l, OALL)
        else:
            retention_sample(l, OALL)
        A.release(mrope)
        Z = None
        if debug_stage >= 4:
            Z = A.alloc(8 * nt // 2, "Z")
            s5_branch(ti, nt, l, Z)
        merge_and_out(nt, l, OALL, Z, debug_stage >= 4)
        A.release(m)

    def tile_program(ti, nt, src, dst):
        load_x(src, nt)
        for l in range(DEPTH):
            if debug_stage >= 2:
                rmsnorm(nt, l * 3 + 0, "xn")
                ffn(nt, l, "ffn1")
            if debug_stage >= 3:
                mixer(ti, nt, l)
            if debug_stage >= 2:
                rmsnorm(nt, l * 3 + 2, "xn")
                ffn(nt, l, "ffn2")
        rmsnorm(nt, 12, "y")
        store_y(dst, nt)

    def program():
        WSR.reset()
        ps_rr[0] = 0
        A.release(top0)
        AR.release(topr0)
        T.dma("sp", CONST.ap, consts, writes=[CONST])
        T.dma("pool", fr(CONSTR), constsr, writes=[CONSTR])
        m = A.mark()
        stg = A.alloc(256, "gstage")
        for r0, nr in ((0, 128), (128, 80)):
            T.dma("sp", stg.ap[0:nr, 0:128], gains.rearrange("r c p -> (r c) p")[r0:r0 + nr, :], writes=[stg])
            p = psum()
            transpose(p, p.ap[:, 0:nr], stg.ap[0:nr, 0:128], ident[0:nr, 0:nr], [stg, CONST])
            cp(GAIN.ap[:, r0:r0 + nr], p.ap[:, 0:nr], [p], [GAIN])
        T.dma("sp", stg.ap[0:96, 128:256], vec8.rearrange("r c p -> (r c) p"), writes=[stg])
        p = psum()
        transpose(p, p.ap[:, 0:96], stg.ap[0:96, 128:256], ident[0:96, 0:96], [stg, CONST])
        cp(VEC8.ap[:, 0:96], p.ap[:, 0:96], [p], [VEC8])
        A.release(m)
        T.issue("dve", lambda e: e.memset(S5ST.ap, 0.0), writes=[S5ST])
        for ti in range(NTILE):
            tile_program(ti, NT, xp[ti * NT:(ti + 1) * NT, :], yp[ti * NT:(ti + 1) * NT, :])
        tile_program(NTILE, NS, xs, ys)
        T.final_wait("sp")

    top0 = A.mark()
    topr0 = AR.mark()
    T.dry = True
    program()
    T.dry = False
    program()
    return nc, es, T


C_ID = 0
C_EPS6 = 128
C_EPS5 = 129
C_MASK = 256
C_QDEC = 384
C_KDI = 384 + 1024
C_MQ = 384 + 2048
C_TV = C_MQ + 512
C_ONE16 = C_TV + 512
C_SEL = C_ONE16 + 16
C_EVEN = C_SEL + 32
C_ODD = C_EVEN + 1
CONST_W = (C_ODD + 1 + 127) // 128 * 128
R_PERM = 0
R_ONES = 128
R_ID = 256
R_ODIV = 384
CONSTR_W = 512


def make_consts():
    c = np.zeros((128, CONST_W), np.float32)
    c[:, C_ID:C_ID + 128] = np.eye(128, dtype=np.float32)
    c[:, C_EPS6] = 1e-6
    c[:, C_EPS5] = 1e-5
    i = np.arange(128)
    c[:, C_MASK:C_MASK + 128] = (i[None, :] >= i[:, None]).astype(np.float32)
    for h in range(H):
        g = 1.0 - 2.0 ** (-5.0 - h)
        c[:, C_QDEC + h * 128:C_QDEC + (h + 1) * 128] = (g ** (i + 1.0))[None, :]
        c[:, C_KDI + h * 128:C_KDI + (h + 1) * 128] = (g ** (-(i + 1.0)))[None, :]
    gl = i // 64
    g8 = i // 16
    for q in range(4):
        c[:, C_MQ + q * 128:C_MQ + (q + 1) * 128] = (g8[None, :] == (2 * q + gl[:, None])).astype(np.float32)
    c[:, C_TV:C_TV + 512] = (np.arange(512) + 1.0)[None, :]
    c[:, C_ONE16:C_ONE16 + 16] = 1.0
    gg = np.arange(64)
    c[0:64, C_SEL:C_SEL + 32] = (gg[:, None] // 2 == np.arange(32)[None, :]).astype(np.float32)
    c[0:64, C_EVEN] = (gg % 2 == 0)
    c[0:64, C_ODD] = (gg % 2 == 1)
    return c


def make_constsr():
    c = np.zeros((128, CONSTR_W), np.float32)
    pm = np.zeros((128, 128), np.float32)
    for m_ in range(128):
        pm[(m_ + 64) % 128, m_] = 1.0
    c[:, R_PERM:R_PERM + 128] = pm
    c[:, R_ONES:R_ONES + 128] = 1.0
    c[:, R_ID:R_ID + 128] = np.eye(128, dtype=np.float32)
    c[:, R_ODIV:R_ODIV + 128] = 1.0 / 128.0
    return c


def make_rope():
    half = 64
    inv = (10000.0 ** (-np.arange(half, dtype=np.float32) / half)).astype(np.float32)
    out = np.zeros((NTILE + 1, 2, 128, NT), np.float32)
    for ti in range(NTILE + 1):
        if ti < NTILE:
            pos = np.arange(ti * NT, (ti + 1) * NT, dtype=np.float32)
        else:
            pos = np.full((NT,), float(PAST), np.float32)
        ang = (pos[None, :] * inv[:, None]).astype(np.float32)
        cs = np.cos(ang).astype(np.float32)
        sn = np.sin(ang).astype(np.float32)
        out[ti, 0, :64] = cs
        out[ti, 0, 64:] = cs
        out[ti, 1, :64] = -sn
        out[ti, 1, 64:] = sn
    return out


_CACHE = {}


def kernel(**inp):
    stage = inp.pop("_debug_stage", 99)
    ncores = inp.pop("_debug_cores", 8)
    if stage not in _CACHE:
        _CACHE[stage] = build(stage)
    nc, es, T = _CACHE[stage]
    f = lambda a: np.ascontiguousarray(np.asarray(a, dtype=np.float32))
    gains = np.zeros((13, 16, 128), np.float32)
    vec8 = np.zeros((12, 8, 128), np.float32)
    for l in range(DEPTH):
        gains[l * 3 + 0] = f(inp["ffn1_norm"][l]).reshape(16, 128)
        gains[l * 3 + 1] = f(inp["mix_norm"][l]).reshape(16, 128)
        gains[l * 3 + 2] = f(inp["ffn2_norm"][l]).reshape(16, 128)
        vec8[l * 3 + 0] = f(inp["ret_gn"][l]).reshape(8, 128)
        vec8[l * 3 + 1] = f(inp["glu_b"][l]).reshape(8, 128)
        vec8[l * 3 + 2] = f(inp["s5_d"][l]).reshape(8, 128)
    gains[12] = f(inp["final_norm"]).reshape(16, 128)
    shared = {k: f(inp[k]) for k in ("ffn1_w1", "ffn1_w3", "ffn1_w2", "w_in", "ret_proj", "glu_w", "s5_proj",
                                     "w_out", "ffn2_w1", "ffn2_w3", "ffn2_w2", "s5_lam_re", "s5_lam_im",
                                     "s5_b_re", "s5_b_im")}
    shared["s5_log_step"] = f(inp["s5_log_step"]).reshape(DEPTH, 64, 1)
    shared["s5_c_re"] = f(inp["s5_c_re"]).reshape(DEPTH, 1024, 64)
    shared["s5_c_im"] = f(inp["s5_c_im"]).reshape(DEPTH, 1024, 64)
    shared["gains"] = gains
    shared["vec8"] = vec8
    shared["consts"] = make_consts()
    shared["constsr"] = make_constsr()
    shared["rope"] = make_rope()
    xpv = f(inp["x_prompt"])
    xsv = f(inp["x_sample"]).reshape(128, D)
    sret = f(inp["state_ret"])
    sre = f(inp["state_s5_re"]).reshape(DEPTH, 128, 4096)
    sim = f(inp["state_s5_im"]).reshape(DEPTH, 128, 4096)
    in_maps = []
    for c in range(ncores):
        d = dict(shared)
        d["xp"] = xpv[c % 4]
        d["xs"] = np.ascontiguousarray(xsv[c * NS:(c + 1) * NS])
        d["st_ret"] = np.ascontiguousarray(sret[:, c * NS:(c + 1) * NS])
        d["st_re"] = np.ascontiguousarray(sre[:, c * NS:(c + 1) * NS])
        d["st_im"] = np.ascontiguousarray(sim[:, c * NS:(c + 1) * NS])
        in_maps.append(d)
    res = run_bass_kernel_spmd(nc, in_maps, core_ids=list(range(ncores)))
    R = res.results
    if ncores < 8:
        return R
    y_p = np.stack([R[c]["yp"] for c in range(4)]).reshape(4, SEQ, D)
    y_s = np.concatenate([R[c]["ys"] for c in range(8)]).reshape(128, 1, D)
    ret_p = np.stack([R[c]["o_ret_p"] for c in range(4)], axis=1)
    re_p = np.stack([R[c]["o_re_p"].reshape(DEPTH, 64, 64) for c in range(4)], axis=1)
    im_p = np.stack([R[c]["o_im_p"].reshape(DEPTH, 64, 64) for c in range(4)], axis=1)
    ret_s = np.concatenate([R[c]["o_ret_s"] for c in range(8)], axis=1)
    re_s = np.concatenate([R[c]["o_re_s"].reshape(DEPTH, NS, 64, 64) for c in range(8)], axis=1)
    im_s = np.concatenate([R[c]["o_im_s"].reshape(DEPTH, NS, 64, 64) for c in range(8)], axis=1)
    return (y_p, y_s, ret_p, re_p, im_p, ret_s, re_s, im_s)
```

```python
import math
from contextlib import ExitStack
import numpy as np
import concourse.bass as bass
import concourse.mybir as mybir
from concourse.bass_utils import run_bass_kernel_spmd

F32 = mybir.dt.float32
F32R = mybir.dt.float32r
BF16 = mybir.dt.bfloat16
I32 = mybir.dt.int32
AF = mybir.ActivationFunctionType
ALU = mybir.AluOpType

D = 2048
DFF = 5632
DEPTH = 4
NCH = D // 128
NJ = DFF // 128
H = 8
NT = 512
NS = 16
SEQ = 2048
NTILE = SEQ // NT
PAST = 16384
INW = 9216
SEM_LIMIT = 30000
NWS = 5
NDMASEM = 6
SLOTW = 128
ARENA_W = 43 * 1024
ARENA_R_W = 7 * 1024


class SemObj:
    __slots__ = ("sem", "count")

    def __init__(self, sem):
        self.sem = sem
        self.count = 0


class Eng:
    def __init__(self, name, obj, T):
        self.name = name
        self.obj = obj
        self.T = T
        self.cur = None
        self.seen = {}

    def mark(self):
        if self.cur is None or self.cur.count >= SEM_LIMIT:
            self.cur = self.T.new_sem(self.name)
        self.cur.count += 1
        return (self.cur, self.cur.count)


class Buf:
    __slots__ = ("ap", "slots", "name")

    def __init__(self, ap, slots, name=""):
        self.ap = ap
        self.slots = slots
        self.name = name


class Tracker:
    def __init__(self, nc, es):
        self.nc = nc
        self.es = es
        self.dry = False
        self.nsem = 0
        self.engs = {}
        for name, obj in (("pe", nc.tensor), ("act", nc.scalar), ("dve", nc.vector),
                          ("pool", nc.gpsimd), ("sp", nc.sync)):
            self.engs[name] = Eng(name, obj, self)
        self.dma_sems = {}
        self.dma_rr = {}
        self.state = {}
        self.n_inst = 0

    def new_sem(self, name):
        self.nsem += 1
        return SemObj(self.es.enter_context(self.nc.semaphore(f"s_{name}_{self.nsem}")))

    def _deps(self, reads, writes):
        deps = {}
        st = self.state
        for b in reads:
            for s in b.slots:
                e = st.get(s)
                if e is not None and e[0] is not None:
                    so, v = e[0]
                    if deps.get(so, 0) < v:
                        deps[so] = v
        for b in writes:
            for s in b.slots:
                e = st.get(s)
                if e is not None:
                    if e[0] is not None:
                        so, v = e[0]
                        if deps.get(so, 0) < v:
                            deps[so] = v
                    for so, v in e[1].items():
                        if deps.get(so, 0) < v:
                            deps[so] = v
        return deps

    def _update(self, mark, reads, writes):
        st = self.state
        so, v = mark
        for b in reads:
            for s in b.slots:
                e = st.get(s)
                if e is None:
                    st[s] = [None, {so: v}]
                else:
                    e[1][so] = v
        for b in writes:
            for s in b.slots:
                st[s] = [mark, {}]

    def _wait(self, E, deps, skip_self=None):
        for so, v in deps.items():
            if skip_self is not None and so is skip_self:
                continue
            if E.seen.get(so, 0) < v:
                E.obj.wait_ge(so.sem, v)
                E.seen[so] = v

    def issue(self, eng, fn, reads=(), writes=()):
        if self.dry:
            return
        E = self.engs[eng]
        deps = self._deps(reads, writes)
        self._wait(E, deps, skip_self=(E.cur if eng == "pe" else None))
        inst = fn(E.obj)
        mark = E.mark()
        inst.then_inc(mark[0].sem, 1)
        self._update(mark, reads, writes)
        self.n_inst += 1

    def dma(self, queue, out, in_, reads=(), writes=()):
        if self.dry:
            return
        E = self.engs[queue]
        lst = self.dma_sems.setdefault(queue, [])
        rr = self.dma_rr.get(queue, 0)
        self.dma_rr[queue] = rr + 1
        if len(lst) < NDMASEM:
            lst.append(self.new_sem("dma" + queue))
        so = lst[rr % NDMASEM]
        deps = self._deps(reads, writes)
        if so.count > 0:
            deps[so] = max(deps.get(so, 0), so.count)
        self._wait(E, deps)
        inst = E.obj.dma_start(out=out, in_=in_)
        so.count += 16
        inst.then_inc(so.sem, 16)
        self._update((so, so.count), reads, writes)
        self.n_inst += 1

    def final_wait(self, eng="sp"):
        if self.dry:
            return
        E = self.engs[eng]
        for q, lst in self.dma_sems.items():
            for so in lst:
                if so.count > 0 and E.seen.get(so, 0) < so.count:
                    E.obj.wait_ge(so.sem, so.count)
                    E.seen[so] = so.count
        for name, e2 in self.engs.items():
            if e2.cur is not None and e2.cur.count > 0 and name != eng:
                E.obj.wait_ge(e2.cur.sem, e2.cur.count)


class Arena:
    def __init__(self, nc, es, T, name="arena", width=None, space="sb"):
        self.W = width
        self.space = space
        self.t = es.enter_context(nc.sbuf_tensor(name, [128, width], F32))
        self.top = 0
        self.T = T

    def alloc(self, words, name=""):
        words = (words + SLOTW - 1) // SLOTW * SLOTW
        o = self.top
        self.top += words
        assert self.top <= self.W, f"arena overflow at {name}: {self.top}"
        return self.view(o, words, name)

    def view(self, o, words, name=""):
        ap = self.t[:, o:o + words]
        slots = [(self.space, i) for i in range(o // SLOTW, (o + words) // SLOTW)]
        return Buf(ap, slots, name)

    def mark(self):
        return self.top

    def release(self, m):
        self.top = m


def sub(buf, o, words):
    s0 = buf.slots[0][1] + o // SLOTW
    s1 = buf.slots[0][1] + (o + words + SLOTW - 1) // SLOTW
    return Buf(buf.ap[:, o:o + words], [(buf.slots[0][0], i) for i in range(s0, s1)], buf.name)


def bf(buf):
    return buf.ap.bitcast(BF16)


def fr(buf):
    return buf.ap.bitcast(F32R)


def build(debug_stage=99):
    nc = bass.Bass("TRN2", target_bir_lowering=False)
    es = ExitStack()
    T = Tracker(nc, es)
    A = Arena(nc, es, T, "arena", ARENA_W, "sb")
    AR = Arena(nc, es, T, "arena_r", ARENA_R_W, "sr")

    def din(name, shape, dt=F32):
        return nc.dram_tensor(name, list(shape), dt, kind="ExternalInput").ap()

    def dout(name, shape):
        return nc.dram_tensor(name, list(shape), F32, kind="ExternalOutput").ap()

    xp = din("xp", [SEQ, D])
    xs = din("xs", [NS, D])
    st_ret = din("st_ret", [DEPTH, NS, H, 128, 128])
    st_re = din("st_re", [DEPTH, NS, 4096])
    st_im = din("st_im", [DEPTH, NS, 4096])
    Wd = {}
    for nm, shp in (("ffn1_w1", [DEPTH, D, DFF]), ("ffn1_w3", [DEPTH, D, DFF]), ("ffn1_w2", [DEPTH, DFF, D]),
                    ("w_in", [DEPTH, D, INW]), ("ret_proj", [DEPTH, 1024, D]), ("glu_w", [DEPTH, 1024, 1024]),
                    ("s5_proj", [DEPTH, 1024, D]), ("w_out", [DEPTH, D, D]),
                    ("ffn2_w1", [DEPTH, D, DFF]), ("ffn2_w3", [DEPTH, D, DFF]), ("ffn2_w2", [DEPTH, DFF, D])):
        Wd[nm] = din(nm, shp)
    gains = din("gains", [13, 16, 128])
    vec8 = din("vec8", [12, 8, 128])
    lam_re = din("s5_lam_re", [DEPTH, 64, 64])
    lam_im = din("s5_lam_im", [DEPTH, 64, 64])
    logstep = din("s5_log_step", [DEPTH, 64, 1])
    b_re = din("s5_b_re", [DEPTH, 64, 64, 16])
    b_im = din("s5_b_im", [DEPTH, 64, 64, 16])
    c_re = din("s5_c_re", [DEPTH, 1024, 64])
    c_im = din("s5_c_im", [DEPTH, 1024, 64])
    consts = din("consts", [128, CONST_W])
    constsr = din("constsr", [128, CONSTR_W])
    rope = din("rope", [NTILE + 1, 2, 128, NT])

    yp = dout("yp", [SEQ, D])
    ys = dout("ys", [NS, D])
    o_ret_p = dout("o_ret_p", [DEPTH, H, 128, 128])
    o_re_p = dout("o_re_p", [DEPTH, 32, 128])
    o_im_p = dout("o_im_p", [DEPTH, 32, 128])
    o_ret_s = dout("o_ret_s", [DEPTH, NS, H, 128, 128])
    o_re_s = dout("o_re_s", [DEPTH, NS * 32, 128])
    o_im_s = dout("o_im_s", [DEPTH, NS * 32, 128])
    scr_ret = nc.dram_tensor("scr_ret", [DEPTH, H, 128, 128], F32, kind="Internal").ap()
    scr_buf = [[Buf(None, [("dram", l, h)]) for h in range(H)] for l in range(DEPTH)]

    PS = []
    for i in range(8):
        t = es.enter_context(nc.psum_tensor(f"ps{i}", [128, 512], F32))
        PS.append(Buf(t[:], [("ps", i)], f"ps{i}"))
    ps_rr = [0]

    ps_free = list(range(8))

    def psum():
        b = PS[ps_free[ps_rr[0] % len(ps_free)]]
        ps_rr[0] += 1
        return b

    def psum_pin():
        b = psum()
        ps_free.remove(b.slots[0][1])
        return b

    def psum_unpin(b):
        ps_free.append(b.slots[0][1])
        ps_free.sort()

    CONST = A.alloc(CONST_W, "const")
    CONSTR = AR.alloc(CONSTR_W, "constr")
    X = A.alloc(NCH * NT, "X")
    XN = A.alloc(NCH * NT // 2, "XN")
    WS = [A.alloc(2048, f"ws{i}") for i in range(NWS)]
    GAIN = A.alloc(13 * 16, "gain")
    VEC8 = A.alloc(12 * 8, "vec8")
    ROPE_H = [None]
    S5ST = A.alloc(DEPTH * 64, "s5st")

    cv = CONST.ap
    ident = cv[:, C_ID:C_ID + 128]
    cr = fr(CONSTR)
    perm_r = cr[:, R_PERM:R_PERM + 128]
    ones_r = cr[:, R_ONES:R_ONES + 128]

    def cbuf():
        return CONST

    class WStream:
        def __init__(self, slots):
            self.slots = slots
            self.N = len(slots)
            self.sched = []
            self.pos = 0
            self.issued = 0

        def reset(self):
            self.pos = 0
            self.issued = 0

        def get(self, mk):
            N = self.N
            if T.dry:
                self.sched.append(mk)
                return self.slots[(len(self.sched) - 1) % N]
            idx = self.pos
            self.pos += 1
            while self.issued < min(len(self.sched), idx + N - 1):
                slot = self.slots[self.issued % N]
                for (o_ap, i_ap) in self.sched[self.issued](slot):
                    T.dma("pool", o_ap, i_ap, writes=[slot])
                self.issued += 1
            return self.slots[idx % N]

    X_FULL = list(X.slots)
    XN_FULL = list(XN.slots)
    WS_EXTRA = [sub(X, 2048, 2048), sub(X, 4096, 2048), sub(X, 6144, 2048), sub(XN, 2048, 2048)]
    WSR_MAIN = WStream(WS)
    WSR_SAMP = WStream(WS + WS_EXTRA)

    class _WH:
        cur = WSR_MAIN

        @staticmethod
        def get(mk):
            return _WH.cur.get(mk)

        @staticmethod
        def reset():
            WSR_MAIN.reset()
            WSR_SAMP.reset()
            _WH.cur = WSR_MAIN

    WSR = _WH

    def slabK(name, l, c0, ncol, kch):
        def mk(slot):
            o = bf(slot)[:, 0:kch * ncol].rearrange("p (k n) -> p k n", k=kch)
            i = Wd[name][l, :, c0:c0 + ncol].rearrange("(k p) n -> p k n", p=128)
            return [(o, i)]
        return mk

    def slabR(name, l, r0, nr):
        def mk(slot):
            o = bf(slot)[:, 0:nr * 2048].rearrange("p (j n) -> p j n", j=nr)
            i = Wd[name][l, r0 * 128:(r0 + nr) * 128, :].rearrange("(j p) n -> p j n", p=128)
            return [(o, i)]
        return mk

    def mm(out_buf, out_ap, lhsT, rhs, start, stop, reads):
        T.issue("pe", lambda e: e.matmul(out_ap, lhsT=lhsT, rhs=rhs, start=start, stop=stop),
                reads=reads, writes=[out_buf])

    def transpose(out_buf, out_ap, in_ap, idn, reads):
        T.issue("pe", lambda e: e.transpose(out_ap, in_ap, idn), reads=reads, writes=[out_buf])

    def act(out_ap, in_ap, func, reads, writes, scale=1.0, bias=0.0):
        T.issue("act", lambda e: e.activation(out=out_ap, in_=in_ap, func=func, bias=bias, scale=scale),
                reads=reads, writes=writes)

    def tt(out_ap, a, b, op, reads, writes, eng="dve"):
        T.issue(eng, lambda e: e.tensor_tensor(out=out_ap, in0=a, in1=b, op=op), reads=reads, writes=writes)

    def ts(out_ap, a, s1, s2, op0, op1, reads, writes, eng="dve"):
        T.issue(eng, lambda e: e.tensor_scalar(out=out_ap, in0=a, scalar1=s1, scalar2=s2, op0=op0, op1=op1),
                reads=reads, writes=writes)

    def stt(out_ap, a, s, b, op0, op1, reads, writes):
        T.issue("dve", lambda e: e.scalar_tensor_tensor(out=out_ap, in0=a, scalar=s, in1=b, op0=op0, op1=op1),
                reads=reads, writes=writes)

    def cp(out_ap, in_ap, reads, writes, eng="dve"):
        T.issue(eng, lambda e: e.tensor_copy(out=out_ap, in_=in_ap), reads=reads, writes=writes)

    def xv(nt):
        return X.ap[:, 0:NCH * nt].rearrange("p (c n) -> p c n", c=NCH)

    def xnv(nt):
        return bf(XN)[:, 0:NCH * nt].rearrange("p (c n) -> p c n", c=NCH)

    def load_x(src, nt):
        m = A.mark()
        nb = (nt + 127) // 128
        rows = min(nt, 128)
        for b in range(nb):
            stg = A.alloc(D, "xstage")
            T.dma("sp", stg.ap[0:rows, :], src[b * 128:b * 128 + rows, :], writes=[stg])
            for c4 in range(4):
                p = psum()
                for q in range(4):
                    c = c4 * 4 + q
                    transpose(p, p.ap[:, q * 128:q * 128 + rows], stg.ap[0:rows, c * 128:(c + 1) * 128],
                              ident[0:rows, 0:rows], [stg, CONST])
                o = xv(nt)[:, c4 * 4:(c4 + 1) * 4, b * 128:b * 128 + rows]
                i = p.ap.rearrange("p (q n) -> p q n", q=4)[:, :, 0:rows]
                (cp if (c4 % 2 == 0) else (lambda o_, i_, r_, w_: act(o_, i_, AF.Copy, r_, w_)))(o, i, [p], [X])
            if b % 2 == 1:
                A.release(m)
        A.release(m)

    def rmsnorm(nt, grow, out_kind):
        m = A.mark()
        mr = AR.mark()
        sq = [AR.alloc(nt, "sq0"), AR.alloc(nt, "sq1")]
        rstd = A.alloc(nt, "rstd")
        p = psum()
        for c in range(NCH):
            s = sq[c % 2]
            act(fr(s)[:, 0:nt], xv(nt)[:, c, :], AF.Square, [X], [s])
            mm(p, p.ap[:, 0:nt], ones_r, fr(s)[:, 0:nt], c == 0, c == NCH - 1, [s, CONSTR])
        act(rstd.ap[:, 0:nt], p.ap[:, 0:nt], AF.Sqrt, [p, CONST], [rstd], scale=1.0 / D,
            bias=cv[:, C_EPS6:C_EPS6 + 1])
        T.issue("dve", lambda e: e.reciprocal(out=rstd.ap[:, 0:nt], in_=rstd.ap[:, 0:nt]), reads=[rstd], writes=[rstd])
        g = GAIN.ap[:, grow * 16:(grow + 1) * 16]
        for c in range(NCH):
            if out_kind == "xn":
                stt(xnv(nt)[:, c, :], xv(nt)[:, c, :], g[:, c:c + 1], rstd.ap[:, 0:nt], ALU.mult, ALU.mult,
                    [X, GAIN, rstd], [XN])
            else:
                stt(xv(nt)[:, c, :], xv(nt)[:, c, :], g[:, c:c + 1], rstd.ap[:, 0:nt], ALU.mult, ALU.mult,
                    [X, GAIN, rstd], [X])
        A.release(m)
        AR.release(mr)

    def ffn(nt, l, pref):
        m = A.mark()
        GJ = 8
        Hb = A.alloc(GJ * nt // 2, "H")
        hv = bf(Hb)[:, 0:GJ * nt].rearrange("p (j n) -> p j n", j=GJ)
        sl = [A.alloc(nt, "silu0"), A.alloc(nt, "silu1")]
        xn = xnv(nt)
        j = 0
        cnt = 0
        while j < NJ:
            gj = min(GJ, NJ - j)
            for jj in range(0, gj, 2):
                w1 = WSR.get(slabK(pref + "_w1", l, (j + jj) * 128, 256, 16))
                w3 = WSR.get(slabK(pref + "_w3", l, (j + jj) * 128, 256, 16))
                w1v = bf(w1)[:, 0:4096].rearrange("p (k n) -> p k n", k=16)
                w3v = bf(w3)[:, 0:4096].rearrange("p (k n) -> p k n", k=16)
                for q in range(2):
                    p1 = psum()
                    p3 = psum()
                    for k in range(16):
                        mm(p1, p1.ap[:, 0:nt], w1v[:, k, q * 128:(q + 1) * 128], xn[:, k, :], k == 0, k == 15, [w1, XN])
                    for k in range(16):
                        mm(p3, p3.ap[:, 0:nt], w3v[:, k, q * 128:(q + 1) * 128], xn[:, k, :], k == 0, k == 15, [w3, XN])
                    s = sl[cnt % 2]
                    cnt += 1
                    act(s.ap[:, 0:nt], p1.ap[:, 0:nt], AF.Silu, [p1], [s])
                    tt(hv[:, jj + q, :], s.ap[:, 0:nt], p3.ap[:, 0:nt], ALU.mult, [s, p3], [Hb])
            for c2 in range(8):
                def mk(slot, c2=c2, j=j, gj=gj):
                    o = bf(slot)[:, 0:gj * 256].rearrange("p (r n) -> p r n", r=gj)
                    i = Wd[pref + "_w2"][l, j * 128:(j + gj) * 128, c2 * 256:(c2 + 1) * 256] \
                        .rearrange("(r p) n -> p r n", p=128)
                    return [(o, i)]
                w2 = WSR.get(mk)
                wv = bf(w2)[:, 0:gj * 256].rearrange("p (r n) -> p r n", r=gj)
                for q in range(2):
                    p = psum()
                    for ji in range(gj):
                        mm(p, p.ap[:, 0:nt], wv[:, ji, q * 128:(q + 1) * 128], hv[:, ji, :], ji == 0, ji == gj - 1,
                           [w2, Hb])
                    cc = c2 * 2 + q
                    stt(xv(nt)[:, cc, :], p.ap[:, 0:nt], 0.5, xv(nt)[:, cc, :], ALU.mult, ALU.add, [p, X], [X])
            j += gj
        A.release(m)

    def store_y(dst, nt):
        m = A.mark()
        nb = (nt + 127) // 128
        rows = min(nt, 128)
        for b in range(nb):
            stg = A.alloc(D, "ystage")
            for c4 in range(4):
                p = psum()
                for q in range(4):
                    c = c4 * 4 + q
                    transpose(p, p.ap[0:rows, q * 128:(q + 1) * 128], xv(nt)[:, c, b * 128:b * 128 + rows], ident, [X, CONST])
                o = stg.ap[0:rows, c4 * 512:(c4 + 1) * 512]
                if c4 % 2 == 0:
                    cp(o, p.ap[0:rows, :], [p], [stg])
                else:
                    act(o, p.ap[0:rows, :], AF.Copy, [p], [stg])
            T.dma("sp", dst[b * 128:b * 128 + rows, :], stg.ap[0:rows, :], reads=[stg])
            if b % 2 == 1:
                A.release(m)
        A.release(m)


    GH = [1.0 - 2.0 ** (-5.0 - h) for h in range(H)]
    GC = [g ** 128 for g in GH]
    maskT = cv[:, C_MASK:C_MASK + 128]
    ident_r = cr[:, R_ID:R_ID + 128]
    onesdiv_r = cr[:, R_ODIV:R_ODIV + 128]

    def head_norm(po_ap, n, l, head, srg_ap, srg_buf, out_ap, out_buf, pbuf):
        m = A.mark()
        mr = AR.mark()
        osb = AR.alloc(n, "osb")
        o2 = AR.alloc(n, "o2")
        mean = A.alloc(n, "mean")
        var = A.alloc(n, "var")
        t1 = A.alloc(n, "hn_t1")
        act(fr(osb)[:, 0:n], po_ap, AF.Copy, [pbuf], [osb])
        act(fr(o2)[:, 0:n], po_ap, AF.Square, [pbuf], [o2])
        pm = psum()
        mm(pm, pm.ap[:, 0:n], onesdiv_r, fr(osb)[:, 0:n], True, True, [CONSTR, osb])
        pv = psum()
        mm(pv, pv.ap[:, 0:n], onesdiv_r, fr(o2)[:, 0:n], True, True, [CONSTR, o2])
        act(mean.ap[:, 0:n], pm.ap[:, 0:n], AF.Copy, [pm], [mean])
        tt(var.ap[:, 0:n], mean.ap[:, 0:n], mean.ap[:, 0:n], ALU.mult, [mean], [var])
        tt(var.ap[:, 0:n], pv.ap[:, 0:n], var.ap[:, 0:n], ALU.subtract, [pv, var], [var])
        act(var.ap[:, 0:n], var.ap[:, 0:n], AF.Sqrt, [var, CONST], [var], bias=cv[:, C_EPS5:C_EPS5 + 1])
        T.issue("dve", lambda e: e.reciprocal(out=var.ap[:, 0:n], in_=var.ap[:, 0:n]), reads=[var], writes=[var])
        tt(t1.ap[:, 0:n], osb.ap[:, 0:n], mean.ap[:, 0:n], ALU.subtract, [osb, mean], [t1])
        tt(t1.ap[:, 0:n], t1.ap[:, 0:n], var.ap[:, 0:n], ALU.mult, [t1, var], [t1])
        gn = VEC8.ap[:, (l * 3 + 0) * 8 + head:(l * 3 + 0) * 8 + head + 1]
        stt(out_ap, t1.ap[:, 0:n], gn, srg_ap, ALU.mult, ALU.mult, [t1, VEC8, srg_buf], [out_buf])
        A.release(m)
        AR.release(mr)

    def proj16(slab, q, n, ncol=256):
        wv = bf(slab)[:, 0:16 * ncol].rearrange("p (k n) -> p k n", k=16)
        p = psum()
        for k in range(16):
            mm(p, p.ap[:, 0:n], wv[:, k, q * 128:(q + 1) * 128], xnv(n)[:, k, :], k == 0, k == 15, [slab, XN])
        return p

    def rotary(p, n, scale, tab_mul, name, res=None):
        if res is None:
            res = AR.alloc(n, name + "rot")
        m_ = A.mark()
        mr_ = AR.mark()
        raw = AR.alloc(n, name + "raw")
        t1 = A.alloc(n, name + "t1")
        act(fr(raw)[:, 0:n], p.ap[:, 0:n], AF.Copy, [p], [raw], scale=scale)
        pr = psum()
        mm(pr, pr.ap[:, 0:n], perm_r, fr(raw)[:, 0:n], True, True, [CONSTR, raw])
        tt(t1.ap[:, 0:n], raw.ap[:, 0:n], ROPE_H[0].ap[:, 0:n], ALU.mult, [raw, ROPE_H[0]], [t1])
        if tab_mul is None:
            tt(fr(res)[:, 0:n], pr.ap[:, 0:n], ROPE_H[0].ap[:, NT:NT + n], ALU.mult, [pr, ROPE_H[0]], [res])
            tt(fr(res)[:, 0:n], res.ap[:, 0:n], t1.ap[:, 0:n], ALU.add, [res, t1], [res])
        else:
            t2 = A.alloc(n, name + "t2")
            tt(t2.ap[:, 0:n], pr.ap[:, 0:n], ROPE_H[0].ap[:, NT:NT + n], ALU.mult, [pr, ROPE_H[0]], [t2])
            tt(t1.ap[:, 0:n], t1.ap[:, 0:n], t2.ap[:, 0:n], ALU.add, [t1, t2], [t1])
            tt(fr(res)[:, 0:n].rearrange("p (c i) -> p c i", i=128),
               t1.ap[:, 0:n].rearrange("p (c i) -> p c i", i=128),
               tab_mul.unsqueeze(1).broadcast_to([128, n // 128, 128]), ALU.mult, [t1, CONST], [res])
        A.release(m_)
        AR.release(mr_)
        return res

    def retention_prompt(ti, l, OALL):
        oall = bf(OALL)[:, 0:H * NT].rearrange("p (h n) -> p h n", h=H)
        n = NT
        ncx = n // 128
        for hp in range(4):
            m = A.mark()
            mr = AR.mark()
            wv_s = WSR.get(slabK("w_in", l, 2048 + hp * 256, 256, 16))
            wvv = bf(wv_s)[:, 0:4096].rearrange("p (k n) -> p k n", k=16)
            vtok = AR.alloc(ncx * 256, "vtok")
            vtv = fr(vtok)[:, 0:ncx * 256].rearrange("p (c e) -> p c e", c=ncx)
            for c2 in range(ncx // 2):
                pv = psum()
                for cc in range(2):
                    c = c2 * 2 + cc
                    for k in range(16):
                        mm(pv, pv.ap[:, cc * 256:(cc + 1) * 256], xnv(n)[:, k, c * 128:(c + 1) * 128], wvv[:, k, :],
                           k == 0, k == 15, [XN, wv_s])
                act(fr(vtok)[:, c2 * 512:(c2 + 1) * 512], pv.ap, AF.Copy, [pv], [vtok])
            qd = [AR.alloc(n, "qd0"), AR.alloc(n, "qd1")]
            kd = [AR.alloc(n, "kd0"), AR.alloc(n, "kd1")]
            wq_s = WSR.get(slabK("w_in", l, hp * 256, 256, 16))
            for hh in range(2):
                pq = proj16(wq_s, hh, n)
                rotary(pq, n, 128.0 ** -0.5, cv[:, C_QDEC + (2 * hp + hh) * 128:C_QDEC + (2 * hp + hh + 1) * 128], f"q{hh}", qd[hh])
            wk_s = WSR.get(slabK("w_in", l, 1024 + hp * 256, 256, 16))
            for hh in range(2):
                pk = proj16(wk_s, hh, n)
                rotary(pk, n, 1.0, cv[:, C_KDI + (2 * hp + hh) * 128:C_KDI + (2 * hp + hh + 1) * 128], f"k{hh}", kd[hh])
            wr_s = WSR.get(slabK("w_in", l, 3072 + hp * 256, 256, 16))
            srg = []
            for hh in range(2):
                pg = proj16(wr_s, hh, n)
                sb = A.alloc(n, f"srg{hh}")
                act(sb.ap[:, 0:n], pg.ap[:, 0:n], AF.Silu, [pg], [sb])
                srg.append(sb)
            for hh in range(2):
                head = 2 * hp + hh
                m2 = A.mark()
                mr2 = AR.mark()
                qv = fr(qd[hh])
                kv = fr(kd[hh])
                ps_ = psum()
                for c in range(ncx):
                    mm(ps_, ps_.ap[:, c * 128:(c + 1) * 128], kv[:, c * 128:(c + 1) * 128], qv[:, c * 128:(c + 1) * 128],
                       True, True, [kd[hh], qd[hh]])
                scT = AR.alloc(n, "scT")
                tt(fr(scT)[:, 0:n].rearrange("p (c i) -> p c i", i=128), ps_.ap[:, 0:n].rearrange("p (c i) -> p c i", i=128),
                   maskT.unsqueeze(1).broadcast_to([128, ncx, 128]), ALU.mult, [ps_, CONST], [scT])
                pt = psum()
                for c in range(ncx):
                    transpose(pt, pt.ap.bitcast(F32R)[:, c * 128:(c + 1) * 128], kv[:, c * 128:(c + 1) * 128], ident_r,
                              [kd[hh], CONSTR])
                kdt = AR.alloc(n, "kdt")
                act(fr(kdt)[:, 0:n], pt.ap[:, 0:n], AF.Copy, [pt], [kdt])
                pu = psum()
                for c in range(ncx):
                    mm(pu, pu.ap[:, c * 128:(c + 1) * 128], fr(kdt)[:, c * 128:(c + 1) * 128],
                       vtv[:, c, hh * 128:(hh + 1) * 128], True, True, [kdt, vtok])
                Sall = AR.alloc((ncx + 1) * 128, "Sall")
                Sv = fr(Sall)[:, 0:(ncx + 1) * 128].rearrange("p (c e) -> p c e", e=128)
                Svf = Sall.ap[:, 0:(ncx + 1) * 128].rearrange("p (c e) -> p c e", e=128)
                if ti == 0:
                    ts(Sv[:, 0, :], pu.ap[:, 0:128], 0.0, None, ALU.mult, ALU.bypass, [pu], [Sall])
                else:
                    T.dma("pool", Sv[:, 0, :], scr_ret[l, head], reads=[scr_buf[l][head]], writes=[Sall])
                tmp = A.alloc(128, "stmp")
                for c in range(ncx):
                    tt(tmp.ap[:, 0:128], pu.ap[:, c * 128:(c + 1) * 128], Svf[:, c, :], ALU.add, [pu, Sall], [tmp])
                    act(Sv[:, c + 1, :], tmp.ap[:, 0:128], AF.Copy, [tmp], [Sall], scale=GC[head])
                dst = o_ret_p[l, head] if ti == NTILE - 1 else scr_ret[l, head]
                T.dma("sp", dst, Svf[:, ncx, :], reads=[Sall], writes=([] if ti == NTILE - 1 else [scr_buf[l][head]]))
                po = psum()
                for c in range(ncx):
                    mm(po, po.ap[:, c * 128:(c + 1) * 128], vtv[:, c, hh * 128:(hh + 1) * 128], fr(scT)[:, c * 128:(c + 1) * 128],
                       True, False, [vtok, scT])
                    mm(po, po.ap[:, c * 128:(c + 1) * 128], Sv[:, c, :], qv[:, c * 128:(c + 1) * 128],
                       False, True, [Sall, qd[hh]])
                head_norm(po.ap[:, 0:n], n, l, head, srg[hh].ap[:, 0:n], srg[hh], oall[:, head, :], OALL, po)
                A.release(m2)
                AR.release(mr2)
            A.release(m)
            AR.release(mr)

    def retention_sample(l, OALL):
        n = NS
        oall = bf(OALL)[:, 0:H * n].rearrange("p (h n) -> p h n", h=H)
        m = A.mark()
        mr = AR.mark()
        QS = A.alloc(H * n, "QS")
        KS = A.alloc(H * n, "KS")
        SRG = A.alloc(H * n, "SRG")
        VT = A.alloc(1024, "VT")
        SEL = A.alloc(n * 128, "SEL")
        cp(SEL.ap[0:n, 0:n * 128].rearrange("p (b d) -> p b d", b=n),
           ident[0:n, 0:n].unsqueeze(2).broadcast_to([n, n, 128]), [CONST], [SEL])
        for hp in range(4):
            m2 = A.mark()
            mr2 = AR.mark()
            wv_s = WSR.get(slabK("w_in", l, 2048 + hp * 256, 256, 16))
            wvv = bf(wv_s)[:, 0:4096].rearrange("p (k n) -> p k n", k=16)
            pv = psum()
            for k in range(16):
                mm(pv, pv.ap[0:n, 0:256], xnv(n)[:, k, :], wvv[:, k, :], k == 0, k == 15, [XN, wv_s])
            cp(VT.ap[0:n, hp * 256:(hp + 1) * 256], pv.ap[0:n, 0:256], [pv], [VT])
            wq_s = WSR.get(slabK("w_in", l, hp * 256, 256, 16))
            for hh in range(2):
                pq = proj16(wq_s, hh, n)
                r = rotary(pq, n, 128.0 ** -0.5, None, "sq")
                cp(QS.ap[:, (2 * hp + hh) * n:(2 * hp + hh + 1) * n], r.ap[:, 0:n], [r], [QS])
            wk_s = WSR.get(slabK("w_in", l, 1024 + hp * 256, 256, 16))
            for hh in range(2):
                pk = proj16(wk_s, hh, n)
                r = rotary(pk, n, 1.0, None, "sk")
                cp(KS.ap[:, (2 * hp + hh) * n:(2 * hp + hh + 1) * n], r.ap[:, 0:n], [r], [KS])
            wr_s = WSR.get(slabK("w_in", l, 3072 + hp * 256, 256, 16))
            for hh in range(2):
                pg = proj16(wr_s, hh, n)
                act(SRG.ap[:, (2 * hp + hh) * n:(2 * hp + hh + 1) * n], pg.ap[:, 0:n], AF.Silu, [pg], [SRG])
            A.release(m2)
            AR.release(mr2)
        po = psum_pin()
        Sb = [A.alloc(1024, "Sb0"), A.alloc(1024, "Sb1")]
        for b in range(n):
            S = Sb[b % 2]
            Sv = S.ap[:, 0:1024].rearrange("p (h e) -> p h e", h=H)
            T.dma("sp", Sv, st_ret[l, b].rearrange("h d e -> d h e"), writes=[S])
            pvb = [psum(), psum()]
            for hf in range(2):
                mm(pvb[hf], pvb[hf].ap[:, 0:512], SEL.ap[0:n, b * 128:(b + 1) * 128], VT.ap[0:n, hf * 512:(hf + 1) * 512],
                   True, True, [SEL, VT])
            for h in range(H):
                act(Sv[:, h, :], Sv[:, h, :], AF.Copy, [S], [S], scale=GH[h])
                stt(Sv[:, h, :], pvb[h // 4].ap[:, (h % 4) * 128:(h % 4 + 1) * 128], KS.ap[:, h * n + b:h * n + b + 1], Sv[:, h, :],
                    ALU.mult, ALU.add, [pvb[h // 4], KS, S], [S])
            for h in range(H):
                mm(po, po.ap[:, h * n + b:h * n + b + 1], Sv[:, h, :], QS.ap[:, h * n + b:h * n + b + 1], True, True, [S, QS])
            T.dma("sp", o_ret_s[l, b].rearrange("h d e -> d h e"), Sv, reads=[S])
        for h in range(H):
            head_norm(po.ap[:, h * n:(h + 1) * n], n, l, h, SRG.ap[:, h * n:(h + 1) * n], SRG, oall[:, h, :], OALL, po)
        psum_unpin(po)
        A.release(m)
        AR.release(mr)

    def merge_and_out(nt, l, OALL, Z, use_s5):
        m = A.mark()
        M = A.alloc(NCH * nt // 2, "M")
        mv = bf(M)[:, 0:NCH * nt].rearrange("p (c n) -> p c n", c=NCH)
        oall = bf(OALL)[:, 0:H * nt].rearrange("p (h n) -> p h n", h=H)
        zv = bf(Z)[:, 0:8 * nt].rearrange("p (h n) -> p h n", h=8) if Z is not None else None
        sg = [A.alloc(nt, f"sg{i}") for i in range(4)]
        for cp_ in range(8):
            wgr = WSR.get(slabK("w_in", l, 5120 + cp_ * 256, 256, 16))
            for q in range(2):
                pg = proj16(wgr, q, nt)
                act(sg[q].ap[:, 0:nt], pg.ap[:, 0:nt], AF.Sigmoid, [pg], [sg[q]])
            wgs = WSR.get(slabK("w_in", l, 7168 + cp_ * 256, 256, 16))
            for q in range(2):
                pg = proj16(wgs, q, nt)
                act(sg[2 + q].ap[:, 0:nt], pg.ap[:, 0:nt], AF.Sigmoid, [pg], [sg[2 + q]])
            wrp = WSR.get(slabK("ret_proj", l, cp_ * 256, 256, 8))
            wrv = bf(wrp)[:, 0:2048].rearrange("p (k n) -> p k n", k=8)
            for q in range(2):
                pb = psum()
                for h in range(H):
                    mm(pb, pb.ap[:, 0:nt], wrv[:, h, q * 128:(q + 1) * 128], oall[:, h, :], h == 0, h == H - 1, [wrp, OALL])
                tt(sg[q].ap[:, 0:nt], sg[q].ap[:, 0:nt], pb.ap[:, 0:nt], ALU.mult, [sg[q], pb], [sg[q]])
            if use_s5:
                wsp = WSR.get(slabK("s5_proj", l, cp_ * 256, 256, 8))
                wsv = bf(wsp)[:, 0:2048].rearrange("p (k n) -> p k n", k=8)
                for q in range(2):
                    pz = psum()
                    for h in range(8):
                        mm(pz, pz.ap[:, 0:nt], wsv[:, h, q * 128:(q + 1) * 128], zv[:, h, :], h == 0, h == 7, [wsp, Z])
                    tt(sg[2 + q].ap[:, 0:nt], sg[2 + q].ap[:, 0:nt], pz.ap[:, 0:nt], ALU.mult, [sg[2 + q], pz], [sg[2 + q]])
                    tt(mv[:, cp_ * 2 + q, :], sg[q].ap[:, 0:nt], sg[2 + q].ap[:, 0:nt], ALU.add, [sg[q], sg[2 + q]], [M])
            else:
                for q in range(2):
                    cp(mv[:, cp_ * 2 + q, :], sg[q].ap[:, 0:nt], [sg[q]], [M])
        for c2 in range(8):
            wo = WSR.get(slabK("w_out", l, c2 * 256, 256, 16))
            wov = bf(wo)[:, 0:4096].rearrange("p (k n) -> p k n", k=16)
            for q in range(2):
                p = psum()
                for k in range(16):
                    mm(p, p.ap[:, 0:nt], wov[:, k, q * 128:(q + 1) * 128], mv[:, k, :], k == 0, k == 15, [wo, M])
                cc = c2 * 2 + q
                tt(xv(nt)[:, cc, :], xv(nt)[:, cc, :], p.ap[:, 0:nt], ALU.add, [X, p], [X])
        A.release(m)


    TWO_PI = 6.28318
    MASKQ = [cv[:, C_MQ + q * 128:C_MQ + (q + 1) * 128] for q in range(4)]

    def s5_branch(ti, nt, l, Z):
        is_s = ti >= NTILE
        m0 = A.mark()
        mr0 = AR.mark()
        Z0 = A.alloc(8 * nt // 2, "Z0")
        z0v = bf(Z0)[:, 0:8 * nt].rearrange("p (c n) -> p c n", c=8)
        zv = bf(Z)[:, 0:8 * nt].rearrange("p (c n) -> p c n", c=8)
        SU = A.alloc(8 * nt // 2, "SU")
        suv = bf(SU)[:, 0:8 * nt].rearrange("p (c n) -> p c n", c=8)
        for cp_ in range(4):
            ws = WSR.get(slabK("w_in", l, 4096 + cp_ * 256, 256, 16))
            for q in range(2):
                p = proj16(ws, q, nt)
                act(suv[:, cp_ * 2 + q, :], p.ap[:, 0:nt], AF.Copy, [p], [SU])
        PL = A.alloc(4 * 32, "s5pl")
        mG = A.mark()
        G = A.alloc(64 * 12, "s5g")
        gt = lambda i: G.ap[0:64, i * 64:(i + 1) * 64]
        LRE, LIM, RHO, TH, SN, CS, XI, T0, T1, SRE, SIM, DTB = range(12)
        T.dma("sp", gt(LRE), lam_re[l], writes=[G])
        T.dma("sp", gt(LIM), lam_im[l], writes=[G])
        T.dma("sp", gt(DTB)[:, 0:1], logstep[l], writes=[G])
        GB = [G]
        act(gt(DTB)[:, 1:2], gt(DTB)[:, 0:1], AF.Exp, GB, GB)
        dtc = gt(DTB)[:, 1:2]
        act(gt(RHO), gt(LRE), AF.Exp, GB, GB, scale=dtc)
        ts(gt(TH), gt(LIM), dtc, 1.0 / (2 * math.pi), ALU.mult, ALU.mult, GB, GB)
        xi = G.ap.bitcast(I32)[0:64, XI * 64:(XI + 1) * 64]
        cp(xi, gt(TH), GB, GB)
        tt(gt(T0), gt(TH), xi, ALU.subtract, GB, GB)
        act(gt(SN), gt(T0), AF.Sin, GB, GB, scale=TWO_PI)
        ts(gt(T1), gt(TH), 0.25, None, ALU.add, ALU.bypass, GB, GB)
        cp(xi, gt(T1), GB, GB)
        tt(gt(T0), gt(T1), xi, ALU.subtract, GB, GB)
        act(gt(CS), gt(T0), AF.Sin, GB, GB, scale=TWO_PI)
        tt(gt(CS), gt(CS), gt(RHO), ALU.mult, GB, GB)
        tt(gt(SN), gt(SN), gt(RHO), ALU.mult, GB, GB)
        ts(gt(CS), gt(CS), -1.0, None, ALU.add, ALU.bypass, GB, GB)
        tt(gt(T0), gt(LRE), gt(LRE), ALU.mult, GB, GB)
        tt(gt(T1), gt(LIM), gt(LIM), ALU.mult, GB, GB)
        tt(gt(T0), gt(T0), gt(T1), ALU.add, GB, GB)
        T.issue("dve", lambda e: e.reciprocal(out=gt(T0), in_=gt(T0)), reads=GB, writes=GB)
        tt(gt(SRE), gt(CS), gt(LRE), ALU.mult, GB, GB)
        tt(gt(T1), gt(SN), gt(LIM), ALU.mult, GB, GB)
        tt(gt(SRE), gt(SRE), gt(T1), ALU.add, GB, GB)
        tt(gt(SRE), gt(SRE), gt(T0), ALU.mult, GB, GB)
        tt(gt(SIM), gt(SN), gt(LRE), ALU.mult, GB, GB)
        tt(gt(T1), gt(CS), gt(LIM), ALU.mult, GB, GB)
        tt(gt(SIM), gt(SIM), gt(T1), ALU.subtract, GB, GB)
        tt(gt(SIM), gt(SIM), gt(T0), ALU.mult, GB, GB)
        Lm = A.alloc(128, "s5L")
        for i, src in enumerate((RHO, TH, SRE, SIM)):
            ts(Lm.ap[0:64, 0:64], gt(src), cv[0:64, C_EVEN:C_EVEN + 1], None, ALU.mult, ALU.bypass, [G, CONST], [Lm])
            ts(Lm.ap[0:64, 64:128], gt(src), cv[0:64, C_ODD:C_ODD + 1], None, ALU.mult, ALU.bypass, [G, CONST], [Lm])
            p = psum()
            mm(p, p.ap[:, 0:32], Lm.ap[0:64, 0:128], cv[0:64, C_SEL:C_SEL + 32], True, True, [Lm, CONST])
            cp(PL.ap[:, i * 32:(i + 1) * 32], p.ap[:, 0:32], [p], [PL])
        A.release(mG)
        pl = lambda i, pair: PL.ap[:, i * 32 + pair:i * 32 + pair + 1]
        if is_s:
            HIN = A.alloc(2 * 512, "hin")
            HOUT = A.alloc(2 * 512, "hout")
            for ri, src in enumerate((st_re, st_im)):
                stg = A.alloc(512, "hstg")
                T.dma("sp", stg.ap[:, 0:512].rearrange("r (k m) -> r k m", k=4),
                      src[l].rearrange("b (pr m) -> (b pr) m", m=128).rearrange("(k r) m -> r k m", r=128), writes=[stg])
                p = psum()
                for k in range(4):
                    transpose(p, p.ap[:, k * 128:(k + 1) * 128], stg.ap[:, k * 128:(k + 1) * 128], ident, [stg, CONST])
                cp(HIN.ap[:, ri * 512:(ri + 1) * 512], p.ap, [p], [HIN])
        tv = cv[:, C_ONE16:C_ONE16 + nt] if is_s else cv[:, C_TV:C_TV + nt]
        wm = A.mark()
        wmr = AR.mark()
        for ch in range(8):
            A.release(wm)
            AR.release(wmr)
            BR = A.alloc(4 * 64, "s5br")
            brv = lambda i: BR.ap[:, i * 64:(i + 1) * 64].rearrange("p (a c) -> p a c", a=4)
            T.dma("sp", brv(0), b_re[l, 8 * ch:8 * ch + 8].rearrange("(a gl) p c -> (gl p) a c", gl=2), writes=[BR])
            T.dma("sp", brv(1), b_im[l, 8 * ch:8 * ch + 8].rearrange("(a gl) p c -> (gl p) a c", gl=2), writes=[BR])
            sre = PL.ap[:, 2 * 32 + 4 * ch:2 * 32 + 4 * ch + 4].unsqueeze(2).broadcast_to([128, 4, 16])
            sim = PL.ap[:, 3 * 32 + 4 * ch:3 * 32 + 4 * ch + 4].unsqueeze(2).broadcast_to([128, 4, 16])
            BB = A.alloc(4 * 64, "s5bb")
            bbv = lambda i: BB.ap[:, i * 64:(i + 1) * 64].rearrange("p (a c) -> p a c", a=4)
            tt(bbv(0), brv(0), sre, ALU.mult, [BR, PL], [BB])
            tt(bbv(2), brv(1), sim, ALU.mult, [BR, PL], [BB])
            tt(bbv(0), bbv(0), bbv(2), ALU.subtract, [BB], [BB])
            tt(bbv(1), brv(1), sre, ALU.mult, [BR, PL], [BB])
            tt(bbv(2), brv(0), sim, ALU.mult, [BR, PL], [BB])
            tt(bbv(1), bbv(1), bbv(2), ALU.add, [BB], [BB])
            BL = A.alloc(8 * 64, "s5bl")
            blv = lambda a, ri: bf(BL)[:, (a * 2 + ri) * 128:(a * 2 + ri + 1) * 128]
            XP = [A.alloc(128, "s5xp0"), A.alloc(128, "s5xp1")]
            for a in range(4):
                for ri in range(2):
                    xp_ = XP[(a * 2 + ri) % 2]
                    tt(xp_.ap[:, 0:128].rearrange("p (g c) -> p g c", g=8),
                       bbv(ri)[:, a:a + 1, :].broadcast_to([128, 8, 16]) if False else
                       BB.ap[:, ri * 64 + a * 16:ri * 64 + (a + 1) * 16].unsqueeze(1).broadcast_to([128, 8, 16]),
                       MASKQ[a].rearrange("p (g c) -> p g c", g=8), ALU.mult, [BB, CONST], [xp_])
                    p = psum()
                    transpose(p, p.ap[:, 0:128], xp_.ap[:, 0:128], ident, [xp_, CONST])
                    act(blv(a, ri), p.ap[:, 0:128], AF.Copy, [p], [BL])
            CC = A.alloc(256, "s5cc")
            for ri, src in enumerate((c_re, c_im)):
                for hf in range(2):
                    T.dma("sp", CC.ap[:, ri * 128 + hf * 64:ri * 128 + (hf + 1) * 64], src[l, ch * 128:(ch + 1) * 128, :], writes=[CC])
            CL = AR.alloc(8 * 128, "s5cl")
            clv = lambda a, ri: fr(CL)[:, (a * 2 + ri) * 128:(a * 2 + ri + 1) * 128]
            for ri in range(2):
                p = psum()
                transpose(p, p.ap[:, 0:128], CC.ap[:, ri * 128:(ri + 1) * 128], ident, [CC, CONST])
                for a in range(4):
                    stt(clv(a, ri), p.ap[:, 0:128], (1.0 if ri == 0 else -1.0), MASKQ[a], ALU.mult, ALU.mult, [p, CONST], [CL])
            py = psum_pin()
            WSETS = [[A.alloc(nt, f"s5w{s_}_{i}") for i in range(6)] for s_ in range(2)]
            PSETS = [[AR.alloc(nt, f"s5p{s_}_{i}") for i in range(4)] for s_ in range(2)]
            for a in range(4):
                pair = 4 * ch + a
                mw = A.mark()
                mrw = AR.mark()
                COS, SIN, XA, XB, TA, TB = WSETS[a % 2]
                XI_ = TB
                w = lambda b_: b_.ap[:, 0:nt]
                xiv = XI_.ap.bitcast(I32)[:, 0:nt]
                ts(w(XA), tv, pl(1, pair), None, ALU.mult, ALU.bypass, [CONST, PL], [XA])
                cp(xiv, w(XA), [XA], [XI_])
                tt(w(XB), w(XA), xiv, ALU.subtract, [XA, XI_], [XB])
                act(w(SIN), w(XB), AF.Sin, [XB], [SIN], scale=TWO_PI)
                ts(w(XA), w(XA), 0.25, None, ALU.add, ALU.bypass, [XA], [XA])
                cp(xiv, w(XA), [XA], [XI_])
                tt(w(XB), w(XA), xiv, ALU.subtract, [XA, XI_], [XB])
                act(w(COS), w(XB), AF.Sin, [XB], [COS], scale=TWO_PI)
                pre = psum()
                pim = psum()
                mm(pre, pre.ap[:, 0:nt], blv(a, 0), suv[:, ch, :], True, True, [BL, SU])
                mm(pim, pim.ap[:, 0:nt], blv(a, 1), suv[:, ch, :], True, True, [BL, SU])
                tt(w(XA), pre.ap[:, 0:nt], w(COS), ALU.mult, [pre, COS], [XA])
                tt(w(TA), pim.ap[:, 0:nt], w(SIN), ALU.mult, [pim, SIN], [TA])
                tt(w(XA), w(XA), w(TA), ALU.add, [XA, TA], [XA])
                tt(w(XB), pim.ap[:, 0:nt], w(COS), ALU.mult, [pim, COS], [XB])
                tt(w(TA), pre.ap[:, 0:nt], w(SIN), ALU.mult, [pre, SIN], [TA])
                tt(w(XB), w(XB), w(TA), ALU.subtract, [XB, TA], [XB])
                rho = pl(0, pair)
                if is_s:
                    hre = HIN.ap[:, pair:512:32]
                    him = HIN.ap[:, 512 + pair:1024:32]
                    stt(w(TA), hre, rho, w(XA), ALU.mult, ALU.add, [HIN, PL, XA], [TA])
                    stt(w(TB), him, rho, w(XB), ALU.mult, ALU.add, [HIN, PL, XB], [TB])
                else:
                    sre0 = S5ST.ap[:, l * 64 + pair:l * 64 + pair + 1]
                    sim0 = S5ST.ap[:, l * 64 + 32 + pair:l * 64 + 32 + pair + 1]
                    T.issue("dve", lambda e: e.tensor_tensor_scan(out=w(TA), data0=rho.broadcast_to([128, nt]), data1=w(XA),
                                                                 initial=sre0, op0=ALU.mult, op1=ALU.add),
                            reads=[PL, XA, S5ST], writes=[TA])
                    T.issue("dve", lambda e: e.tensor_tensor_scan(out=w(TB), data0=rho.broadcast_to([128, nt]), data1=w(XB),
                                                                 initial=sim0, op0=ALU.mult, op1=ALU.add),
                            reads=[PL, XB, S5ST], writes=[TB])
                P_ = PSETS[a % 2]
                pw = lambda i: fr(P_[i])[:, 0:nt]
                tt(pw(0), w(COS), w(TA), ALU.mult, [COS, TA], [P_[0]])
                stt(pw(1), w(SIN), -1.0, w(TB), ALU.mult, ALU.mult, [SIN, TB], [P_[1]])
                tt(pw(2), w(SIN), w(TA), ALU.mult, [SIN, TA], [P_[2]])
                tt(pw(3), w(COS), w(TB), ALU.mult, [COS, TB], [P_[3]])
                for i in range(4):
                    mm(py, py.ap[:, 0:nt], clv(a, 0 if i < 2 else 1), pw(i), (a == 0 and i == 0), (a == 3 and i == 3), [CL, P_[i]])
                if is_s:
                    tt(HOUT.ap[:, pair:512:32], P_[0].ap[:, 0:nt], P_[1].ap[:, 0:nt], ALU.add, [P_[0], P_[1]], [HOUT])
                    tt(HOUT.ap[:, 512 + pair:1024:32], P_[2].ap[:, 0:nt], P_[3].ap[:, 0:nt], ALU.add, [P_[2], P_[3]], [HOUT])
                else:
                    tt(S5ST.ap[:, l * 64 + pair:l * 64 + pair + 1], P_[0].ap[:, nt - 1:nt], P_[1].ap[:, nt - 1:nt], ALU.add,
                       [P_[0], P_[1]], [S5ST])
                    tt(S5ST.ap[:, l * 64 + 32 + pair:l * 64 + 32 + pair + 1], P_[2].ap[:, nt - 1:nt], P_[3].ap[:, nt - 1:nt], ALU.add,
                       [P_[2], P_[3]], [S5ST])
                A.release(mw)
                AR.release(mrw)
            YF = A.alloc(nt, "s5yf")
            YT = A.alloc(nt, "s5yt")
            dcol = VEC8.ap[:, (l * 3 + 2) * 8 + ch:(l * 3 + 2) * 8 + ch + 1]
            stt(YF.ap[:, 0:nt], suv[:, ch, :], dcol, py.ap[:, 0:nt], ALU.mult, ALU.add, [SU, VEC8, py], [YF])
            psum_unpin(py)
            act(YT.ap[:, 0:nt], YF.ap[:, 0:nt], AF.Square, [YF], [YT])
            ts(YT.ap[:, 0:nt], YT.ap[:, 0:nt], 0.044715, 1.0, ALU.mult, ALU.add, [YT], [YT])
            tt(YT.ap[:, 0:nt], YT.ap[:, 0:nt], YF.ap[:, 0:nt], ALU.mult, [YT, YF], [YT])
            act(YT.ap[:, 0:nt], YT.ap[:, 0:nt], AF.Sigmoid, [YT], [YT], scale=2.0 * math.sqrt(2.0 / math.pi))
            tt(z0v[:, ch, :], YT.ap[:, 0:nt], YF.ap[:, 0:nt], ALU.mult, [YT, YF], [Z0])
        if is_s:
            for ri, dstt in enumerate((o_re_s, o_im_s)):
                stg = A.alloc(512, "hostg")
                p = psum()
                for k in range(4):
                    transpose(p, p.ap[:, k * 128:(k + 1) * 128], HOUT.ap[:, ri * 512 + k * 128:ri * 512 + (k + 1) * 128], ident, [HOUT, CONST])
                cp(stg.ap[:, 0:512], p.ap, [p], [stg])
                T.dma("sp", dstt[l].rearrange("(k r) m -> r k m", r=128), stg.ap[:, 0:512].rearrange("r (k m) -> r k m", k=4), reads=[stg])
        elif ti == NTILE - 1:
            for ri, dstt in enumerate((o_re_p, o_im_p)):
                stg = A.alloc(128, "hostg")
                p = psum()
                transpose(p, p.ap[0:32, 0:128], S5ST.ap[:, l * 64 + ri * 32:l * 64 + (ri + 1) * 32], ident, [S5ST, CONST])
                cp(stg.ap[0:32, 0:128], p.ap[0:32, 0:128], [p], [stg])
                T.dma("sp", dstt[l], stg.ap[0:32, 0:128], reads=[stg])
        A.release(wm)
        gb = A.alloc(nt, "glusig")
        for cp_ in range(4):
            wg = WSR.get(slabK("glu_w", l, cp_ * 256, 256, 8))
            wgv = bf(wg)[:, 0:2048].rearrange("p (k n) -> p k n", k=8)
            for q in range(2):
                c = cp_ * 2 + q
                p = psum()
                for k in range(8):
                    mm(p, p.ap[:, 0:nt], wgv[:, k, q * 128:(q + 1) * 128], z0v[:, k, :], k == 0, k == 7, [wg, Z0])
                act(gb.ap[:, 0:nt], p.ap[:, 0:nt], AF.Sigmoid, [p, VEC8], [gb],
                    bias=VEC8.ap[:, (l * 3 + 1) * 8 + c:(l * 3 + 1) * 8 + c + 1])
                tt(zv[:, c, :], z0v[:, c, :], gb.ap[:, 0:nt], ALU.mult, [Z0, gb], [Z])
        A.release(m0)
        AR.release(mr0)

    def mixer(ti, nt, l):
        m = A.mark()
        rmsnorm(nt, l * 3 + 1, "xn")
        OALL = A.alloc(H * nt // 2, "OALL")
        mrope = A.mark()
        ROPE_H[0] = A.alloc(2 * NT, "rope")
        T.dma("sp", ROPE_H[0].ap[:, 0:2 * NT].rearrange("p (a n) -> p a n", a=2), rope[ti].rearrange("a p n -> p a n"),
              writes=[ROPE_H[0]])
        if ti < NTILE:
            retention_prompt(ti, l, OALL)
        else:
            retention_sample(l, OALL)
        A.release(mrope)
        Z = None
        if debug_stage >= 4:
            Z = A.alloc(8 * nt // 2, "Z")
            s5_branch(ti, nt, l, Z)
        merge_and_out(nt, l, OALL, Z, debug_stage >= 4)
        A.release(m)

    def tile_program(ti, nt, src, dst):
        load_x(src, nt)
        for l in range(DEPTH):
            if debug_stage >= 2:
                rmsnorm(nt, l * 3 + 0, "xn")
                ffn(nt, l, "ffn1")
            if debug_stage >= 3:
                mixer(ti, nt, l)
            if debug_stage >= 2:
                rmsnorm(nt, l * 3 + 2, "xn")
                ffn(nt, l, "ffn2")
        rmsnorm(nt, 12, "y")
        store_y(dst, nt)

    def program():
        WSR.reset()
        X.slots = list(X_FULL)
        XN.slots = list(XN_FULL)
        ps_rr[0] = 0
        A.release(top0)
        AR.release(topr0)
        T.dma("sp", CONST.ap, consts, writes=[CONST])
        T.dma("pool", fr(CONSTR), constsr, writes=[CONSTR])
        m = A.mark()
        stg = A.alloc(256, "gstage")
        for r0, nr in ((0, 128), (128, 80)):
            T.dma("sp", stg.ap[0:nr, 0:128], gains.rearrange("r c p -> (r c) p")[r0:r0 + nr, :], writes=[stg])
            p = psum()
            transpose(p, p.ap[:, 0:nr], stg.ap[0:nr, 0:128], ident[0:nr, 0:nr], [stg, CONST])
            cp(GAIN.ap[:, r0:r0 + nr], p.ap[:, 0:nr], [p], [GAIN])
        T.dma("sp", stg.ap[0:96, 128:256], vec8.rearrange("r c p -> (r c) p"), writes=[stg])
        p = psum()
        transpose(p, p.ap[:, 0:96], stg.ap[0:96, 128:256], ident[0:96, 0:96], [stg, CONST])
        cp(VEC8.ap[:, 0:96], p.ap[:, 0:96], [p], [VEC8])
        A.release(m)
        T.issue("dve", lambda e: e.memset(S5ST.ap, 0.0), writes=[S5ST])
        for ti in range(NTILE):
            tile_program(ti, NT, xp[ti * NT:(ti + 1) * NT, :], yp[ti * NT:(ti + 1) * NT, :])
        X.slots = X_FULL[:2]
        XN.slots = XN_FULL[:1]
        _WH.cur = WSR_SAMP
        tile_program(NTILE, NS, xs, ys)
        T.final_wait("sp")

    top0 = A.mark()
    topr0 = AR.mark()
    T.dry = True
    program()
    T.dry = False
    program()
    return nc, es, T


C_ID = 0
C_EPS6 = 128
C_EPS5 = 129
C_MASK = 256
C_QDEC = 384
C_KDI = 384 + 1024
C_MQ = 384 + 2048
C_TV = C_MQ + 512
C_ONE16 = C_TV + 512
C_SEL = C_ONE16 + 16
C_EVEN = C_SEL + 32
C_ODD = C_EVEN + 1
CONST_W = (C_ODD + 1 + 127) // 128 * 128
R_PERM = 0
R_ONES = 128
R_ID = 256
R_ODIV = 384
CONSTR_W = 512


def make_consts():
    c = np.zeros((128, CONST_W), np.float32)
    c[:, C_ID:C_ID + 128] = np.eye(128, dtype=np.float32)
    c[:, C_EPS6] = 1e-6
    c[:, C_EPS5] = 1e-5
    i = np.arange(128)
    c[:, C_MASK:C_MASK + 128] = (i[None, :] >= i[:, None]).astype(np.float32)
    for h in range(H):
        g = 1.0 - 2.0 ** (-5.0 - h)
        c[:, C_QDEC + h * 128:C_QDEC + (h + 1) * 128] = (g ** (i + 1.0))[None, :]
        c[:, C_KDI + h * 128:C_KDI + (h + 1) * 128] = (g ** (-(i + 1.0)))[None, :]
    gl = i // 64
    g8 = i // 16
    for q in range(4):
        c[:, C_MQ + q * 128:C_MQ + (q + 1) * 128] = (g8[None, :] == (2 * q + gl[:, None])).astype(np.float32)
    c[:, C_TV:C_TV + 512] = (np.arange(512) + 1.0)[None, :]
    c[:, C_ONE16:C_ONE16 + 16] = 1.0
    gg = np.arange(64)
    c[0:64, C_SEL:C_SEL + 32] = (gg[:, None] // 2 == np.arange(32)[None, :]).astype(np.float32)
    c[0:64, C_EVEN] = (gg % 2 == 0)
    c[0:64, C_ODD] = (gg % 2 == 1)
    return c


def make_constsr():
    c = np.zeros((128, CONSTR_W), np.float32)
    pm = np.zeros((128, 128), np.float32)
    for m_ in range(128):
        pm[(m_ + 64) % 128, m_] = 1.0
    c[:, R_PERM:R_PERM + 128] = pm
    c[:, R_ONES:R_ONES + 128] = 1.0
    c[:, R_ID:R_ID + 128] = np.eye(128, dtype=np.float32)
    c[:, R_ODIV:R_ODIV + 128] = 1.0 / 128.0
    return c


def make_rope():
    half = 64
    inv = (10000.0 ** (-np.arange(half, dtype=np.float32) / half)).astype(np.float32)
    out = np.zeros((NTILE + 1, 2, 128, NT), np.float32)
    for ti in range(NTILE + 1):
        if ti < NTILE:
            pos = np.arange(ti * NT, (ti + 1) * NT, dtype=np.float32)
        else:
            pos = np.full((NT,), float(PAST), np.float32)
        ang = (pos[None, :] * inv[:, None]).astype(np.float32)
        cs = np.cos(ang).astype(np.float32)
        sn = np.sin(ang).astype(np.float32)
        out[ti, 0, :64] = cs
        out[ti, 0, 64:] = cs
        out[ti, 1, :64] = -sn
        out[ti, 1, 64:] = sn
    return out


_CACHE = {}


def kernel(**inp):
    stage = inp.pop("_debug_stage", 99)
    ncores = inp.pop("_debug_cores", 8)
    if stage not in _CACHE:
        _CACHE[stage] = build(stage)
    nc, es, T = _CACHE[stage]
    f = lambda a: np.ascontiguousarray(np.asarray(a, dtype=np.float32))
    gains = np.zeros((13, 16, 128), np.float32)
    vec8 = np.zeros((12, 8, 128), np.float32)
    for l in range(DEPTH):
        gains[l * 3 + 0] = f(inp["ffn1_norm"][l]).reshape(16, 128)
        gains[l * 3 + 1] = f(inp["mix_norm"][l]).reshape(16, 128)
        gains[l * 3 + 2] = f(inp["ffn2_norm"][l]).reshape(16, 128)
        vec8[l * 3 + 0] = f(inp["ret_gn"][l]).reshape(8, 128)
        vec8[l * 3 + 1] = f(inp["glu_b"][l]).reshape(8, 128)
        vec8[l * 3 + 2] = f(inp["s5_d"][l]).reshape(8, 128)
    gains[12] = f(inp["final_norm"]).reshape(16, 128)
    shared = {k: f(inp[k]) for k in ("ffn1_w1", "ffn1_w3", "ffn1_w2", "w_in", "ret_proj", "glu_w", "s5_proj",
                                     "w_out", "ffn2_w1", "ffn2_w3", "ffn2_w2", "s5_lam_re", "s5_lam_im",
                                     "s5_b_re", "s5_b_im")}
    shared["s5_log_step"] = f(inp["s5_log_step"]).reshape(DEPTH, 64, 1)
    shared["s5_c_re"] = f(inp["s5_c_re"]).reshape(DEPTH, 1024, 64)
    shared["s5_c_im"] = f(inp["s5_c_im"]).reshape(DEPTH, 1024, 64)
    shared["gains"] = gains
    shared["vec8"] = vec8
    shared["consts"] = make_consts()
    shared["constsr"] = make_constsr()
    shared["rope"] = make_rope()
    xpv = f(inp["x_prompt"])
    xsv = f(inp["x_sample"]).reshape(128, D)
    sret = f(inp["state_ret"])
    sre = f(inp["state_s5_re"]).reshape(DEPTH, 128, 4096)
    sim = f(inp["state_s5_im"]).reshape(DEPTH, 128, 4096)
    in_maps = []
    for c in range(ncores):
        d = dict(shared)
        d["xp"] = xpv[c % 4]
        d["xs"] = np.ascontiguousarray(xsv[c * NS:(c + 1) * NS])
        d["st_ret"] = np.ascontiguousarray(sret[:, c * NS:(c + 1) * NS])
        d["st_re"] = np.ascontiguousarray(sre[:, c * NS:(c + 1) * NS])
        d["st_im"] = np.ascontiguousarray(sim[:, c * NS:(c + 1) * NS])
        in_maps.append(d)
    if ncores < 8:
        res = run_bass_kernel_spmd(nc, in_maps, core_ids=list(range(ncores)), trace=True)
        print("DEBUG exec_time_ns", res.exec_time_ns)
        return res.results
    res = run_bass_kernel_spmd(nc, in_maps, core_ids=list(range(ncores)))
    R = res.results
    y_p = np.stack([R[c]["yp"] for c in range(4)]).reshape(4, SEQ, D)
    y_s = np.concatenate([R[c]["ys"] for c in range(8)]).reshape(128, 1, D)
    ret_p = np.stack([R[c]["o_ret_p"] for c in range(4)], axis=1)
    re_p = np.stack([R[c]["o_re_p"].reshape(DEPTH, 64, 64) for c in range(4)], axis=1)
    im_p = np.stack([R[c]["o_im_p"].reshape(DEPTH, 64, 64) for c in range(4)], axis=1)
    ret_s = np.concatenate([R[c]["o_ret_s"] for c in range(8)], axis=1)
    re_s = np.concatenate([R[c]["o_re_s"].reshape(DEPTH, NS, 64, 64) for c in range(8)], axis=1)
    im_s = np.concatenate([R[c]["o_im_s"].reshape(DEPTH, NS, 64, 64) for c in range(8)], axis=1)
    return (y_p, y_s, ret_p, re_p, im_p, ret_s, re_s, im_s)
```

```python
import math
from contextlib import ExitStack
import numpy as np
import concourse.bass as bass
import concourse.mybir as mybir
from concourse.bass_utils import run_bass_kernel_spmd

F32 = mybir.dt.float32
F32R = mybir.dt.float32r
BF16 = mybir.dt.bfloat16
I32 = mybir.dt.int32
AF = mybir.ActivationFunctionType
ALU = mybir.AluOpType

D = 2048
DFF = 5632
DEPTH = 4
NCH = D // 128
NJ = DFF // 128
H = 8
NT = 512
NS = 16
SEQ = 2048
NTILE = SEQ // NT
PAST = 16384
INW = 9216
SEM_LIMIT = 30000
NWS = 5
NDMASEM = 6
SLOTW = 128
ARENA_W = 43 * 1024
ARENA_R_W = 7 * 1024


class SemObj:
    __slots__ = ("sem", "count")

    def __init__(self, sem):
        self.sem = sem
        self.count = 0


class Eng:
    def __init__(self, name, obj, T):
        self.name = name
        self.obj = obj
        self.T = T
        self.cur = None
        self.seen = {}

    def mark(self):
        if self.cur is None or self.cur.count >= SEM_LIMIT:
            self.cur = self.T.new_sem(self.name)
        self.cur.count += 1
        return (self.cur, self.cur.count)


class Buf:
    __slots__ = ("ap", "slots", "name")

    def __init__(self, ap, slots, name=""):
        self.ap = ap
        self.slots = slots
        self.name = name


class Tracker:
    def __init__(self, nc, es):
        self.nc = nc
        self.es = es
        self.dry = False
        self.nsem = 0
        self.engs = {}
        for name, obj in (("pe", nc.tensor), ("act", nc.scalar), ("dve", nc.vector),
                          ("pool", nc.gpsimd), ("sp", nc.sync)):
            self.engs[name] = Eng(name, obj, self)
        self.dma_sems = {}
        self.dma_rr = {}
        self.state = {}
        self.n_inst = 0

    def new_sem(self, name):
        self.nsem += 1
        return SemObj(self.es.enter_context(self.nc.semaphore(f"s_{name}_{self.nsem}")))

    def _deps(self, reads, writes):
        deps = {}
        st = self.state
        for b in reads:
            for s in b.slots:
                e = st.get(s)
                if e is not None and e[0] is not None:
                    so, v = e[0]
                    if deps.get(so, 0) < v:
                        deps[so] = v
        for b in writes:
            for s in b.slots:
                e = st.get(s)
                if e is not None:
                    if e[0] is not None:
                        so, v = e[0]
                        if deps.get(so, 0) < v:
                            deps[so] = v
                    for so, v in e[1].items():
                        if deps.get(so, 0) < v:
                            deps[so] = v
        return deps

    def _update(self, mark, reads, writes):
        st = self.state
        so, v = mark
        for b in reads:
            for s in b.slots:
                e = st.get(s)
                if e is None:
                    st[s] = [None, {so: v}]
                else:
                    e[1][so] = v
        for b in writes:
            for s in b.slots:
                st[s] = [mark, {}]

    def _wait(self, E, deps, skip_self=None):
        for so, v in deps.items():
            if skip_self is not None and so is skip_self:
                continue
            if E.seen.get(so, 0) < v:
                E.obj.wait_ge(so.sem, v)
                E.seen[so] = v

    def issue(self, eng, fn, reads=(), writes=()):
        if self.dry:
            return
        E = self.engs[eng]
        deps = self._deps(reads, writes)
        self._wait(E, deps, skip_self=(E.cur if eng == "pe" else None))
        inst = fn(E.obj)
        mark = E.mark()
        inst.then_inc(mark[0].sem, 1)
        self._update(mark, reads, writes)
        self.n_inst += 1

    def dma(self, queue, out, in_, reads=(), writes=()):
        if self.dry:
            return
        E = self.engs[queue]
        lst = self.dma_sems.setdefault(queue, [])
        rr = self.dma_rr.get(queue, 0)
        self.dma_rr[queue] = rr + 1
        if len(lst) < NDMASEM:
            lst.append(self.new_sem("dma" + queue))
        so = lst[rr % NDMASEM]
        deps = self._deps(reads, writes)
        if so.count > 0:
            deps[so] = max(deps.get(so, 0), so.count)
        self._wait(E, deps)
        inst = E.obj.dma_start(out=out, in_=in_)
        so.count += 16
        inst.then_inc(so.sem, 16)
        self._update((so, so.count), reads, writes)
        self.n_inst += 1

    def final_wait(self, eng="sp"):
        if self.dry:
            return
        E = self.engs[eng]
        for q, lst in self.dma_sems.items():
            for so in lst:
                if so.count > 0 and E.seen.get(so, 0) < so.count:
                    E.obj.wait_ge(so.sem, so.count)
                    E.seen[so] = so.count
        for name, e2 in self.engs.items():
            if e2.cur is not None and e2.cur.count > 0 and name != eng:
                E.obj.wait_ge(e2.cur.sem, e2.cur.count)


class Arena:
    def __init__(self, nc, es, T, name="arena", width=None, space="sb"):
        self.W = width
        self.space = space
        self.t = es.enter_context(nc.sbuf_tensor(name, [128, width], F32))
        self.top = 0
        self.T = T

    def alloc(self, words, name=""):
        words = (words + SLOTW - 1) // SLOTW * SLOTW
        o = self.top
        self.top += words
        assert self.top <= self.W, f"arena overflow at {name}: {self.top}"
        return self.view(o, words, name)

    def view(self, o, words, name=""):
        ap = self.t[:, o:o + words]
        slots = [(self.space, i) for i in range(o // SLOTW, (o + words) // SLOTW)]
        return Buf(ap, slots, name)

    def mark(self):
        return self.top

    def release(self, m):
        self.top = m


def sub(buf, o, words):
    s0 = buf.slots[0][1] + o // SLOTW
    s1 = buf.slots[0][1] + (o + words + SLOTW - 1) // SLOTW
    return Buf(buf.ap[:, o:o + words], [(buf.slots[0][0], i) for i in range(s0, s1)], buf.name)


def bf(buf):
    return buf.ap.bitcast(BF16)


def fr(buf):
    return buf.ap.bitcast(F32R)


def build(debug_stage=99):
    nc = bass.Bass("TRN2", target_bir_lowering=False)
    es = ExitStack()
    T = Tracker(nc, es)
    A = Arena(nc, es, T, "arena", ARENA_W, "sb")
    AR = Arena(nc, es, T, "arena_r", ARENA_R_W, "sr")

    def din(name, shape, dt=F32):
        return nc.dram_tensor(name, list(shape), dt, kind="ExternalInput").ap()

    def dout(name, shape):
        return nc.dram_tensor(name, list(shape), F32, kind="ExternalOutput").ap()

    xp = din("xp", [SEQ, D])
    xs = din("xs", [NS, D])
    st_ret = din("st_ret", [DEPTH, NS, H, 128, 128])
    st_re = din("st_re", [DEPTH, NS, 4096])
    st_im = din("st_im", [DEPTH, NS, 4096])
    Wd = {}
    for nm, shp in (("ffn1_w1", [DEPTH, D, DFF]), ("ffn1_w3", [DEPTH, D, DFF]), ("ffn1_w2", [DEPTH, DFF, D]),
                    ("w_in", [DEPTH, D, INW]), ("ret_proj", [DEPTH, 1024, D]), ("glu_w", [DEPTH, 1024, 1024]),
                    ("s5_proj", [DEPTH, 1024, D]), ("w_out", [DEPTH, D, D]),
                    ("ffn2_w1", [DEPTH, D, DFF]), ("ffn2_w3", [DEPTH, D, DFF]), ("ffn2_w2", [DEPTH, DFF, D])):
        Wd[nm] = din(nm, shp)
    gains = din("gains", [13, 16, 128])
    vec8 = din("vec8", [12, 8, 128])
    lam_re = din("s5_lam_re", [DEPTH, 64, 64])
    lam_im = din("s5_lam_im", [DEPTH, 64, 64])
    logstep = din("s5_log_step", [DEPTH, 64, 1])
    b_re = din("s5_b_re", [DEPTH, 64, 64, 16])
    b_im = din("s5_b_im", [DEPTH, 64, 64, 16])
    c_re = din("s5_c_re", [DEPTH, 1024, 64])
    c_im = din("s5_c_im", [DEPTH, 1024, 64])
    consts = din("consts", [128, CONST_W])
    constsr = din("constsr", [128, CONSTR_W])
    rope = din("rope", [NTILE + 1, 2, 128, NT])

    yp = dout("yp", [SEQ, D])
    ys = dout("ys", [NS, D])
    o_ret_p = dout("o_ret_p", [DEPTH, H, 128, 128])
    o_re_p = dout("o_re_p", [DEPTH, 32, 128])
    o_im_p = dout("o_im_p", [DEPTH, 32, 128])
    o_ret_s = dout("o_ret_s", [DEPTH, NS, H, 128, 128])
    o_re_s = dout("o_re_s", [DEPTH, NS * 32, 128])
    o_im_s = dout("o_im_s", [DEPTH, NS * 32, 128])
    scr_ret = nc.dram_tensor("scr_ret", [DEPTH, H, 128, 128], F32, kind="Internal").ap()
    scr_buf = [[Buf(None, [("dram", l, h)]) for h in range(H)] for l in range(DEPTH)]

    PS = []
    for i in range(8):
        t = es.enter_context(nc.psum_tensor(f"ps{i}", [128, 512], F32))
        PS.append(Buf(t[:], [("ps", i)], f"ps{i}"))
    ps_rr = [0]

    ps_free = list(range(8))

    def psum():
        b = PS[ps_free[ps_rr[0] % len(ps_free)]]
        ps_rr[0] += 1
        return b

    def psum_pin():
        b = psum()
        ps_free.remove(b.slots[0][1])
        return b

    def psum_unpin(b):
        ps_free.append(b.slots[0][1])
        ps_free.sort()

    CONST = A.alloc(CONST_W, "const")
    CONSTR = AR.alloc(CONSTR_W, "constr")
    X = A.alloc(NCH * NT, "X")
    XN = A.alloc(NCH * NT // 2, "XN")
    WS = [A.alloc(2048, f"ws{i}") for i in range(NWS)]
    GAIN = A.alloc(13 * 16, "gain")
    VEC8 = A.alloc(12 * 8, "vec8")
    ROPE_H = [None]
    S5ST = A.alloc(DEPTH * 64, "s5st")

    cv = CONST.ap
    ident = cv[:, C_ID:C_ID + 128]
    cr = fr(CONSTR)
    perm_r = cr[:, R_PERM:R_PERM + 128]
    ones_r = cr[:, R_ONES:R_ONES + 128]

    def cbuf():
        return CONST

    class WStream:
        def __init__(self, slots):
            self.slots = slots
            self.N = len(slots)
            self.sched = []
            self.pos = 0
            self.issued = 0

        def reset(self):
            self.pos = 0
            self.issued = 0

        def get(self, mk):
            N = self.N
            if T.dry:
                self.sched.append(mk)
                return self.slots[(len(self.sched) - 1) % N]
            idx = self.pos
            self.pos += 1
            while self.issued < min(len(self.sched), idx + N - 1):
                slot = self.slots[self.issued % N]
                for (o_ap, i_ap) in self.sched[self.issued](slot):
                    T.dma("pool", o_ap, i_ap, writes=[slot])
                self.issued += 1
            return self.slots[idx % N]

    X_FULL = list(X.slots)
    XN_FULL = list(XN.slots)
    WS_EXTRA = [sub(X, 2048, 2048), sub(X, 4096, 2048), sub(X, 6144, 2048), sub(XN, 2048, 2048)]
    WSR_MAIN = WStream(WS)
    WSR_SAMP = WStream(WS + WS_EXTRA)

    class _WH:
        cur = WSR_MAIN

        @staticmethod
        def get(mk):
            return _WH.cur.get(mk)

        @staticmethod
        def reset():
            WSR_MAIN.reset()
            WSR_SAMP.reset()
            _WH.cur = WSR_MAIN

    WSR = _WH

    def slabK(name, l, c0, ncol, kch):
        def mk(slot):
            o = bf(slot)[:, 0:kch * ncol].rearrange("p (k n) -> p k n", k=kch)
            i = Wd[name][l, :, c0:c0 + ncol].rearrange("(k p) n -> p k n", p=128)
            return [(o, i)]
        return mk

    def slabR(name, l, r0, nr):
        def mk(slot):
            o = bf(slot)[:, 0:nr * 2048].rearrange("p (j n) -> p j n", j=nr)
            i = Wd[name][l, r0 * 128:(r0 + nr) * 128, :].rearrange("(j p) n -> p j n", p=128)
            return [(o, i)]
        return mk

    def mm(out_buf, out_ap, lhsT, rhs, start, stop, reads):
        T.issue("pe", lambda e: e.matmul(out_ap, lhsT=lhsT, rhs=rhs, start=start, stop=stop),
                reads=reads, writes=[out_buf])

    def transpose(out_buf, out_ap, in_ap, idn, reads):
        T.issue("pe", lambda e: e.transpose(out_ap, in_ap, idn), reads=reads, writes=[out_buf])

    def act(out_ap, in_ap, func, reads, writes, scale=1.0, bias=0.0):
        T.issue("act", lambda e: e.activation(out=out_ap, in_=in_ap, func=func, bias=bias, scale=scale),
                reads=reads, writes=writes)

    def tt(out_ap, a, b, op, reads, writes, eng="dve"):
        T.issue(eng, lambda e: e.tensor_tensor(out=out_ap, in0=a, in1=b, op=op), reads=reads, writes=writes)

    def ts(out_ap, a, s1, s2, op0, op1, reads, writes, eng="dve"):
        T.issue(eng, lambda e: e.tensor_scalar(out=out_ap, in0=a, scalar1=s1, scalar2=s2, op0=op0, op1=op1),
                reads=reads, writes=writes)

    def stt(out_ap, a, s, b, op0, op1, reads, writes):
        T.issue("dve", lambda e: e.scalar_tensor_tensor(out=out_ap, in0=a, scalar=s, in1=b, op0=op0, op1=op1),
                reads=reads, writes=writes)

    def cp(out_ap, in_ap, reads, writes, eng="dve"):
        T.issue(eng, lambda e: e.tensor_copy(out=out_ap, in_=in_ap), reads=reads, writes=writes)

    def xv(nt):
        return X.ap[:, 0:NCH * nt].rearrange("p (c n) -> p c n", c=NCH)

    def xnv(nt):
        return bf(XN)[:, 0:NCH * nt].rearrange("p (c n) -> p c n", c=NCH)

    def load_x(src, nt):
        m = A.mark()
        nb = (nt + 127) // 128
        rows = min(nt, 128)
        for b in range(nb):
            stg = A.alloc(D, "xstage")
            T.dma("sp", stg.ap[0:rows, :], src[b * 128:b * 128 + rows, :], writes=[stg])
            for c4 in range(4):
                p = psum()
                for q in range(4):
                    c = c4 * 4 + q
                    transpose(p, p.ap[:, q * 128:q * 128 + rows], stg.ap[0:rows, c * 128:(c + 1) * 128],
                              ident[0:rows, 0:rows], [stg, CONST])
                o = xv(nt)[:, c4 * 4:(c4 + 1) * 4, b * 128:b * 128 + rows]
                i = p.ap.rearrange("p (q n) -> p q n", q=4)[:, :, 0:rows]
                (cp if (c4 % 2 == 0) else (lambda o_, i_, r_, w_: act(o_, i_, AF.Copy, r_, w_)))(o, i, [p], [X])
            if b % 2 == 1:
                A.release(m)
        A.release(m)

    def rmsnorm(nt, grow, out_kind):
        m = A.mark()
        mr = AR.mark()
        sq = [AR.alloc(nt, "sq0"), AR.alloc(nt, "sq1")]
        rstd = A.alloc(nt, "rstd")
        p = psum()
        for c in range(NCH):
            s = sq[c % 2]
            act(fr(s)[:, 0:nt], xv(nt)[:, c, :], AF.Square, [X], [s])
            mm(p, p.ap[:, 0:nt], ones_r, fr(s)[:, 0:nt], c == 0, c == NCH - 1, [s, CONSTR])
        act(rstd.ap[:, 0:nt], p.ap[:, 0:nt], AF.Sqrt, [p, CONST], [rstd], scale=1.0 / D,
            bias=cv[:, C_EPS6:C_EPS6 + 1])
        T.issue("dve", lambda e: e.reciprocal(out=rstd.ap[:, 0:nt], in_=rstd.ap[:, 0:nt]), reads=[rstd], writes=[rstd])
        g = GAIN.ap[:, grow * 16:(grow + 1) * 16]
        for c in range(NCH):
            if out_kind == "xn":
                stt(xnv(nt)[:, c, :], xv(nt)[:, c, :], g[:, c:c + 1], rstd.ap[:, 0:nt], ALU.mult, ALU.mult,
                    [X, GAIN, rstd], [XN])
            else:
                stt(xv(nt)[:, c, :], xv(nt)[:, c, :], g[:, c:c + 1], rstd.ap[:, 0:nt], ALU.mult, ALU.mult,
                    [X, GAIN, rstd], [X])
        A.release(m)
        AR.release(mr)

    def ffn(nt, l, pref):
        m = A.mark()
        GJ = 8
        Hb = A.alloc(GJ * nt // 2, "H")
        hv = bf(Hb)[:, 0:GJ * nt].rearrange("p (j n) -> p j n", j=GJ)
        sl = [A.alloc(nt, "silu0"), A.alloc(nt, "silu1")]
        xn = xnv(nt)
        j = 0
        cnt = 0
        while j < NJ:
            gj = min(GJ, NJ - j)
            for jj in range(0, gj, 2):
                w1 = WSR.get(slabK(pref + "_w1", l, (j + jj) * 128, 256, 16))
                w3 = WSR.get(slabK(pref + "_w3", l, (j + jj) * 128, 256, 16))
                w1v = bf(w1)[:, 0:4096].rearrange("p (k n) -> p k n", k=16)
                w3v = bf(w3)[:, 0:4096].rearrange("p (k n) -> p k n", k=16)
                for q in range(2):
                    p1 = psum()
                    p3 = psum()
                    for k in range(16):
                        mm(p1, p1.ap[:, 0:nt], w1v[:, k, q * 128:(q + 1) * 128], xn[:, k, :], k == 0, k == 15, [w1, XN])
                    for k in range(16):
                        mm(p3, p3.ap[:, 0:nt], w3v[:, k, q * 128:(q + 1) * 128], xn[:, k, :], k == 0, k == 15, [w3, XN])
                    s = sl[cnt % 2]
                    cnt += 1
                    act(s.ap[:, 0:nt], p1.ap[:, 0:nt], AF.Silu, [p1], [s])
                    tt(hv[:, jj + q, :], s.ap[:, 0:nt], p3.ap[:, 0:nt], ALU.mult, [s, p3], [Hb])
            for c2 in range(8):
                def mk(slot, c2=c2, j=j, gj=gj):
                    o = bf(slot)[:, 0:gj * 256].rearrange("p (r n) -> p r n", r=gj)
                    i = Wd[pref + "_w2"][l, j * 128:(j + gj) * 128, c2 * 256:(c2 + 1) * 256] \
                        .rearrange("(r p) n -> p r n", p=128)
                    return [(o, i)]
                w2 = WSR.get(mk)
                wv = bf(w2)[:, 0:gj * 256].rearrange("p (r n) -> p r n", r=gj)
                for q in range(2):
                    p = psum()
                    for ji in range(gj):
                        mm(p, p.ap[:, 0:nt], wv[:, ji, q * 128:(q + 1) * 128], hv[:, ji, :], ji == 0, ji == gj - 1,
                           [w2, Hb])
                    cc = c2 * 2 + q
                    stt(xv(nt)[:, cc, :], p.ap[:, 0:nt], 0.5, xv(nt)[:, cc, :], ALU.mult, ALU.add, [p, X], [X])
            j += gj
        A.release(m)

    def store_y(dst, nt):
        m = A.mark()
        nb = (nt + 127) // 128
        rows = min(nt, 128)
        for b in range(nb):
            stg = A.alloc(D, "ystage")
            for c4 in range(4):
                p = psum()
                for q in range(4):
                    c = c4 * 4 + q
                    transpose(p, p.ap[0:rows, q * 128:(q + 1) * 128], xv(nt)[:, c, b * 128:b * 128 + rows], ident, [X, CONST])
                o = stg.ap[0:rows, c4 * 512:(c4 + 1) * 512]
                if c4 % 2 == 0:
                    cp(o, p.ap[0:rows, :], [p], [stg])
                else:
                    act(o, p.ap[0:rows, :], AF.Copy, [p], [stg])
            T.dma("sp", dst[b * 128:b * 128 + rows, :], stg.ap[0:rows, :], reads=[stg])
            if b % 2 == 1:
                A.release(m)
        A.release(m)


    GH = [1.0 - 2.0 ** (-5.0 - h) for h in range(H)]
    GC = [g ** 128 for g in GH]
    maskT = cv[:, C_MASK:C_MASK + 128]
    ident_r = cr[:, R_ID:R_ID + 128]
    onesdiv_r = cr[:, R_ODIV:R_ODIV + 128]

    def head_norm(po_ap, n, l, head, srg_ap, srg_buf, out_ap, out_buf, pbuf):
        m = A.mark()
        mr = AR.mark()
        osb = AR.alloc(n, "osb")
        o2 = AR.alloc(n, "o2")
        mean = A.alloc(n, "mean")
        var = A.alloc(n, "var")
        t1 = A.alloc(n, "hn_t1")
        act(fr(osb)[:, 0:n], po_ap, AF.Copy, [pbuf], [osb])
        act(fr(o2)[:, 0:n], po_ap, AF.Square, [pbuf], [o2])
        pm = psum()
        mm(pm, pm.ap[:, 0:n], onesdiv_r, fr(osb)[:, 0:n], True, True, [CONSTR, osb])
        pv = psum()
        mm(pv, pv.ap[:, 0:n], onesdiv_r, fr(o2)[:, 0:n], True, True, [CONSTR, o2])
        act(mean.ap[:, 0:n], pm.ap[:, 0:n], AF.Copy, [pm], [mean])
        tt(var.ap[:, 0:n], mean.ap[:, 0:n], mean.ap[:, 0:n], ALU.mult, [mean], [var])
        tt(var.ap[:, 0:n], pv.ap[:, 0:n], var.ap[:, 0:n], ALU.subtract, [pv, var], [var])
        act(var.ap[:, 0:n], var.ap[:, 0:n], AF.Sqrt, [var, CONST], [var], bias=cv[:, C_EPS5:C_EPS5 + 1])
        T.issue("dve", lambda e: e.reciprocal(out=var.ap[:, 0:n], in_=var.ap[:, 0:n]), reads=[var], writes=[var])
        tt(t1.ap[:, 0:n], osb.ap[:, 0:n], mean.ap[:, 0:n], ALU.subtract, [osb, mean], [t1])
        tt(t1.ap[:, 0:n], t1.ap[:, 0:n], var.ap[:, 0:n], ALU.mult, [t1, var], [t1])
        gn = VEC8.ap[:, (l * 3 + 0) * 8 + head:(l * 3 + 0) * 8 + head + 1]
        stt(out_ap, t1.ap[:, 0:n], gn, srg_ap, ALU.mult, ALU.mult, [t1, VEC8, srg_buf], [out_buf])
        A.release(m)
        AR.release(mr)

    def proj16(slab, q, n, ncol=256):
        wv = bf(slab)[:, 0:16 * ncol].rearrange("p (k n) -> p k n", k=16)
        p = psum()
        for k in range(16):
            mm(p, p.ap[:, 0:n], wv[:, k, q * 128:(q + 1) * 128], xnv(n)[:, k, :], k == 0, k == 15, [slab, XN])
        return p

    def rotary(p, n, scale, tab_mul, name, res=None):
        if res is None:
            res = AR.alloc(n, name + "rot")
        m_ = A.mark()
        mr_ = AR.mark()
        raw = AR.alloc(n, name + "raw")
        t1 = A.alloc(n, name + "t1")
        act(fr(raw)[:, 0:n], p.ap[:, 0:n], AF.Copy, [p], [raw], scale=scale)
        pr = psum()
        mm(pr, pr.ap[:, 0:n], perm_r, fr(raw)[:, 0:n], True, True, [CONSTR, raw])
        tt(t1.ap[:, 0:n], raw.ap[:, 0:n], ROPE_H[0].ap[:, 0:n], ALU.mult, [raw, ROPE_H[0]], [t1])
        if tab_mul is None:
            tt(fr(res)[:, 0:n], pr.ap[:, 0:n], ROPE_H[0].ap[:, NT:NT + n], ALU.mult, [pr, ROPE_H[0]], [res])
            tt(fr(res)[:, 0:n], res.ap[:, 0:n], t1.ap[:, 0:n], ALU.add, [res, t1], [res])
        else:
            t2 = A.alloc(n, name + "t2")
            tt(t2.ap[:, 0:n], pr.ap[:, 0:n], ROPE_H[0].ap[:, NT:NT + n], ALU.mult, [pr, ROPE_H[0]], [t2])
            tt(t1.ap[:, 0:n], t1.ap[:, 0:n], t2.ap[:, 0:n], ALU.add, [t1, t2], [t1])
            tt(fr(res)[:, 0:n].rearrange("p (c i) -> p c i", i=128),
               t1.ap[:, 0:n].rearrange("p (c i) -> p c i", i=128),
               tab_mul.unsqueeze(1).broadcast_to([128, n // 128, 128]), ALU.mult, [t1, CONST], [res])
        A.release(m_)
        AR.release(mr_)
        return res

    def retention_prompt(ti, l, OALL):
        oall = bf(OALL)[:, 0:H * NT].rearrange("p (h n) -> p h n", h=H)
        n = NT
        ncx = n // 128
        for hp in range(4):
            m = A.mark()
            mr = AR.mark()
            wv_s = WSR.get(slabK("w_in", l, 2048 + hp * 256, 256, 16))
            wvv = bf(wv_s)[:, 0:4096].rearrange("p (k n) -> p k n", k=16)
            vtok = AR.alloc(ncx * 256, "vtok")
            vtv = fr(vtok)[:, 0:ncx * 256].rearrange("p (c e) -> p c e", c=ncx)
            for c2 in range(ncx // 2):
                pv = psum()
                for cc in range(2):
                    c = c2 * 2 + cc
                    for k in range(16):
                        mm(pv, pv.ap[:, cc * 256:(cc + 1) * 256], xnv(n)[:, k, c * 128:(c + 1) * 128], wvv[:, k, :],
                           k == 0, k == 15, [XN, wv_s])
                act(fr(vtok)[:, c2 * 512:(c2 + 1) * 512], pv.ap, AF.Copy, [pv], [vtok])
            qd = [AR.alloc(n, "qd0"), AR.alloc(n, "qd1")]
            kd = [AR.alloc(n, "kd0"), AR.alloc(n, "kd1")]
            wq_s = WSR.get(slabK("w_in", l, hp * 256, 256, 16))
            for hh in range(2):
                pq = proj16(wq_s, hh, n)
                rotary(pq, n, 128.0 ** -0.5, cv[:, C_QDEC + (2 * hp + hh) * 128:C_QDEC + (2 * hp + hh + 1) * 128], f"q{hh}", qd[hh])
            wk_s = WSR.get(slabK("w_in", l, 1024 + hp * 256, 256, 16))
            for hh in range(2):
                pk = proj16(wk_s, hh, n)
                rotary(pk, n, 1.0, cv[:, C_KDI + (2 * hp + hh) * 128:C_KDI + (2 * hp + hh + 1) * 128], f"k{hh}", kd[hh])
            wr_s = WSR.get(slabK("w_in", l, 3072 + hp * 256, 256, 16))
            srg = []
            for hh in range(2):
                pg = proj16(wr_s, hh, n)
                sb = A.alloc(n, f"srg{hh}")
                act(sb.ap[:, 0:n], pg.ap[:, 0:n], AF.Silu, [pg], [sb])
                srg.append(sb)
            for hh in range(2):
                head = 2 * hp + hh
                m2 = A.mark()
                mr2 = AR.mark()
                qv = fr(qd[hh])
                kv = fr(kd[hh])
                ps_ = psum()
                for c in range(ncx):
                    mm(ps_, ps_.ap[:, c * 128:(c + 1) * 128], kv[:, c * 128:(c + 1) * 128], qv[:, c * 128:(c + 1) * 128],
                       True, True, [kd[hh], qd[hh]])
                scT = AR.alloc(n, "scT")
                tt(fr(scT)[:, 0:n].rearrange("p (c i) -> p c i", i=128), ps_.ap[:, 0:n].rearrange("p (c i) -> p c i", i=128),
                   maskT.unsqueeze(1).broadcast_to([128, ncx, 128]), ALU.mult, [ps_, CONST], [scT])
                pt = psum()
                for c in range(ncx):
                    transpose(pt, pt.ap.bitcast(F32R)[:, c * 128:(c + 1) * 128], kv[:, c * 128:(c + 1) * 128], ident_r,
                              [kd[hh], CONSTR])
                kdt = AR.alloc(n, "kdt")
                act(fr(kdt)[:, 0:n], pt.ap[:, 0:n], AF.Copy, [pt], [kdt])
                pu = psum()
                for c in range(ncx):
                    mm(pu, pu.ap[:, c * 128:(c + 1) * 128], fr(kdt)[:, c * 128:(c + 1) * 128],
                       vtv[:, c, hh * 128:(hh + 1) * 128], True, True, [kdt, vtok])
                Sall = AR.alloc((ncx + 1) * 128, "Sall")
                Sv = fr(Sall)[:, 0:(ncx + 1) * 128].rearrange("p (c e) -> p c e", e=128)
                Svf = Sall.ap[:, 0:(ncx + 1) * 128].rearrange("p (c e) -> p c e", e=128)
                if ti == 0:
                    ts(Sv[:, 0, :], pu.ap[:, 0:128], 0.0, None, ALU.mult, ALU.bypass, [pu], [Sall])
                else:
                    T.dma("pool", Sv[:, 0, :], scr_ret[l, head], reads=[scr_buf[l][head]], writes=[Sall])
                tmp = A.alloc(128, "stmp")
                for c in range(ncx):
                    tt(tmp.ap[:, 0:128], pu.ap[:, c * 128:(c + 1) * 128], Svf[:, c, :], ALU.add, [pu, Sall], [tmp])
                    act(Sv[:, c + 1, :], tmp.ap[:, 0:128], AF.Copy, [tmp], [Sall], scale=GC[head])
                dst = o_ret_p[l, head] if ti == NTILE - 1 else scr_ret[l, head]
                T.dma("sp", dst, Svf[:, ncx, :], reads=[Sall], writes=([] if ti == NTILE - 1 else [scr_buf[l][head]]))
                po = psum()
                for c in range(ncx):
                    mm(po, po.ap[:, c * 128:(c + 1) * 128], vtv[:, c, hh * 128:(hh + 1) * 128], fr(scT)[:, c * 128:(c + 1) * 128],
                       True, False, [vtok, scT])
                    mm(po, po.ap[:, c * 128:(c + 1) * 128], Sv[:, c, :], qv[:, c * 128:(c + 1) * 128],
                       False, True, [Sall, qd[hh]])
                head_norm(po.ap[:, 0:n], n, l, head, srg[hh].ap[:, 0:n], srg[hh], oall[:, head, :], OALL, po)
                A.release(m2)
                AR.release(mr2)
            A.release(m)
            AR.release(mr)

    def retention_sample(l, OALL):
        n = NS
        oall = bf(OALL)[:, 0:H * n].rearrange("p (h n) -> p h n", h=H)
        m = A.mark()
        mr = AR.mark()
        QS = A.alloc(H * n, "QS")
        KS = A.alloc(H * n, "KS")
        SRG = A.alloc(H * n, "SRG")
        VT = A.alloc(1024, "VT")
        SEL = A.alloc(n * 128, "SEL")
        cp(SEL.ap[0:n, 0:n * 128].rearrange("p (b d) -> p b d", b=n),
           ident[0:n, 0:n].unsqueeze(2).broadcast_to([n, n, 128]), [CONST], [SEL])
        for hp in range(4):
            m2 = A.mark()
            mr2 = AR.mark()
            wv_s = WSR.get(slabK("w_in", l, 2048 + hp * 256, 256, 16))
            wvv = bf(wv_s)[:, 0:4096].rearrange("p (k n) -> p k n", k=16)
            pv = psum()
            for k in range(16):
                mm(pv, pv.ap[0:n, 0:256], xnv(n)[:, k, :], wvv[:, k, :], k == 0, k == 15, [XN, wv_s])
            cp(VT.ap[0:n, hp * 256:(hp + 1) * 256], pv.ap[0:n, 0:256], [pv], [VT])
            wq_s = WSR.get(slabK("w_in", l, hp * 256, 256, 16))
            for hh in range(2):
                pq = proj16(wq_s, hh, n)
                r = rotary(pq, n, 128.0 ** -0.5, None, "sq")
                cp(QS.ap[:, (2 * hp + hh) * n:(2 * hp + hh + 1) * n], r.ap[:, 0:n], [r], [QS])
            wk_s = WSR.get(slabK("w_in", l, 1024 + hp * 256, 256, 16))
            for hh in range(2):
                pk = proj16(wk_s, hh, n)
                r = rotary(pk, n, 1.0, None, "sk")
                cp(KS.ap[:, (2 * hp + hh) * n:(2 * hp + hh + 1) * n], r.ap[:, 0:n], [r], [KS])
            wr_s = WSR.get(slabK("w_in", l, 3072 + hp * 256, 256, 16))
            for hh in range(2):
                pg = proj16(wr_s, hh, n)
                act(SRG.ap[:, (2 * hp + hh) * n:(2 * hp + hh + 1) * n], pg.ap[:, 0:n], AF.Silu, [pg], [SRG])
            A.release(m2)
            AR.release(mr2)
        po = psum_pin()
        Sb = [A.alloc(1024, "Sb0"), A.alloc(1024, "Sb1")]
        for b in range(n):
            S = Sb[b % 2]
            Sv = S.ap[:, 0:1024].rearrange("p (h e) -> p h e", h=H)
            T.dma("sp", Sv, st_ret[l, b].rearrange("h d e -> d h e"), writes=[S])
            pvb = [psum(), psum()]
            for hf in range(2):
                mm(pvb[hf], pvb[hf].ap[:, 0:512], SEL.ap[0:n, b * 128:(b + 1) * 128], VT.ap[0:n, hf * 512:(hf + 1) * 512],
                   True, True, [SEL, VT])
            for h in range(H):
                act(Sv[:, h, :], Sv[:, h, :], AF.Copy, [S], [S], scale=GH[h])
                stt(Sv[:, h, :], pvb[h // 4].ap[:, (h % 4) * 128:(h % 4 + 1) * 128], KS.ap[:, h * n + b:h * n + b + 1], Sv[:, h, :],
                    ALU.mult, ALU.add, [pvb[h // 4], KS, S], [S])
            for h in range(H):
                mm(po, po.ap[:, h * n + b:h * n + b + 1], Sv[:, h, :], QS.ap[:, h * n + b:h * n + b + 1], True, True, [S, QS])
            T.dma("sp", o_ret_s[l, b].rearrange("h d e -> d h e"), Sv, reads=[S])
        for h in range(H):
            head_norm(po.ap[:, h * n:(h + 1) * n], n, l, h, SRG.ap[:, h * n:(h + 1) * n], SRG, oall[:, h, :], OALL, po)
        psum_unpin(po)
        A.release(m)
        AR.release(mr)

    def merge_and_out(nt, l, OALL, Z, use_s5):
        m = A.mark()
        M = A.alloc(NCH * nt // 2, "M")
        mv = bf(M)[:, 0:NCH * nt].rearrange("p (c n) -> p c n", c=NCH)
        oall = bf(OALL)[:, 0:H * nt].rearrange("p (h n) -> p h n", h=H)
        zv = bf(Z)[:, 0:8 * nt].rearrange("p (h n) -> p h n", h=8) if Z is not None else None
        sg = [A.alloc(nt, f"sg{i}") for i in range(4)]
        for cp_ in range(8):
            wgr = WSR.get(slabK("w_in", l, 5120 + cp_ * 256, 256, 16))
            for q in range(2):
                pg = proj16(wgr, q, nt)
                act(sg[q].ap[:, 0:nt], pg.ap[:, 0:nt], AF.Sigmoid, [pg], [sg[q]])
            wgs = WSR.get(slabK("w_in", l, 7168 + cp_ * 256, 256, 16))
            for q in range(2):
                pg = proj16(wgs, q, nt)
                act(sg[2 + q].ap[:, 0:nt], pg.ap[:, 0:nt], AF.Sigmoid, [pg], [sg[2 + q]])
            wrp = WSR.get(slabK("ret_proj", l, cp_ * 256, 256, 8))
            wrv = bf(wrp)[:, 0:2048].rearrange("p (k n) -> p k n", k=8)
            for q in range(2):
                pb = psum()
                for h in range(H):
                    mm(pb, pb.ap[:, 0:nt], wrv[:, h, q * 128:(q + 1) * 128], oall[:, h, :], h == 0, h == H - 1, [wrp, OALL])
                tt(sg[q].ap[:, 0:nt], sg[q].ap[:, 0:nt], pb.ap[:, 0:nt], ALU.mult, [sg[q], pb], [sg[q]])
            if use_s5:
                wsp = WSR.get(slabK("s5_proj", l, cp_ * 256, 256, 8))
                wsv = bf(wsp)[:, 0:2048].rearrange("p (k n) -> p k n", k=8)
                for q in range(2):
                    pz = psum()
                    for h in range(8):
                        mm(pz, pz.ap[:, 0:nt], wsv[:, h, q * 128:(q + 1) * 128], zv[:, h, :], h == 0, h == 7, [wsp, Z])
                    tt(sg[2 + q].ap[:, 0:nt], sg[2 + q].ap[:, 0:nt], pz.ap[:, 0:nt], ALU.mult, [sg[2 + q], pz], [sg[2 + q]])
                    tt(mv[:, cp_ * 2 + q, :], sg[q].ap[:, 0:nt], sg[2 + q].ap[:, 0:nt], ALU.add, [sg[q], sg[2 + q]], [M])
            else:
                for q in range(2):
                    cp(mv[:, cp_ * 2 + q, :], sg[q].ap[:, 0:nt], [sg[q]], [M])
        for c2 in range(8):
            wo = WSR.get(slabK("w_out", l, c2 * 256, 256, 16))
            wov = bf(wo)[:, 0:4096].rearrange("p (k n) -> p k n", k=16)
            for q in range(2):
                p = psum()
                for k in range(16):
                    mm(p, p.ap[:, 0:nt], wov[:, k, q * 128:(q + 1) * 128], mv[:, k, :], k == 0, k == 15, [wo, M])
                cc = c2 * 2 + q
                tt(xv(nt)[:, cc, :], xv(nt)[:, cc, :], p.ap[:, 0:nt], ALU.add, [X, p], [X])
        A.release(m)


    TWO_PI = 6.28318
    PENG = "dve"
    MASKQ = [cv[:, C_MQ + q * 128:C_MQ + (q + 1) * 128] for q in range(4)]

    def s5_branch(ti, nt, l, Z):
        is_s = ti >= NTILE
        m0 = A.mark()
        mr0 = AR.mark()
        Z0 = A.alloc(8 * nt // 2, "Z0")
        z0v = bf(Z0)[:, 0:8 * nt].rearrange("p (c n) -> p c n", c=8)
        zv = bf(Z)[:, 0:8 * nt].rearrange("p (c n) -> p c n", c=8)
        SU = A.alloc(8 * nt // 2, "SU")
        suv = bf(SU)[:, 0:8 * nt].rearrange("p (c n) -> p c n", c=8)
        for cp_ in range(4):
            ws = WSR.get(slabK("w_in", l, 4096 + cp_ * 256, 256, 16))
            for q in range(2):
                p = proj16(ws, q, nt)
                act(suv[:, cp_ * 2 + q, :], p.ap[:, 0:nt], AF.Copy, [p], [SU])
        PL = A.alloc(4 * 32, "s5pl")
        mG = A.mark()
        G = A.alloc(64 * 12, "s5g")
        gt = lambda i: G.ap[0:64, i * 64:(i + 1) * 64]
        LRE, LIM, RHO, TH, SN, CS, XI, T0, T1, SRE, SIM, DTB = range(12)
        T.dma("sp", gt(LRE), lam_re[l], writes=[G])
        T.dma("sp", gt(LIM), lam_im[l], writes=[G])
        T.dma("sp", gt(DTB)[:, 0:1], logstep[l], writes=[G])
        GB = [G]
        act(gt(DTB)[:, 1:2], gt(DTB)[:, 0:1], AF.Exp, GB, GB)
        dtc = gt(DTB)[:, 1:2]
        act(gt(RHO), gt(LRE), AF.Exp, GB, GB, scale=dtc)
        ts(gt(TH), gt(LIM), dtc, 1.0 / (2 * math.pi), ALU.mult, ALU.mult, GB, GB)
        xi = G.ap.bitcast(I32)[0:64, XI * 64:(XI + 1) * 64]
        cp(xi, gt(TH), GB, GB)
        tt(gt(T0), gt(TH), xi, ALU.subtract, GB, GB)
        act(gt(SN), gt(T0), AF.Sin, GB, GB, scale=TWO_PI)
        ts(gt(T1), gt(TH), 0.25, None, ALU.add, ALU.bypass, GB, GB)
        cp(xi, gt(T1), GB, GB)
        tt(gt(T0), gt(T1), xi, ALU.subtract, GB, GB)
        act(gt(CS), gt(T0), AF.Sin, GB, GB, scale=TWO_PI)
        tt(gt(CS), gt(CS), gt(RHO), ALU.mult, GB, GB)
        tt(gt(SN), gt(SN), gt(RHO), ALU.mult, GB, GB)
        ts(gt(CS), gt(CS), -1.0, None, ALU.add, ALU.bypass, GB, GB)
        tt(gt(T0), gt(LRE), gt(LRE), ALU.mult, GB, GB)
        tt(gt(T1), gt(LIM), gt(LIM), ALU.mult, GB, GB)
        tt(gt(T0), gt(T0), gt(T1), ALU.add, GB, GB)
        T.issue("dve", lambda e: e.reciprocal(out=gt(T0), in_=gt(T0)), reads=GB, writes=GB)
        tt(gt(SRE), gt(CS), gt(LRE), ALU.mult, GB, GB)
        tt(gt(T1), gt(SN), gt(LIM), ALU.mult, GB, GB)
        tt(gt(SRE), gt(SRE), gt(T1), ALU.add, GB, GB)
        tt(gt(SRE), gt(SRE), gt(T0), ALU.mult, GB, GB)
        tt(gt(SIM), gt(SN), gt(LRE), ALU.mult, GB, GB)
        tt(gt(T1), gt(CS), gt(LIM), ALU.mult, GB, GB)
        tt(gt(SIM), gt(SIM), gt(T1), ALU.subtract, GB, GB)
        tt(gt(SIM), gt(SIM), gt(T0), ALU.mult, GB, GB)
        Lm = A.alloc(128, "s5L")
        for i, src in enumerate((RHO, TH, SRE, SIM)):
            ts(Lm.ap[0:64, 0:64], gt(src), cv[0:64, C_EVEN:C_EVEN + 1], None, ALU.mult, ALU.bypass, [G, CONST], [Lm])
            ts(Lm.ap[0:64, 64:128], gt(src), cv[0:64, C_ODD:C_ODD + 1], None, ALU.mult, ALU.bypass, [G, CONST], [Lm])
            p = psum()
            mm(p, p.ap[:, 0:32], Lm.ap[0:64, 0:128], cv[0:64, C_SEL:C_SEL + 32], True, True, [Lm, CONST])
            cp(PL.ap[:, i * 32:(i + 1) * 32], p.ap[:, 0:32], [p], [PL])
        A.release(mG)
        pl = lambda i, pair: PL.ap[:, i * 32 + pair:i * 32 + pair + 1]
        if is_s:
            HIN = A.alloc(2 * 512, "hin")
            HOUT = A.alloc(2 * 512, "hout")
            for ri, src in enumerate((st_re, st_im)):
                stg = A.alloc(512, "hstg")
                T.dma("sp", stg.ap[:, 0:512].rearrange("r (k m) -> r k m", k=4),
                      src[l].rearrange("b (pr m) -> (b pr) m", m=128).rearrange("(k r) m -> r k m", r=128), writes=[stg])
                p = psum()
                for k in range(4):
                    transpose(p, p.ap[:, k * 128:(k + 1) * 128], stg.ap[:, k * 128:(k + 1) * 128], ident, [stg, CONST])
                cp(HIN.ap[:, ri * 512:(ri + 1) * 512], p.ap, [p], [HIN])
        tv = cv[:, C_ONE16:C_ONE16 + nt] if is_s else cv[:, C_TV:C_TV + nt]
        wm = A.mark()
        wmr = AR.mark()
        for ch in range(8):
            A.release(wm)
            AR.release(wmr)
            BR = A.alloc(4 * 64, "s5br")
            brv = lambda i: BR.ap[:, i * 64:(i + 1) * 64].rearrange("p (a c) -> p a c", a=4)
            T.dma("sp", brv(0), b_re[l, 8 * ch:8 * ch + 8].rearrange("(a gl) p c -> (gl p) a c", gl=2), writes=[BR])
            T.dma("sp", brv(1), b_im[l, 8 * ch:8 * ch + 8].rearrange("(a gl) p c -> (gl p) a c", gl=2), writes=[BR])
            sre = PL.ap[:, 2 * 32 + 4 * ch:2 * 32 + 4 * ch + 4].unsqueeze(2).broadcast_to([128, 4, 16])
            sim = PL.ap[:, 3 * 32 + 4 * ch:3 * 32 + 4 * ch + 4].unsqueeze(2).broadcast_to([128, 4, 16])
            BB = A.alloc(4 * 64, "s5bb")
            bbv = lambda i: BB.ap[:, i * 64:(i + 1) * 64].rearrange("p (a c) -> p a c", a=4)
            tt(bbv(0), brv(0), sre, ALU.mult, [BR, PL], [BB])
            tt(bbv(2), brv(1), sim, ALU.mult, [BR, PL], [BB])
            tt(bbv(0), bbv(0), bbv(2), ALU.subtract, [BB], [BB])
            tt(bbv(1), brv(1), sre, ALU.mult, [BR, PL], [BB])
            tt(bbv(2), brv(0), sim, ALU.mult, [BR, PL], [BB])
            tt(bbv(1), bbv(1), bbv(2), ALU.add, [BB], [BB])
            BL = A.alloc(8 * 64, "s5bl")
            blv = lambda a, ri: bf(BL)[:, (a * 2 + ri) * 128:(a * 2 + ri + 1) * 128]
            XP = [A.alloc(128, "s5xp0"), A.alloc(128, "s5xp1")]
            for a in range(4):
                for ri in range(2):
                    xp_ = XP[(a * 2 + ri) % 2]
                    tt(xp_.ap[:, 0:128].rearrange("p (g c) -> p g c", g=8),
                       bbv(ri)[:, a:a + 1, :].broadcast_to([128, 8, 16]) if False else
                       BB.ap[:, ri * 64 + a * 16:ri * 64 + (a + 1) * 16].unsqueeze(1).broadcast_to([128, 8, 16]),
                       MASKQ[a].rearrange("p (g c) -> p g c", g=8), ALU.mult, [BB, CONST], [xp_])
                    p = psum()
                    transpose(p, p.ap[:, 0:128], xp_.ap[:, 0:128], ident, [xp_, CONST])
                    act(blv(a, ri), p.ap[:, 0:128], AF.Copy, [p], [BL])
            CC = A.alloc(256, "s5cc")
            for ri, src in enumerate((c_re, c_im)):
                for hf in range(2):
                    T.dma("sp", CC.ap[:, ri * 128 + hf * 64:ri * 128 + (hf + 1) * 64], src[l, ch * 128:(ch + 1) * 128, :], writes=[CC])
            CL = AR.alloc(8 * 128, "s5cl")
            clv = lambda a, ri: fr(CL)[:, (a * 2 + ri) * 128:(a * 2 + ri + 1) * 128]
            for ri in range(2):
                p = psum()
                transpose(p, p.ap[:, 0:128], CC.ap[:, ri * 128:(ri + 1) * 128], ident, [CC, CONST])
                for a in range(4):
                    stt(clv(a, ri), p.ap[:, 0:128], (1.0 if ri == 0 else -1.0), MASKQ[a], ALU.mult, ALU.mult, [p, CONST], [CL])
            py = psum_pin()
            WSETS = [[A.alloc(nt, f"s5w{s_}_{i}") for i in range(6)] for s_ in range(2)]
            PSETS = [[AR.alloc(nt, f"s5p{s_}_{i}") for i in range(4)] for s_ in range(2)]
            def pair_ops(a):
                pair = 4 * ch + a
                COS, SIN, XA, XB, TA, TB = WSETS[a % 2]
                XI_ = TB
                w = lambda b_: b_.ap[:, 0:nt]
                xiv = XI_.ap.bitcast(I32)[:, 0:nt]
                act(w(XA), tv, AF.Identity, [CONST, PL], [XA], scale=pl(1, pair))
                cp(xiv, w(XA), [XA], [XI_])
                yield
                tt(w(XB), w(XA), xiv, ALU.subtract, [XA, XI_], [XB])
                yield
                act(w(SIN), w(XB), AF.Sin, [XB], [SIN], scale=TWO_PI)
                act(w(XA), tv, AF.Identity, [CONST, PL], [XA], scale=pl(1, pair), bias=cv[:, C_QUARTER:C_QUARTER + 1])
                cp(xiv, w(XA), [XA], [XI_])
                yield
                tt(w(XB), w(XA), xiv, ALU.subtract, [XA, XI_], [XB])
                yield
                act(w(COS), w(XB), AF.Sin, [XB], [COS], scale=TWO_PI)
                pre = psum()
                pim = psum()
                mm(pre, pre.ap[:, 0:nt], blv(a, 0), suv[:, ch, :], True, True, [BL, SU])
                mm(pim, pim.ap[:, 0:nt], blv(a, 1), suv[:, ch, :], True, True, [BL, SU])
                tt(w(XA), pre.ap[:, 0:nt], w(COS), ALU.mult, [pre, COS], [XA])
                yield
                tt(w(TA), pim.ap[:, 0:nt], w(SIN), ALU.mult, [pim, SIN], [TA])
                yield
                tt(w(XA), w(XA), w(TA), ALU.add, [XA, TA], [XA])
                yield
                tt(w(XB), pim.ap[:, 0:nt], w(COS), ALU.mult, [pim, COS], [XB])
                yield
                tt(w(TA), pre.ap[:, 0:nt], w(SIN), ALU.mult, [pre, SIN], [TA])
                yield
                tt(w(XB), w(XB), w(TA), ALU.subtract, [XB, TA], [XB])
                yield
                rho = pl(0, pair)
                if is_s:
                    hre = HIN.ap[:, pair:512:32]
                    him = HIN.ap[:, 512 + pair:1024:32]
                    stt(w(TA), hre, rho, w(XA), ALU.mult, ALU.add, [HIN, PL, XA], [TA])
                    yield
                    stt(w(TB), him, rho, w(XB), ALU.mult, ALU.add, [HIN, PL, XB], [TB])
                    yield
                else:
                    sre0 = S5ST.ap[:, l * 64 + pair:l * 64 + pair + 1]
                    sim0 = S5ST.ap[:, l * 64 + 32 + pair:l * 64 + 32 + pair + 1]
                    T.issue("dve", lambda e: e.tensor_tensor_scan(out=w(TA), data0=rho.broadcast_to([128, nt]), data1=w(XA),
                                                                 initial=sre0, op0=ALU.mult, op1=ALU.add),
                            reads=[PL, XA, S5ST], writes=[TA])
                    yield
                    T.issue("dve", lambda e: e.tensor_tensor_scan(out=w(TB), data0=rho.broadcast_to([128, nt]), data1=w(XB),
                                                                 initial=sim0, op0=ALU.mult, op1=ALU.add),
                            reads=[PL, XB, S5ST], writes=[TB])
                    yield
                P_ = PSETS[a % 2]
                pw = lambda i: fr(P_[i])[:, 0:nt]
                tt(pw(0), w(COS), w(TA), ALU.mult, [COS, TA], [P_[0]], eng=PENG)
                yield
                stt(pw(1), w(SIN), -1.0, w(TB), ALU.mult, ALU.mult, [SIN, TB], [P_[1]])
                yield
                tt(pw(2), w(SIN), w(TA), ALU.mult, [SIN, TA], [P_[2]], eng=PENG)
                yield
                tt(pw(3), w(COS), w(TB), ALU.mult, [COS, TB], [P_[3]], eng=PENG)
                yield
                for i in range(4):
                    mm(py, py.ap[:, 0:nt], clv(a, 0 if i < 2 else 1), pw(i), (a == 0 and i == 0), (a == 3 and i == 3), [CL, P_[i]])
                if is_s:
                    tt(HOUT.ap[:, pair:512:32], P_[0].ap[:, 0:nt], P_[1].ap[:, 0:nt], ALU.add, [P_[0], P_[1]], [HOUT])
                    yield
                    tt(HOUT.ap[:, 512 + pair:1024:32], P_[2].ap[:, 0:nt], P_[3].ap[:, 0:nt], ALU.add, [P_[2], P_[3]], [HOUT])
                    yield
                else:
                    tt(S5ST.ap[:, l * 64 + pair:l * 64 + pair + 1], P_[0].ap[:, nt - 1:nt], P_[1].ap[:, nt - 1:nt], ALU.add,
                       [P_[0], P_[1]], [S5ST])
                    yield
                    tt(S5ST.ap[:, l * 64 + 32 + pair:l * 64 + 32 + pair + 1], P_[2].ap[:, nt - 1:nt], P_[3].ap[:, nt - 1:nt], ALU.add,
                       [P_[2], P_[3]], [S5ST])
                    yield

            for a0_ in (0, 2):
                gens = [pair_ops(a0_), pair_ops(a0_ + 1)]
                alive = [True, True]
                while alive[0] or alive[1]:
                    for gi in range(2):
                        if alive[gi]:
                            try:
                                next(gens[gi])
                            except StopIteration:
                                alive[gi] = False
            YF = A.alloc(nt, "s5yf")
            YT = A.alloc(nt, "s5yt")
            dcol = VEC8.ap[:, (l * 3 + 2) * 8 + ch:(l * 3 + 2) * 8 + ch + 1]
            stt(YF.ap[:, 0:nt], suv[:, ch, :], dcol, py.ap[:, 0:nt], ALU.mult, ALU.add, [SU, VEC8, py], [YF])
            psum_unpin(py)
            act(YT.ap[:, 0:nt], YF.ap[:, 0:nt], AF.Square, [YF], [YT])
            ts(YT.ap[:, 0:nt], YT.ap[:, 0:nt], 0.044715, 1.0, ALU.mult, ALU.add, [YT], [YT])
            tt(YT.ap[:, 0:nt], YT.ap[:, 0:nt], YF.ap[:, 0:nt], ALU.mult, [YT, YF], [YT])
            act(YT.ap[:, 0:nt], YT.ap[:, 0:nt], AF.Sigmoid, [YT], [YT], scale=2.0 * math.sqrt(2.0 / math.pi))
            tt(z0v[:, ch, :], YT.ap[:, 0:nt], YF.ap[:, 0:nt], ALU.mult, [YT, YF], [Z0])
        if is_s:
            for ri, dstt in enumerate((o_re_s, o_im_s)):
                stg = A.alloc(512, "hostg")
                p = psum()
                for k in range(4):
                    transpose(p, p.ap[:, k * 128:(k + 1) * 128], HOUT.ap[:, ri * 512 + k * 128:ri * 512 + (k + 1) * 128], ident, [HOUT, CONST])
                cp(stg.ap[:, 0:512], p.ap, [p], [stg])
                T.dma("sp", dstt[l].rearrange("(k r) m -> r k m", r=128), stg.ap[:, 0:512].rearrange("r (k m) -> r k m", k=4), reads=[stg])
        elif ti == NTILE - 1:
            for ri, dstt in enumerate((o_re_p, o_im_p)):
                stg = A.alloc(128, "hostg")
                p = psum()
                transpose(p, p.ap[0:32, 0:128], S5ST.ap[:, l * 64 + ri * 32:l * 64 + (ri + 1) * 32], ident, [S5ST, CONST])
                cp(stg.ap[0:32, 0:128], p.ap[0:32, 0:128], [p], [stg])
                T.dma("sp", dstt[l], stg.ap[0:32, 0:128], reads=[stg])
        A.release(wm)
        gb = A.alloc(nt, "glusig")
        for cp_ in range(4):
            wg = WSR.get(slabK("glu_w", l, cp_ * 256, 256, 8))
            wgv = bf(wg)[:, 0:2048].rearrange("p (k n) -> p k n", k=8)
            for q in range(2):
                c = cp_ * 2 + q
                p = psum()
                for k in range(8):
                    mm(p, p.ap[:, 0:nt], wgv[:, k, q * 128:(q + 1) * 128], z0v[:, k, :], k == 0, k == 7, [wg, Z0])
                act(gb.ap[:, 0:nt], p.ap[:, 0:nt], AF.Sigmoid, [p, VEC8], [gb],
                    bias=VEC8.ap[:, (l * 3 + 1) * 8 + c:(l * 3 + 1) * 8 + c + 1])
                tt(zv[:, c, :], z0v[:, c, :], gb.ap[:, 0:nt], ALU.mult, [Z0, gb], [Z])
        A.release(m0)
        AR.release(mr0)

    def mixer(ti, nt, l):
        m = A.mark()
        rmsnorm(nt, l * 3 + 1, "xn")
        OALL = A.alloc(H * nt // 2, "OALL")
        mrope = A.mark()
        ROPE_H[0] = A.alloc(2 * NT, "rope")
        T.dma("sp", ROPE_H[0].ap[:, 0:2 * NT].rearrange("p (a n) -> p a n", a=2), rope[ti].rearrange("a p n -> p a n"),
              writes=[ROPE_H[0]])
        if ti < NTILE:
            retention_prompt(ti, l, OALL)
        else:
            retention_sample(l, OALL)
        A.release(mrope)
        Z = None
        if debug_stage >= 4:
            Z = A.alloc(8 * nt // 2, "Z")
            s5_branch(ti, nt, l, Z)
        merge_and_out(nt, l, OALL, Z, debug_stage >= 4)
        A.release(m)

    def tile_program(ti, nt, src, dst):
        load_x(src, nt)
        for l in range(DEPTH):
            if debug_stage >= 2:
                rmsnorm(nt, l * 3 + 0, "xn")
                ffn(nt, l, "ffn1")
            if debug_stage >= 3:
                mixer(ti, nt, l)
            if debug_stage >= 2:
                rmsnorm(nt, l * 3 + 2, "xn")
                ffn(nt, l, "ffn2")
        rmsnorm(nt, 12, "y")
        store_y(dst, nt)

    def program():
        WSR.reset()
        X.slots = list(X_FULL)
        XN.slots = list(XN_FULL)
        ps_rr[0] = 0
        A.release(top0)
        AR.release(topr0)
        T.dma("sp", CONST.ap, consts, writes=[CONST])
        T.dma("pool", fr(CONSTR), constsr, writes=[CONSTR])
        m = A.mark()
        stg = A.alloc(256, "gstage")
        for r0, nr in ((0, 128), (128, 80)):
            T.dma("sp", stg.ap[0:nr, 0:128], gains.rearrange("r c p -> (r c) p")[r0:r0 + nr, :], writes=[stg])
            p = psum()
            transpose(p, p.ap[:, 0:nr], stg.ap[0:nr, 0:128], ident[0:nr, 0:nr], [stg, CONST])
            cp(GAIN.ap[:, r0:r0 + nr], p.ap[:, 0:nr], [p], [GAIN])
        T.dma("sp", stg.ap[0:96, 128:256], vec8.rearrange("r c p -> (r c) p"), writes=[stg])
        p = psum()
        transpose(p, p.ap[:, 0:96], stg.ap[0:96, 128:256], ident[0:96, 0:96], [stg, CONST])
        cp(VEC8.ap[:, 0:96], p.ap[:, 0:96], [p], [VEC8])
        A.release(m)
        T.issue("dve", lambda e: e.memset(S5ST.ap, 0.0), writes=[S5ST])
        for ti in range(NTILE):
            tile_program(ti, NT, xp[ti * NT:(ti + 1) * NT, :], yp[ti * NT:(ti + 1) * NT, :])
        X.slots = X_FULL[:2]
        XN.slots = XN_FULL[:1]
        _WH.cur = WSR_SAMP
        tile_program(NTILE, NS, xs, ys)
        T.final_wait("sp")

    top0 = A.mark()
    topr0 = AR.mark()
    T.dry = True
    program()
    T.dry = False
    program()
    return nc, es, T


C_ID = 0
C_EPS6 = 128
C_EPS5 = 129
C_MASK = 256
C_QDEC = 384
C_KDI = 384 + 1024
C_MQ = 384 + 2048
C_TV = C_MQ + 512
C_ONE16 = C_TV + 512
C_SEL = C_ONE16 + 16
C_EVEN = C_SEL + 32
C_ODD = C_EVEN + 1
C_QUARTER = C_ODD + 1
CONST_W = (C_QUARTER + 1 + 127) // 128 * 128
R_PERM = 0
R_ONES = 128
R_ID = 256
R_ODIV = 384
CONSTR_W = 512


def make_consts():
    c = np.zeros((128, CONST_W), np.float32)
    c[:, C_ID:C_ID + 128] = np.eye(128, dtype=np.float32)
    c[:, C_EPS6] = 1e-6
    c[:, C_EPS5] = 1e-5
    i = np.arange(128)
    c[:, C_MASK:C_MASK + 128] = (i[None, :] >= i[:, None]).astype(np.float32)
    for h in range(H):
        g = 1.0 - 2.0 ** (-5.0 - h)
        c[:, C_QDEC + h * 128:C_QDEC + (h + 1) * 128] = (g ** (i + 1.0))[None, :]
        c[:, C_KDI + h * 128:C_KDI + (h + 1) * 128] = (g ** (-(i + 1.0)))[None, :]
    gl = i // 64
    g8 = i // 16
    for q in range(4):
        c[:, C_MQ + q * 128:C_MQ + (q + 1) * 128] = (g8[None, :] == (2 * q + gl[:, None])).astype(np.float32)
    c[:, C_TV:C_TV + 512] = (np.arange(512) + 1.0)[None, :]
    c[:, C_ONE16:C_ONE16 + 16] = 1.0
    gg = np.arange(64)
    c[0:64, C_SEL:C_SEL + 32] = (gg[:, None] // 2 == np.arange(32)[None, :]).astype(np.float32)
    c[0:64, C_EVEN] = (gg % 2 == 0)
    c[0:64, C_ODD] = (gg % 2 == 1)
    c[:, C_QUARTER] = 0.25
    return c


def make_constsr():
    c = np.zeros((128, CONSTR_W), np.float32)
    pm = np.zeros((128, 128), np.float32)
    for m_ in range(128):
        pm[(m_ + 64) % 128, m_] = 1.0
    c[:, R_PERM:R_PERM + 128] = pm
    c[:, R_ONES:R_ONES + 128] = 1.0
    c[:, R_ID:R_ID + 128] = np.eye(128, dtype=np.float32)
    c[:, R_ODIV:R_ODIV + 128] = 1.0 / 128.0
    return c


def make_rope():
    half = 64
    inv = (10000.0 ** (-np.arange(half, dtype=np.float32) / half)).astype(np.float32)
    out = np.zeros((NTILE + 1, 2, 128, NT), np.float32)
    for ti in range(NTILE + 1):
        if ti < NTILE:
            pos = np.arange(ti * NT, (ti + 1) * NT, dtype=np.float32)
        else:
            pos = np.full((NT,), float(PAST), np.float32)
        ang = (pos[None, :] * inv[:, None]).astype(np.float32)
        cs = np.cos(ang).astype(np.float32)
        sn = np.sin(ang).astype(np.float32)
        out[ti, 0, :64] = cs
        out[ti, 0, 64:] = cs
        out[ti, 1, :64] = -sn
        out[ti, 1, 64:] = sn
    return out


_CACHE = {}


def kernel(**inp):
    stage = inp.pop("_debug_stage", 99)
    ncores = inp.pop("_debug_cores", 8)
    if stage not in _CACHE:
        _CACHE[stage] = build(stage)
    nc, es, T = _CACHE[stage]
    f = lambda a: np.ascontiguousarray(np.asarray(a, dtype=np.float32))
    gains = np.zeros((13, 16, 128), np.float32)
    vec8 = np.zeros((12, 8, 128), np.float32)
    for l in range(DEPTH):
        gains[l * 3 + 0] = f(inp["ffn1_norm"][l]).reshape(16, 128)
        gains[l * 3 + 1] = f(inp["mix_norm"][l]).reshape(16, 128)
        gains[l * 3 + 2] = f(inp["ffn2_norm"][l]).reshape(16, 128)
        vec8[l * 3 + 0] = f(inp["ret_gn"][l]).reshape(8, 128)
        vec8[l * 3 + 1] = f(inp["glu_b"][l]).reshape(8, 128)
        vec8[l * 3 + 2] = f(inp["s5_d"][l]).reshape(8, 128)
    gains[12] = f(inp["final_norm"]).reshape(16, 128)
    shared = {k: f(inp[k]) for k in ("ffn1_w1", "ffn1_w3", "ffn1_w2", "w_in", "ret_proj", "glu_w", "s5_proj",
                                     "w_out", "ffn2_w1", "ffn2_w3", "ffn2_w2", "s5_lam_re", "s5_lam_im",
                                     "s5_b_re", "s5_b_im")}
    shared["s5_log_step"] = f(inp["s5_log_step"]).reshape(DEPTH, 64, 1)
    shared["s5_c_re"] = f(inp["s5_c_re"]).reshape(DEPTH, 1024, 64)
    shared["s5_c_im"] = f(inp["s5_c_im"]).reshape(DEPTH, 1024, 64)
    shared["gains"] = gains
    shared["vec8"] = vec8
    shared["consts"] = make_consts()
    shared["constsr"] = make_constsr()
    shared["rope"] = make_rope()
    xpv = f(inp["x_prompt"])
    xsv = f(inp["x_sample"]).reshape(128, D)
    sret = f(inp["state_ret"])
    sre = f(inp["state_s5_re"]).reshape(DEPTH, 128, 4096)
    sim = f(inp["state_s5_im"]).reshape(DEPTH, 128, 4096)
    in_maps = []
    for c in range(ncores):
        d = dict(shared)
        d["xp"] = xpv[c % 4]
        d["xs"] = np.ascontiguousarray(xsv[c * NS:(c + 1) * NS])
        d["st_ret"] = np.ascontiguousarray(sret[:, c * NS:(c + 1) * NS])
        d["st_re"] = np.ascontiguousarray(sre[:, c * NS:(c + 1) * NS])
        d["st_im"] = np.ascontiguousarray(sim[:, c * NS:(c + 1) * NS])
        in_maps.append(d)
    if ncores < 8:
        res = run_bass_kernel_spmd(nc, in_maps, core_ids=list(range(ncores)), trace=True)
        print("DEBUG exec_time_ns", res.exec_time_ns)
        return res.results
    res = run_bass_kernel_spmd(nc, in_maps, core_ids=list(range(ncores)))
    R = res.results
    y_p = np.stack([R[c]["yp"] for c in range(4)]).reshape(4, SEQ, D)
    y_s = np.concatenate([R[c]["ys"] for c in range(8)]).reshape(128, 1, D)
    ret_p = np.stack([R[c]["o_ret_p"] for c in range(4)], axis=1)
    re_p = np.stack([R[c]["o_re_p"].reshape(DEPTH, 64, 64) for c in range(4)], axis=1)
    im_p = np.stack([R[c]["o_im_p"].reshape(DEPTH, 64, 64) for c in range(4)], axis=1)
    ret_s = np.concatenate([R[c]["o_ret_s"] for c in range(8)], axis=1)
    re_s = np.concatenate([R[c]["o_re_s"].reshape(DEPTH, NS, 64, 64) for c in range(8)], axis=1)
    im_s = np.concatenate([R[c]["o_im_s"].reshape(DEPTH, NS, 64, 64) for c in range(8)], axis=1)
    return (y_p, y_s, ret_p, re_p, im_p, ret_s, re_s, im_s)
```

```python
import math
from contextlib import ExitStack
import numpy as np
import concourse.bass as bass
import concourse.mybir as mybir
from concourse.bass_utils import run_bass_kernel_spmd

F32 = mybir.dt.float32
F32R = mybir.dt.float32r
BF16 = mybir.dt.bfloat16
I32 = mybir.dt.int32
AF = mybir.ActivationFunctionType
ALU = mybir.AluOpType

D = 2048
DFF = 5632
DEPTH = 4
NCH = D // 128
NJ = DFF // 128
H = 8
NT = 512
NS = 16
SEQ = 2048
NTILE = SEQ // NT
PAST = 16384
INW = 9216
SEM_LIMIT = 30000
NWS = 5
NDMASEM = 6
SLOTW = 128
ARENA_W = 43 * 1024
ARENA_R_W = 9088


class SemObj:
    __slots__ = ("sem", "count")

    def __init__(self, sem):
        self.sem = sem
        self.count = 0


class Eng:
    def __init__(self, name, obj, T):
        self.name = name
        self.obj = obj
        self.T = T
        self.cur = None
        self.seen = {}

    def mark(self):
        if self.cur is None or self.cur.count >= SEM_LIMIT:
            self.cur = self.T.new_sem(self.name)
        self.cur.count += 1
        return (self.cur, self.cur.count)


class Buf:
    __slots__ = ("ap", "slots", "name")

    def __init__(self, ap, slots, name=""):
        self.ap = ap
        self.slots = slots
        self.name = name


class Tracker:
    def __init__(self, nc, es):
        self.nc = nc
        self.es = es
        self.dry = False
        self.nsem = 0
        self.engs = {}
        for name, obj in (("pe", nc.tensor), ("act", nc.scalar), ("dve", nc.vector),
                          ("pool", nc.gpsimd), ("sp", nc.sync)):
            self.engs[name] = Eng(name, obj, self)
        self.dma_sems = {}
        self.dma_rr = {}
        self.state = {}
        self.n_inst = 0

    def new_sem(self, name):
        self.nsem += 1
        return SemObj(self.es.enter_context(self.nc.semaphore(f"s_{name}_{self.nsem}")))

    def _deps(self, reads, writes):
        deps = {}
        st = self.state
        for b in reads:
            for s in b.slots:
                e = st.get(s)
                if e is not None and e[0] is not None:
                    so, v = e[0]
                    if deps.get(so, 0) < v:
                        deps[so] = v
        for b in writes:
            for s in b.slots:
                e = st.get(s)
                if e is not None:
                    if e[0] is not None:
                        so, v = e[0]
                        if deps.get(so, 0) < v:
                            deps[so] = v
                    for so, v in e[1].items():
                        if deps.get(so, 0) < v:
                            deps[so] = v
        return deps

    def _update(self, mark, reads, writes):
        st = self.state
        so, v = mark
        for b in reads:
            for s in b.slots:
                e = st.get(s)
                if e is None:
                    st[s] = [None, {so: v}]
                else:
                    e[1][so] = v
        for b in writes:
            for s in b.slots:
                st[s] = [mark, {}]

    def _wait(self, E, deps, skip_self=None):
        for so, v in deps.items():
            if skip_self is not None and so is skip_self:
                continue
            if E.seen.get(so, 0) < v:
                E.obj.wait_ge(so.sem, v)
                E.seen[so] = v

    def issue(self, eng, fn, reads=(), writes=()):
        if self.dry:
            return
        E = self.engs[eng]
        deps = self._deps(reads, writes)
        self._wait(E, deps, skip_self=(E.cur if eng == "pe" else None))
        inst = fn(E.obj)
        mark = E.mark()
        inst.then_inc(mark[0].sem, 1)
        self._update(mark, reads, writes)
        self.n_inst += 1

    def dma(self, queue, out, in_, reads=(), writes=()):
        if self.dry:
            return
        E = self.engs[queue]
        lst = self.dma_sems.setdefault(queue, [])
        rr = self.dma_rr.get(queue, 0)
        self.dma_rr[queue] = rr + 1
        if len(lst) < NDMASEM:
            lst.append(self.new_sem("dma" + queue))
        so = lst[rr % NDMASEM]
        deps = self._deps(reads, writes)
        if so.count > 0:
            deps[so] = max(deps.get(so, 0), so.count)
        self._wait(E, deps)
        inst = E.obj.dma_start(out=out, in_=in_)
        so.count += 16
        inst.then_inc(so.sem, 16)
        self._update((so, so.count), reads, writes)
        self.n_inst += 1

    def final_wait(self, eng="sp"):
        if self.dry:
            return
        E = self.engs[eng]
        for q, lst in self.dma_sems.items():
            for so in lst:
                if so.count > 0 and E.seen.get(so, 0) < so.count:
                    E.obj.wait_ge(so.sem, so.count)
                    E.seen[so] = so.count
        for name, e2 in self.engs.items():
            if e2.cur is not None and e2.cur.count > 0 and name != eng:
                E.obj.wait_ge(e2.cur.sem, e2.cur.count)


class Arena:
    def __init__(self, nc, es, T, name="arena", width=None, space="sb"):
        self.W = width
        self.space = space
        self.t = es.enter_context(nc.sbuf_tensor(name, [128, width], F32))
        self.top = 0
        self.T = T

    def alloc(self, words, name=""):
        words = (words + SLOTW - 1) // SLOTW * SLOTW
        o = self.top
        self.top += words
        assert self.top <= self.W, f"arena overflow at {name}: {self.top}"
        return self.view(o, words, name)

    def view(self, o, words, name=""):
        ap = self.t[:, o:o + words]
        slots = [(self.space, i) for i in range(o // SLOTW, (o + words) // SLOTW)]
        return Buf(ap, slots, name)

    def mark(self):
        return self.top

    def release(self, m):
        self.top = m


def sub(buf, o, words):
    s0 = buf.slots[0][1] + o // SLOTW
    s1 = buf.slots[0][1] + (o + words + SLOTW - 1) // SLOTW
    return Buf(buf.ap[:, o:o + words], [(buf.slots[0][0], i) for i in range(s0, s1)], buf.name)


def bf(buf):
    return buf.ap.bitcast(BF16)


def fr(buf):
    return buf.ap.bitcast(F32R)


def build(debug_stage=99):
    nc = bass.Bass("TRN2", target_bir_lowering=False)
    es = ExitStack()
    T = Tracker(nc, es)
    A = Arena(nc, es, T, "arena", ARENA_W, "sb")
    AR = Arena(nc, es, T, "arena_r", ARENA_R_W, "sr")

    def din(name, shape, dt=F32):
        return nc.dram_tensor(name, list(shape), dt, kind="ExternalInput").ap()

    def dout(name, shape):
        return nc.dram_tensor(name, list(shape), F32, kind="ExternalOutput").ap()

    xp = din("xp", [SEQ, D])
    xs = din("xs", [NS, D])
    st_ret = din("st_ret", [DEPTH, NS, H, 128, 128])
    st_re = din("st_re", [DEPTH, NS, 4096])
    st_im = din("st_im", [DEPTH, NS, 4096])
    Wd = {}
    for nm, shp in (("ffn1_w1", [DEPTH, D, DFF]), ("ffn1_w3", [DEPTH, D, DFF]), ("ffn1_w2", [DEPTH, DFF, D]),
                    ("w_in", [DEPTH, D, INW]), ("ret_proj", [DEPTH, 1024, D]), ("glu_w", [DEPTH, 1024, 1024]),
                    ("s5_proj", [DEPTH, 1024, D]), ("w_out", [DEPTH, D, D]),
                    ("ffn2_w1", [DEPTH, D, DFF]), ("ffn2_w3", [DEPTH, D, DFF]), ("ffn2_w2", [DEPTH, DFF, D])):
        Wd[nm] = din(nm, shp)
    gains = din("gains", [13, 16, 128])
    vec8 = din("vec8", [12, 8, 128])
    lam_re = din("s5_lam_re", [DEPTH, 64, 64])
    lam_im = din("s5_lam_im", [DEPTH, 64, 64])
    logstep = din("s5_log_step", [DEPTH, 64, 1])
    b_re = din("s5_b_re", [DEPTH, 64, 64, 16])
    b_im = din("s5_b_im", [DEPTH, 64, 64, 16])
    c_re = din("s5_c_re", [DEPTH, 1024, 64])
    c_im = din("s5_c_im", [DEPTH, 1024, 64])
    consts = din("consts", [128, CONST_W])
    constsr = din("constsr", [128, CONSTR_W])
    rope = din("rope", [NTILE + 1, 2, 128, NT])

    yp = dout("yp", [SEQ, D])
    ys = dout("ys", [NS, D])
    o_ret_p = dout("o_ret_p", [DEPTH, H, 128, 128])
    o_re_p = dout("o_re_p", [DEPTH, 32, 128])
    o_im_p = dout("o_im_p", [DEPTH, 32, 128])
    o_ret_s = dout("o_ret_s", [DEPTH, NS, H, 128, 128])
    o_re_s = dout("o_re_s", [DEPTH, NS * 32, 128])
    o_im_s = dout("o_im_s", [DEPTH, NS * 32, 128])
    scr_ret = nc.dram_tensor("scr_ret", [DEPTH, H, 128, 128], F32, kind="Internal").ap()
    scr_buf = [[Buf(None, [("dram", l, h)]) for h in range(H)] for l in range(DEPTH)]

    PS = []
    for i in range(8):
        t = es.enter_context(nc.psum_tensor(f"ps{i}", [128, 512], F32))
        PS.append(Buf(t[:], [("ps", i)], f"ps{i}"))
    ps_rr = [0]

    ps_free = list(range(8))

    def psum():
        b = PS[ps_free[ps_rr[0] % len(ps_free)]]
        ps_rr[0] += 1
        return b

    def psum_pin():
        b = psum()
        ps_free.remove(b.slots[0][1])
        return b

    def psum_unpin(b):
        ps_free.append(b.slots[0][1])
        ps_free.sort()

    CONST = A.alloc(CONST_W, "const")
    CONSTR = AR.alloc(CONSTR_W, "constr")
    X = A.alloc(NCH * NT, "X")
    XN = A.alloc(NCH * NT // 2, "XN")
    WS = [A.alloc(2048, f"ws{i}") for i in range(NWS)]
    GAIN = A.alloc(13 * 16, "gain")
    VEC8 = A.alloc(12 * 8, "vec8")
    ROPE_H = [None]
    S5ST = A.alloc(DEPTH * 64, "s5st")

    cv = CONST.ap
    ident = cv[:, C_ID:C_ID + 128]
    cr = fr(CONSTR)
    perm_r = cr[:, R_PERM:R_PERM + 128]
    ones_r = cr[:, R_ONES:R_ONES + 128]

    def cbuf():
        return CONST

    class WStream:
        def __init__(self, slots):
            self.slots = slots
            self.N = len(slots)
            self.sched = []
            self.pos = 0
            self.issued = 0

        def reset(self):
            self.pos = 0
            self.issued = 0

        def get(self, mk):
            N = self.N
            if T.dry:
                self.sched.append(mk)
                return self.slots[(len(self.sched) - 1) % N]
            idx = self.pos
            self.pos += 1
            while self.issued < min(len(self.sched), idx + N - 1):
                slot = self.slots[self.issued % N]
                for (o_ap, i_ap) in self.sched[self.issued](slot):
                    T.dma("pool", o_ap, i_ap, writes=[slot])
                self.issued += 1
            return self.slots[idx % N]

    X_FULL = list(X.slots)
    XN_FULL = list(XN.slots)
    WS_EXTRA = [sub(X, 2048, 2048), sub(X, 4096, 2048), sub(X, 6144, 2048), sub(XN, 2048, 2048)]
    WSR_MAIN = WStream(WS)
    WSR_SAMP = WStream(WS + WS_EXTRA)

    class _WH:
        cur = WSR_MAIN

        @staticmethod
        def get(mk):
            return _WH.cur.get(mk)

        @staticmethod
        def reset():
            WSR_MAIN.reset()
            WSR_SAMP.reset()
            _WH.cur = WSR_MAIN

    WSR = _WH

    def slabK(name, l, c0, ncol, kch):
        def mk(slot):
            o = bf(slot)[:, 0:kch * ncol].rearrange("p (k n) -> p k n", k=kch)
            i = Wd[name][l, :, c0:c0 + ncol].rearrange("(k p) n -> p k n", p=128)
            return [(o, i)]
        return mk

    def slabR(name, l, r0, nr):
        def mk(slot):
            o = bf(slot)[:, 0:nr * 2048].rearrange("p (j n) -> p j n", j=nr)
            i = Wd[name][l, r0 * 128:(r0 + nr) * 128, :].rearrange("(j p) n -> p j n", p=128)
            return [(o, i)]
        return mk

    def mm(out_buf, out_ap, lhsT, rhs, start, stop, reads):
        T.issue("pe", lambda e: e.matmul(out_ap, lhsT=lhsT, rhs=rhs, start=start, stop=stop),
                reads=reads, writes=[out_buf])

    def transpose(out_buf, out_ap, in_ap, idn, reads):
        T.issue("pe", lambda e: e.transpose(out_ap, in_ap, idn), reads=reads, writes=[out_buf])

    def act(out_ap, in_ap, func, reads, writes, scale=1.0, bias=0.0):
        T.issue("act", lambda e: e.activation(out=out_ap, in_=in_ap, func=func, bias=bias, scale=scale),
                reads=reads, writes=writes)

    def tt(out_ap, a, b, op, reads, writes, eng="dve"):
        T.issue(eng, lambda e: e.tensor_tensor(out=out_ap, in0=a, in1=b, op=op), reads=reads, writes=writes)

    def ts(out_ap, a, s1, s2, op0, op1, reads, writes, eng="dve"):
        T.issue(eng, lambda e: e.tensor_scalar(out=out_ap, in0=a, scalar1=s1, scalar2=s2, op0=op0, op1=op1),
                reads=reads, writes=writes)

    def stt(out_ap, a, s, b, op0, op1, reads, writes):
        T.issue("dve", lambda e: e.scalar_tensor_tensor(out=out_ap, in0=a, scalar=s, in1=b, op0=op0, op1=op1),
                reads=reads, writes=writes)

    def cp(out_ap, in_ap, reads, writes, eng="dve"):
        T.issue(eng, lambda e: e.tensor_copy(out=out_ap, in_=in_ap), reads=reads, writes=writes)

    def xv(nt):
        return X.ap[:, 0:NCH * nt].rearrange("p (c n) -> p c n", c=NCH)

    def xnv(nt):
        return bf(XN)[:, 0:NCH * nt].rearrange("p (c n) -> p c n", c=NCH)

    def load_x(src, nt):
        m = A.mark()
        nb = (nt + 127) // 128
        rows = min(nt, 128)
        for b in range(nb):
            stg = A.alloc(D, "xstage")
            T.dma("sp", stg.ap[0:rows, :], src[b * 128:b * 128 + rows, :], writes=[stg])
            for c4 in range(4):
                p = psum()
                for q in range(4):
                    c = c4 * 4 + q
                    transpose(p, p.ap[:, q * 128:q * 128 + rows], stg.ap[0:rows, c * 128:(c + 1) * 128],
                              ident[0:rows, 0:rows], [stg, CONST])
                o = xv(nt)[:, c4 * 4:(c4 + 1) * 4, b * 128:b * 128 + rows]
                i = p.ap.rearrange("p (q n) -> p q n", q=4)[:, :, 0:rows]
                (cp if (c4 % 2 == 0) else (lambda o_, i_, r_, w_: act(o_, i_, AF.Copy, r_, w_)))(o, i, [p], [X])
            if b % 2 == 1:
                A.release(m)
        A.release(m)

    def rmsnorm(nt, grow, out_kind):
        m = A.mark()
        mr = AR.mark()
        sq = [AR.alloc(nt, "sq0"), AR.alloc(nt, "sq1")]
        rstd = A.alloc(nt, "rstd")
        p = psum()
        for c in range(NCH):
            s = sq[c % 2]
            act(fr(s)[:, 0:nt], xv(nt)[:, c, :], AF.Square, [X], [s])
            mm(p, p.ap[:, 0:nt], ones_r, fr(s)[:, 0:nt], c == 0, c == NCH - 1, [s, CONSTR])
        act(rstd.ap[:, 0:nt], p.ap[:, 0:nt], AF.Sqrt, [p, CONST], [rstd], scale=1.0 / D,
            bias=cv[:, C_EPS6:C_EPS6 + 1])
        T.issue("dve", lambda e: e.reciprocal(out=rstd.ap[:, 0:nt], in_=rstd.ap[:, 0:nt]), reads=[rstd], writes=[rstd])
        g = GAIN.ap[:, grow * 16:(grow + 1) * 16]
        for c in range(NCH):
            if out_kind == "xn":
                stt(xnv(nt)[:, c, :], xv(nt)[:, c, :], g[:, c:c + 1], rstd.ap[:, 0:nt], ALU.mult, ALU.mult,
                    [X, GAIN, rstd], [XN])
            else:
                stt(xv(nt)[:, c, :], xv(nt)[:, c, :], g[:, c:c + 1], rstd.ap[:, 0:nt], ALU.mult, ALU.mult,
                    [X, GAIN, rstd], [X])
        A.release(m)
        AR.release(mr)

    def ffn(nt, l, pref):
        m = A.mark()
        GJ = 8
        Hb = A.alloc(GJ * nt // 2, "H")
        hv = bf(Hb)[:, 0:GJ * nt].rearrange("p (j n) -> p j n", j=GJ)
        sl = [A.alloc(nt, "silu0"), A.alloc(nt, "silu1")]
        xn = xnv(nt)
        j = 0
        cnt = 0
        while j < NJ:
            gj = min(GJ, NJ - j)
            for jj in range(0, gj, 2):
                w1 = WSR.get(slabK(pref + "_w1", l, (j + jj) * 128, 256, 16))
                w3 = WSR.get(slabK(pref + "_w3", l, (j + jj) * 128, 256, 16))
                w1v = bf(w1)[:, 0:4096].rearrange("p (k n) -> p k n", k=16)
                w3v = bf(w3)[:, 0:4096].rearrange("p (k n) -> p k n", k=16)
                for q in range(2):
                    p1 = psum()
                    p3 = psum()
                    for k in range(16):
                        mm(p1, p1.ap[:, 0:nt], w1v[:, k, q * 128:(q + 1) * 128], xn[:, k, :], k == 0, k == 15, [w1, XN])
                    for k in range(16):
                        mm(p3, p3.ap[:, 0:nt], w3v[:, k, q * 128:(q + 1) * 128], xn[:, k, :], k == 0, k == 15, [w3, XN])
                    s = sl[cnt % 2]
                    cnt += 1
                    act(s.ap[:, 0:nt], p1.ap[:, 0:nt], AF.Silu, [p1], [s])
                    tt(hv[:, jj + q, :], s.ap[:, 0:nt], p3.ap[:, 0:nt], ALU.mult, [s, p3], [Hb])
            for c2 in range(8):
                def mk(slot, c2=c2, j=j, gj=gj):
                    o = bf(slot)[:, 0:gj * 256].rearrange("p (r n) -> p r n", r=gj)
                    i = Wd[pref + "_w2"][l, j * 128:(j + gj) * 128, c2 * 256:(c2 + 1) * 256] \
                        .rearrange("(r p) n -> p r n", p=128)
                    return [(o, i)]
                w2 = WSR.get(mk)
                wv = bf(w2)[:, 0:gj * 256].rearrange("p (r n) -> p r n", r=gj)
                for q in range(2):
                    p = psum()
                    for ji in range(gj):
                        mm(p, p.ap[:, 0:nt], wv[:, ji, q * 128:(q + 1) * 128], hv[:, ji, :], ji == 0, ji == gj - 1,
                           [w2, Hb])
                    cc = c2 * 2 + q
                    stt(xv(nt)[:, cc, :], p.ap[:, 0:nt], 0.5, xv(nt)[:, cc, :], ALU.mult, ALU.add, [p, X], [X])
            j += gj
        A.release(m)

    def store_y(dst, nt):
        m = A.mark()
        nb = (nt + 127) // 128
        rows = min(nt, 128)
        for b in range(nb):
            stg = A.alloc(D, "ystage")
            for c4 in range(4):
                p = psum()
                for q in range(4):
                    c = c4 * 4 + q
                    transpose(p, p.ap[0:rows, q * 128:(q + 1) * 128], xv(nt)[:, c, b * 128:b * 128 + rows], ident, [X, CONST])
                o = stg.ap[0:rows, c4 * 512:(c4 + 1) * 512]
                if c4 % 2 == 0:
                    cp(o, p.ap[0:rows, :], [p], [stg])
                else:
                    act(o, p.ap[0:rows, :], AF.Copy, [p], [stg])
            T.dma("sp", dst[b * 128:b * 128 + rows, :], stg.ap[0:rows, :], reads=[stg])
            if b % 2 == 1:
                A.release(m)
        A.release(m)


    GH = [1.0 - 2.0 ** (-5.0 - h) for h in range(H)]
    GC = [g ** 128 for g in GH]
    maskT = cv[:, C_MASK:C_MASK + 128]
    ident_r = cr[:, R_ID:R_ID + 128]
    onesdiv_r = cr[:, R_ODIV:R_ODIV + 128]

    def head_norm_gen(po_ap, n, l, head, srg_ap, srg_buf, out_ap, out_buf, pbuf, bufs):
        osb, o2, mean, var, t1 = bufs
        act(fr(osb)[:, 0:n], po_ap, AF.Copy, [pbuf], [osb])
        act(fr(o2)[:, 0:n], po_ap, AF.Square, [pbuf], [o2])
        yield
        pm = psum()
        mm(pm, pm.ap[:, 0:n], onesdiv_r, fr(osb)[:, 0:n], True, True, [CONSTR, osb])
        pv = psum()
        mm(pv, pv.ap[:, 0:n], onesdiv_r, fr(o2)[:, 0:n], True, True, [CONSTR, o2])
        yield
        act(mean.ap[:, 0:n], pm.ap[:, 0:n], AF.Copy, [pm], [mean])
        yield
        tt(var.ap[:, 0:n], mean.ap[:, 0:n], mean.ap[:, 0:n], ALU.mult, [mean], [var])
        yield
        tt(var.ap[:, 0:n], pv.ap[:, 0:n], var.ap[:, 0:n], ALU.subtract, [pv, var], [var])
        yield
        act(var.ap[:, 0:n], var.ap[:, 0:n], AF.Sqrt, [var, CONST], [var], bias=cv[:, C_EPS5:C_EPS5 + 1])
        yield
        T.issue("dve", lambda e: e.reciprocal(out=var.ap[:, 0:n], in_=var.ap[:, 0:n]), reads=[var], writes=[var])
        yield
        tt(t1.ap[:, 0:n], osb.ap[:, 0:n], mean.ap[:, 0:n], ALU.subtract, [osb, mean], [t1])
        yield
        tt(t1.ap[:, 0:n], t1.ap[:, 0:n], var.ap[:, 0:n], ALU.mult, [t1, var], [t1])
        yield
        gn = VEC8.ap[:, (l * 3 + 0) * 8 + head:(l * 3 + 0) * 8 + head + 1]
        stt(out_ap, t1.ap[:, 0:n], gn, srg_ap, ALU.mult, ALU.mult, [t1, VEC8, srg_buf], [out_buf])
        yield

    def head_norm(po_ap, n, l, head, srg_ap, srg_buf, out_ap, out_buf, pbuf):
        m = A.mark()
        mr = AR.mark()
        bufs = (AR.alloc(n, "osb"), AR.alloc(n, "o2"), A.alloc(n, "mean"), A.alloc(n, "var"), A.alloc(n, "hn_t1"))
        for _ in head_norm_gen(po_ap, n, l, head, srg_ap, srg_buf, out_ap, out_buf, pbuf, bufs):
            pass
        A.release(m)
        AR.release(mr)

    def proj16(slab, q, n, ncol=256):
        wv = bf(slab)[:, 0:16 * ncol].rearrange("p (k n) -> p k n", k=16)
        p = psum()
        for k in range(16):
            mm(p, p.ap[:, 0:n], wv[:, k, q * 128:(q + 1) * 128], xnv(n)[:, k, :], k == 0, k == 15, [slab, XN])
        return p

    def rotary(p, n, scale, tab_mul, name, res=None):
        if res is None:
            res = AR.alloc(n, name + "rot")
        m_ = A.mark()
        mr_ = AR.mark()
        raw = AR.alloc(n, name + "raw")
        t1 = A.alloc(n, name + "t1")
        act(fr(raw)[:, 0:n], p.ap[:, 0:n], AF.Copy, [p], [raw], scale=scale)
        pr = psum()
        mm(pr, pr.ap[:, 0:n], perm_r, fr(raw)[:, 0:n], True, True, [CONSTR, raw])
        tt(t1.ap[:, 0:n], raw.ap[:, 0:n], ROPE_H[0].ap[:, 0:n], ALU.mult, [raw, ROPE_H[0]], [t1])
        if tab_mul is None:
            tt(fr(res)[:, 0:n], pr.ap[:, 0:n], ROPE_H[0].ap[:, NT:NT + n], ALU.mult, [pr, ROPE_H[0]], [res])
            tt(fr(res)[:, 0:n], res.ap[:, 0:n], t1.ap[:, 0:n], ALU.add, [res, t1], [res])
        else:
            t2 = A.alloc(n, name + "t2")
            tt(t2.ap[:, 0:n], pr.ap[:, 0:n], ROPE_H[0].ap[:, NT:NT + n], ALU.mult, [pr, ROPE_H[0]], [t2])
            tt(t1.ap[:, 0:n], t1.ap[:, 0:n], t2.ap[:, 0:n], ALU.add, [t1, t2], [t1])
            tt(fr(res)[:, 0:n].rearrange("p (c i) -> p c i", i=128),
               t1.ap[:, 0:n].rearrange("p (c i) -> p c i", i=128),
               tab_mul.unsqueeze(1).broadcast_to([128, n // 128, 128]), ALU.mult, [t1, CONST], [res])
        A.release(m_)
        AR.release(mr_)
        return res

    def retention_prompt(ti, l, OALL):
        oall = bf(OALL)[:, 0:H * NT].rearrange("p (h n) -> p h n", h=H)
        n = NT
        ncx = n // 128
        for hp in range(4):
            m = A.mark()
            mr = AR.mark()
            wv_s = WSR.get(slabK("w_in", l, 2048 + hp * 256, 256, 16))
            wvv = bf(wv_s)[:, 0:4096].rearrange("p (k n) -> p k n", k=16)
            vtok = AR.alloc(ncx * 256, "vtok")
            vtv = fr(vtok)[:, 0:ncx * 256].rearrange("p (c e) -> p c e", c=ncx)
            for c2 in range(ncx // 2):
                pv = psum()
                for cc in range(2):
                    c = c2 * 2 + cc
                    for k in range(16):
                        mm(pv, pv.ap[:, cc * 256:(cc + 1) * 256], xnv(n)[:, k, c * 128:(c + 1) * 128], wvv[:, k, :],
                           k == 0, k == 15, [XN, wv_s])
                act(fr(vtok)[:, c2 * 512:(c2 + 1) * 512], pv.ap, AF.Copy, [pv], [vtok])
            qd = [AR.alloc(n, "qd0"), AR.alloc(n, "qd1")]
            kd = [AR.alloc(n, "kd0"), AR.alloc(n, "kd1")]
            wq_s = WSR.get(slabK("w_in", l, hp * 256, 256, 16))
            for hh in range(2):
                pq = proj16(wq_s, hh, n)
                rotary(pq, n, 128.0 ** -0.5, cv[:, C_QDEC + (2 * hp + hh) * 128:C_QDEC + (2 * hp + hh + 1) * 128], f"q{hh}", qd[hh])
            wk_s = WSR.get(slabK("w_in", l, 1024 + hp * 256, 256, 16))
            for hh in range(2):
                pk = proj16(wk_s, hh, n)
                rotary(pk, n, 1.0, cv[:, C_KDI + (2 * hp + hh) * 128:C_KDI + (2 * hp + hh + 1) * 128], f"k{hh}", kd[hh])
            wr_s = WSR.get(slabK("w_in", l, 3072 + hp * 256, 256, 16))
            srg = []
            for hh in range(2):
                pg = proj16(wr_s, hh, n)
                sb = A.alloc(n, f"srg{hh}")
                act(sb.ap[:, 0:n], pg.ap[:, 0:n], AF.Silu, [pg], [sb])
                srg.append(sb)
            def head_ops(hh, HB):
                head = 2 * hp + hh
                scT, kdt, Sall, tmp, hn = HB
                qv = fr(qd[hh])
                kv = fr(kd[hh])
                ps_ = psum()
                for c in range(ncx):
                    mm(ps_, ps_.ap[:, c * 128:(c + 1) * 128], kv[:, c * 128:(c + 1) * 128], qv[:, c * 128:(c + 1) * 128],
                       True, True, [kd[hh], qd[hh]])
                tt(fr(scT)[:, 0:n].rearrange("p (c i) -> p c i", i=128), ps_.ap[:, 0:n].rearrange("p (c i) -> p c i", i=128),
                   maskT.unsqueeze(1).broadcast_to([128, ncx, 128]), ALU.mult, [ps_, CONST], [scT])
                yield
                pt = psum()
                for c in range(ncx):
                    transpose(pt, pt.ap.bitcast(F32R)[:, c * 128:(c + 1) * 128], kv[:, c * 128:(c + 1) * 128], ident_r,
                              [kd[hh], CONSTR])
                act(fr(kdt)[:, 0:n], pt.ap[:, 0:n], AF.Copy, [pt], [kdt])
                yield
                pu = psum()
                for c in range(ncx):
                    mm(pu, pu.ap[:, c * 128:(c + 1) * 128], fr(kdt)[:, c * 128:(c + 1) * 128],
                       vtv[:, c, hh * 128:(hh + 1) * 128], True, True, [kdt, vtok])
                Sv = fr(Sall)[:, 0:(ncx + 1) * 128].rearrange("p (c e) -> p c e", e=128)
                Svf = Sall.ap[:, 0:(ncx + 1) * 128].rearrange("p (c e) -> p c e", e=128)
                if ti == 0:
                    ts(Sv[:, 0, :], pu.ap[:, 0:128], 0.0, None, ALU.mult, ALU.bypass, [pu], [Sall])
                else:
                    T.dma("pool", Sv[:, 0, :], scr_ret[l, head], reads=[scr_buf[l][head]], writes=[Sall])
                yield
                for c in range(ncx):
                    tt(tmp.ap[:, 0:128], pu.ap[:, c * 128:(c + 1) * 128], Svf[:, c, :], ALU.add, [pu, Sall], [tmp])
                    yield
                    act(Sv[:, c + 1, :], tmp.ap[:, 0:128], AF.Copy, [tmp], [Sall], scale=GC[head])
                    yield
                dst = o_ret_p[l, head] if ti == NTILE - 1 else scr_ret[l, head]
                T.dma("sp", dst, Svf[:, ncx, :], reads=[Sall], writes=([] if ti == NTILE - 1 else [scr_buf[l][head]]))
                po = psum()
                for c in range(ncx):
                    mm(po, po.ap[:, c * 128:(c + 1) * 128], vtv[:, c, hh * 128:(hh + 1) * 128], fr(scT)[:, c * 128:(c + 1) * 128],
                       True, False, [vtok, scT])
                    mm(po, po.ap[:, c * 128:(c + 1) * 128], Sv[:, c, :], qv[:, c * 128:(c + 1) * 128],
                       False, True, [Sall, qd[hh]])
                yield
                yield from head_norm_gen(po.ap[:, 0:n], n, l, head, srg[hh].ap[:, 0:n], srg[hh], oall[:, head, :], OALL, po, hn)

            HBS = []
            for hh in range(2):
                HBS.append((AR.alloc(n, "scT"), AR.alloc(n, "kdt"), AR.alloc((ncx + 1) * 128, "Sall"), A.alloc(128, "stmp"),
                            (AR.alloc(n, "osb"), AR.alloc(n, "o2"), A.alloc(n, "mean"), A.alloc(n, "var"), A.alloc(n, "hn_t1"))))
            gens = [head_ops(0, HBS[0]), head_ops(1, HBS[1])]
            alive = [True, True]
            while alive[0] or alive[1]:
                for gi in range(2):
                    if alive[gi]:
                        try:
                            next(gens[gi])
                        except StopIteration:
                            alive[gi] = False
            A.release(m)
            AR.release(mr)

    def retention_sample(l, OALL):
        n = NS
        oall = bf(OALL)[:, 0:H * n].rearrange("p (h n) -> p h n", h=H)
        m = A.mark()
        mr = AR.mark()
        QS = A.alloc(H * n, "QS")
        KS = A.alloc(H * n, "KS")
        SRG = A.alloc(H * n, "SRG")
        VT = A.alloc(1024, "VT")
        SEL = A.alloc(n * 128, "SEL")
        cp(SEL.ap[0:n, 0:n * 128].rearrange("p (b d) -> p b d", b=n),
           ident[0:n, 0:n].unsqueeze(2).broadcast_to([n, n, 128]), [CONST], [SEL])
        for hp in range(4):
            m2 = A.mark()
            mr2 = AR.mark()
            wv_s = WSR.get(slabK("w_in", l, 2048 + hp * 256, 256, 16))
            wvv = bf(wv_s)[:, 0:4096].rearrange("p (k n) -> p k n", k=16)
            pv = psum()
            for k in range(16):
                mm(pv, pv.ap[0:n, 0:256], xnv(n)[:, k, :], wvv[:, k, :], k == 0, k == 15, [XN, wv_s])
            cp(VT.ap[0:n, hp * 256:(hp + 1) * 256], pv.ap[0:n, 0:256], [pv], [VT])
            wq_s = WSR.get(slabK("w_in", l, hp * 256, 256, 16))
            for hh in range(2):
                pq = proj16(wq_s, hh, n)
                r = rotary(pq, n, 128.0 ** -0.5, None, "sq")
                cp(QS.ap[:, (2 * hp + hh) * n:(2 * hp + hh + 1) * n], r.ap[:, 0:n], [r], [QS])
            wk_s = WSR.get(slabK("w_in", l, 1024 + hp * 256, 256, 16))
            for hh in range(2):
                pk = proj16(wk_s, hh, n)
                r = rotary(pk, n, 1.0, None, "sk")
                cp(KS.ap[:, (2 * hp + hh) * n:(2 * hp + hh + 1) * n], r.ap[:, 0:n], [r], [KS])
            wr_s = WSR.get(slabK("w_in", l, 3072 + hp * 256, 256, 16))
            for hh in range(2):
                pg = proj16(wr_s, hh, n)
                act(SRG.ap[:, (2 * hp + hh) * n:(2 * hp + hh + 1) * n], pg.ap[:, 0:n], AF.Silu, [pg], [SRG])
            A.release(m2)
            AR.release(mr2)
        po = psum_pin()
        Sb = [A.alloc(1024, "Sb0"), A.alloc(1024, "Sb1")]
        for b in range(n):
            S = Sb[b % 2]
            Sv = S.ap[:, 0:1024].rearrange("p (h e) -> p h e", h=H)
            T.dma("sp", Sv, st_ret[l, b].rearrange("h d e -> d h e"), writes=[S])
            pvb = [psum(), psum()]
            for hf in range(2):
                mm(pvb[hf], pvb[hf].ap[:, 0:512], SEL.ap[0:n, b * 128:(b + 1) * 128], VT.ap[0:n, hf * 512:(hf + 1) * 512],
                   True, True, [SEL, VT])
            for h in range(H):
                act(Sv[:, h, :], Sv[:, h, :], AF.Copy, [S], [S], scale=GH[h])
                stt(Sv[:, h, :], pvb[h // 4].ap[:, (h % 4) * 128:(h % 4 + 1) * 128], KS.ap[:, h * n + b:h * n + b + 1], Sv[:, h, :],
                    ALU.mult, ALU.add, [pvb[h // 4], KS, S], [S])
            for h in range(H):
                mm(po, po.ap[:, h * n + b:h * n + b + 1], Sv[:, h, :], QS.ap[:, h * n + b:h * n + b + 1], True, True, [S, QS])
            T.dma("sp", o_ret_s[l, b].rearrange("h d e -> d h e"), Sv, reads=[S])
        for h in range(H):
            head_norm(po.ap[:, h * n:(h + 1) * n], n, l, h, SRG.ap[:, h * n:(h + 1) * n], SRG, oall[:, h, :], OALL, po)
        psum_unpin(po)
        A.release(m)
        AR.release(mr)

    def merge_and_out(nt, l, OALL, Z, use_s5):
        m = A.mark()
        M = A.alloc(NCH * nt // 2, "M")
        mv = bf(M)[:, 0:NCH * nt].rearrange("p (c n) -> p c n", c=NCH)
        oall = bf(OALL)[:, 0:H * nt].rearrange("p (h n) -> p h n", h=H)
        zv = bf(Z)[:, 0:8 * nt].rearrange("p (h n) -> p h n", h=8) if Z is not None else None
        sg = [A.alloc(nt, f"sg{i}") for i in range(4)]
        for cp_ in range(8):
            wgr = WSR.get(slabK("w_in", l, 5120 + cp_ * 256, 256, 16))
            for q in range(2):
                pg = proj16(wgr, q, nt)
                act(sg[q].ap[:, 0:nt], pg.ap[:, 0:nt], AF.Sigmoid, [pg], [sg[q]])
            wgs = WSR.get(slabK("w_in", l, 7168 + cp_ * 256, 256, 16))
            for q in range(2):
                pg = proj16(wgs, q, nt)
                act(sg[2 + q].ap[:, 0:nt], pg.ap[:, 0:nt], AF.Sigmoid, [pg], [sg[2 + q]])
            wrp = WSR.get(slabK("ret_proj", l, cp_ * 256, 256, 8))
            wrv = bf(wrp)[:, 0:2048].rearrange("p (k n) -> p k n", k=8)
            for q in range(2):
                pb = psum()
                for h in range(H):
                    mm(pb, pb.ap[:, 0:nt], wrv[:, h, q * 128:(q + 1) * 128], oall[:, h, :], h == 0, h == H - 1, [wrp, OALL])
                tt(sg[q].ap[:, 0:nt], sg[q].ap[:, 0:nt], pb.ap[:, 0:nt], ALU.mult, [sg[q], pb], [sg[q]])
            if use_s5:
                wsp = WSR.get(slabK("s5_proj", l, cp_ * 256, 256, 8))
                wsv = bf(wsp)[:, 0:2048].rearrange("p (k n) -> p k n", k=8)
                for q in range(2):
                    pz = psum()
                    for h in range(8):
                        mm(pz, pz.ap[:, 0:nt], wsv[:, h, q * 128:(q + 1) * 128], zv[:, h, :], h == 0, h == 7, [wsp, Z])
                    tt(sg[2 + q].ap[:, 0:nt], sg[2 + q].ap[:, 0:nt], pz.ap[:, 0:nt], ALU.mult, [sg[2 + q], pz], [sg[2 + q]])
                    tt(mv[:, cp_ * 2 + q, :], sg[q].ap[:, 0:nt], sg[2 + q].ap[:, 0:nt], ALU.add, [sg[q], sg[2 + q]], [M])
            else:
                for q in range(2):
                    cp(mv[:, cp_ * 2 + q, :], sg[q].ap[:, 0:nt], [sg[q]], [M])
        for c2 in range(8):
            wo = WSR.get(slabK("w_out", l, c2 * 256, 256, 16))
            wov = bf(wo)[:, 0:4096].rearrange("p (k n) -> p k n", k=16)
            for q in range(2):
                p = psum()
                for k in range(16):
                    mm(p, p.ap[:, 0:nt], wov[:, k, q * 128:(q + 1) * 128], mv[:, k, :], k == 0, k == 15, [wo, M])
                cc = c2 * 2 + q
                tt(xv(nt)[:, cc, :], xv(nt)[:, cc, :], p.ap[:, 0:nt], ALU.add, [X, p], [X])
        A.release(m)


    TWO_PI = 6.28318
    PENG = "dve"
    MASKQ = [cv[:, C_MQ + q * 128:C_MQ + (q + 1) * 128] for q in range(4)]

    def s5_branch(ti, nt, l, Z):
        is_s = ti >= NTILE
        m0 = A.mark()
        mr0 = AR.mark()
        Z0 = A.alloc(8 * nt // 2, "Z0")
        z0v = bf(Z0)[:, 0:8 * nt].rearrange("p (c n) -> p c n", c=8)
        zv = bf(Z)[:, 0:8 * nt].rearrange("p (c n) -> p c n", c=8)
        SU = A.alloc(8 * nt // 2, "SU")
        suv = bf(SU)[:, 0:8 * nt].rearrange("p (c n) -> p c n", c=8)
        for cp_ in range(4):
            ws = WSR.get(slabK("w_in", l, 4096 + cp_ * 256, 256, 16))
            for q in range(2):
                p = proj16(ws, q, nt)
                act(suv[:, cp_ * 2 + q, :], p.ap[:, 0:nt], AF.Copy, [p], [SU])
        PL = A.alloc(4 * 32, "s5pl")
        mG = A.mark()
        G = A.alloc(64 * 12, "s5g")
        gt = lambda i: G.ap[0:64, i * 64:(i + 1) * 64]
        LRE, LIM, RHO, TH, SN, CS, XI, T0, T1, SRE, SIM, DTB = range(12)
        T.dma("sp", gt(LRE), lam_re[l], writes=[G])
        T.dma("sp", gt(LIM), lam_im[l], writes=[G])
        T.dma("sp", gt(DTB)[:, 0:1], logstep[l], writes=[G])
        GB = [G]
        act(gt(DTB)[:, 1:2], gt(DTB)[:, 0:1], AF.Exp, GB, GB)
        dtc = gt(DTB)[:, 1:2]
        act(gt(RHO), gt(LRE), AF.Exp, GB, GB, scale=dtc)
        ts(gt(TH), gt(LIM), dtc, 1.0 / (2 * math.pi), ALU.mult, ALU.mult, GB, GB)
        xi = G.ap.bitcast(I32)[0:64, XI * 64:(XI + 1) * 64]
        cp(xi, gt(TH), GB, GB)
        tt(gt(T0), gt(TH), xi, ALU.subtract, GB, GB)
        act(gt(SN), gt(T0), AF.Sin, GB, GB, scale=TWO_PI)
        ts(gt(T1), gt(TH), 0.25, None, ALU.add, ALU.bypass, GB, GB)
        cp(xi, gt(T1), GB, GB)
        tt(gt(T0), gt(T1), xi, ALU.subtract, GB, GB)
        act(gt(CS), gt(T0), AF.Sin, GB, GB, scale=TWO_PI)
        tt(gt(CS), gt(CS), gt(RHO), ALU.mult, GB, GB)
        tt(gt(SN), gt(SN), gt(RHO), ALU.mult, GB, GB)
        ts(gt(CS), gt(CS), -1.0, None, ALU.add, ALU.bypass, GB, GB)
        tt(gt(T0), gt(LRE), gt(LRE), ALU.mult, GB, GB)
        tt(gt(T1), gt(LIM), gt(LIM), ALU.mult, GB, GB)
        tt(gt(T0), gt(T0), gt(T1), ALU.add, GB, GB)
        T.issue("dve", lambda e: e.reciprocal(out=gt(T0), in_=gt(T0)), reads=GB, writes=GB)
        tt(gt(SRE), gt(CS), gt(LRE), ALU.mult, GB, GB)
        tt(gt(T1), gt(SN), gt(LIM), ALU.mult, GB, GB)
        tt(gt(SRE), gt(SRE), gt(T1), ALU.add, GB, GB)
        tt(gt(SRE), gt(SRE), gt(T0), ALU.mult, GB, GB)
        tt(gt(SIM), gt(SN), gt(LRE), ALU.mult, GB, GB)
        tt(gt(T1), gt(CS), gt(LIM), ALU.mult, GB, GB)
        tt(gt(SIM), gt(SIM), gt(T1), ALU.subtract, GB, GB)
        tt(gt(SIM), gt(SIM), gt(T0), ALU.mult, GB, GB)
        Lm = A.alloc(128, "s5L")
        for i, src in enumerate((RHO, TH, SRE, SIM)):
            ts(Lm.ap[0:64, 0:64], gt(src), cv[0:64, C_EVEN:C_EVEN + 1], None, ALU.mult, ALU.bypass, [G, CONST], [Lm])
            ts(Lm.ap[0:64, 64:128], gt(src), cv[0:64, C_ODD:C_ODD + 1], None, ALU.mult, ALU.bypass, [G, CONST], [Lm])
            p = psum()
            mm(p, p.ap[:, 0:32], Lm.ap[0:64, 0:128], cv[0:64, C_SEL:C_SEL + 32], True, True, [Lm, CONST])
            cp(PL.ap[:, i * 32:(i + 1) * 32], p.ap[:, 0:32], [p], [PL])
        A.release(mG)
        pl = lambda i, pair: PL.ap[:, i * 32 + pair:i * 32 + pair + 1]
        if is_s:
            HIN = A.alloc(2 * 512, "hin")
            HOUT = A.alloc(2 * 512, "hout")
            for ri, src in enumerate((st_re, st_im)):
                stg = A.alloc(512, "hstg")
                T.dma("sp", stg.ap[:, 0:512].rearrange("r (k m) -> r k m", k=4),
                      src[l].rearrange("b (pr m) -> (b pr) m", m=128).rearrange("(k r) m -> r k m", r=128), writes=[stg])
                p = psum()
                for k in range(4):
                    transpose(p, p.ap[:, k * 128:(k + 1) * 128], stg.ap[:, k * 128:(k + 1) * 128], ident, [stg, CONST])
                cp(HIN.ap[:, ri * 512:(ri + 1) * 512], p.ap, [p], [HIN])
        tv = cv[:, C_ONE16:C_ONE16 + nt] if is_s else cv[:, C_TV:C_TV + nt]
        wm = A.mark()
        wmr = AR.mark()
        for ch in range(8):
            A.release(wm)
            AR.release(wmr)
            BR = A.alloc(4 * 64, "s5br")
            brv = lambda i: BR.ap[:, i * 64:(i + 1) * 64].rearrange("p (a c) -> p a c", a=4)
            T.dma("sp", brv(0), b_re[l, 8 * ch:8 * ch + 8].rearrange("(a gl) p c -> (gl p) a c", gl=2), writes=[BR])
            T.dma("sp", brv(1), b_im[l, 8 * ch:8 * ch + 8].rearrange("(a gl) p c -> (gl p) a c", gl=2), writes=[BR])
            sre = PL.ap[:, 2 * 32 + 4 * ch:2 * 32 + 4 * ch + 4].unsqueeze(2).broadcast_to([128, 4, 16])
            sim = PL.ap[:, 3 * 32 + 4 * ch:3 * 32 + 4 * ch + 4].unsqueeze(2).broadcast_to([128, 4, 16])
            BB = A.alloc(4 * 64, "s5bb")
            bbv = lambda i: BB.ap[:, i * 64:(i + 1) * 64].rearrange("p (a c) -> p a c", a=4)
            tt(bbv(0), brv(0), sre, ALU.mult, [BR, PL], [BB])
            tt(bbv(2), brv(1), sim, ALU.mult, [BR, PL], [BB])
            tt(bbv(0), bbv(0), bbv(2), ALU.subtract, [BB], [BB])
            tt(bbv(1), brv(1), sre, ALU.mult, [BR, PL], [BB])
            tt(bbv(2), brv(0), sim, ALU.mult, [BR, PL], [BB])
            tt(bbv(1), bbv(1), bbv(2), ALU.add, [BB], [BB])
            BL = A.alloc(8 * 64, "s5bl")
            blv = lambda a, ri: bf(BL)[:, (a * 2 + ri) * 128:(a * 2 + ri + 1) * 128]
            XP = [A.alloc(128, "s5xp0"), A.alloc(128, "s5xp1")]
            for a in range(4):
                for ri in range(2):
                    xp_ = XP[(a * 2 + ri) % 2]
                    tt(xp_.ap[:, 0:128].rearrange("p (g c) -> p g c", g=8),
                       bbv(ri)[:, a:a + 1, :].broadcast_to([128, 8, 16]) if False else
                       BB.ap[:, ri * 64 + a * 16:ri * 64 + (a + 1) * 16].unsqueeze(1).broadcast_to([128, 8, 16]),
                       MASKQ[a].rearrange("p (g c) -> p g c", g=8), ALU.mult, [BB, CONST], [xp_])
                    p = psum()
                    transpose(p, p.ap[:, 0:128], xp_.ap[:, 0:128], ident, [xp_, CONST])
                    act(blv(a, ri), p.ap[:, 0:128], AF.Copy, [p], [BL])
            CC = A.alloc(256, "s5cc")
            for ri, src in enumerate((c_re, c_im)):
                for hf in range(2):
                    T.dma("sp", CC.ap[:, ri * 128 + hf * 64:ri * 128 + (hf + 1) * 64], src[l, ch * 128:(ch + 1) * 128, :], writes=[CC])
            CL = AR.alloc(8 * 128, "s5cl")
            clv = lambda a, ri: fr(CL)[:, (a * 2 + ri) * 128:(a * 2 + ri + 1) * 128]
            for ri in range(2):
                p = psum()
                transpose(p, p.ap[:, 0:128], CC.ap[:, ri * 128:(ri + 1) * 128], ident, [CC, CONST])
                for a in range(4):
                    stt(clv(a, ri), p.ap[:, 0:128], (1.0 if ri == 0 else -1.0), MASKQ[a], ALU.mult, ALU.mult, [p, CONST], [CL])
            py = psum_pin()
            WSETS = [[A.alloc(nt, f"s5w{s_}_{i}") for i in range(6)] for s_ in range(2)]
            PSETS = [[AR.alloc(nt, f"s5p{s_}_{i}") for i in range(4)] for s_ in range(2)]
            def pair_ops(a):
                pair = 4 * ch + a
                COS, SIN, XA, XB, TA, TB = WSETS[a % 2]
                XI_ = TB
                w = lambda b_: b_.ap[:, 0:nt]
                xiv = XI_.ap.bitcast(I32)[:, 0:nt]
                act(w(XA), tv, AF.Identity, [CONST, PL], [XA], scale=pl(1, pair))
                cp(xiv, w(XA), [XA], [XI_])
                yield
                tt(w(XB), w(XA), xiv, ALU.subtract, [XA, XI_], [XB])
                yield
                act(w(SIN), w(XB), AF.Sin, [XB], [SIN], scale=TWO_PI)
                act(w(XA), tv, AF.Identity, [CONST, PL], [XA], scale=pl(1, pair), bias=cv[:, C_QUARTER:C_QUARTER + 1])
                cp(xiv, w(XA), [XA], [XI_])
                yield
                tt(w(XB), w(XA), xiv, ALU.subtract, [XA, XI_], [XB])
                yield
                act(w(COS), w(XB), AF.Sin, [XB], [COS], scale=TWO_PI)
                pre = psum()
                pim = psum()
                mm(pre, pre.ap[:, 0:nt], blv(a, 0), suv[:, ch, :], True, True, [BL, SU])
                mm(pim, pim.ap[:, 0:nt], blv(a, 1), suv[:, ch, :], True, True, [BL, SU])
                tt(w(XA), pre.ap[:, 0:nt], w(COS), ALU.mult, [pre, COS], [XA])
                yield
                tt(w(TA), pim.ap[:, 0:nt], w(SIN), ALU.mult, [pim, SIN], [TA])
                yield
                tt(w(XA), w(XA), w(TA), ALU.add, [XA, TA], [XA])
                yield
                tt(w(XB), pim.ap[:, 0:nt], w(COS), ALU.mult, [pim, COS], [XB])
                yield
                tt(w(TA), pre.ap[:, 0:nt], w(SIN), ALU.mult, [pre, SIN], [TA])
                yield
                tt(w(XB), w(XB), w(TA), ALU.subtract, [XB, TA], [XB])
                yield
                rho = pl(0, pair)
                if is_s:
                    hre = HIN.ap[:, pair:512:32]
                    him = HIN.ap[:, 512 + pair:1024:32]
                    stt(w(TA), hre, rho, w(XA), ALU.mult, ALU.add, [HIN, PL, XA], [TA])
                    yield
                    stt(w(TB), him, rho, w(XB), ALU.mult, ALU.add, [HIN, PL, XB], [TB])
                    yield
                else:
                    sre0 = S5ST.ap[:, l * 64 + pair:l * 64 + pair + 1]
                    sim0 = S5ST.ap[:, l * 64 + 32 + pair:l * 64 + 32 + pair + 1]
                    T.issue("dve", lambda e: e.tensor_tensor_scan(out=w(TA), data0=rho.broadcast_to([128, nt]), data1=w(XA),
                                                                 initial=sre0, op0=ALU.mult, op1=ALU.add),
                            reads=[PL, XA, S5ST], writes=[TA])
                    yield
                    T.issue("dve", lambda e: e.tensor_tensor_scan(out=w(TB), data0=rho.broadcast_to([128, nt]), data1=w(XB),
                                                                 initial=sim0, op0=ALU.mult, op1=ALU.add),
                            reads=[PL, XB, S5ST], writes=[TB])
                    yield
                P_ = PSETS[a % 2]
                pw = lambda i: fr(P_[i])[:, 0:nt]
                tt(pw(0), w(COS), w(TA), ALU.mult, [COS, TA], [P_[0]], eng=PENG)
                yield
                stt(pw(1), w(SIN), -1.0, w(TB), ALU.mult, ALU.mult, [SIN, TB], [P_[1]])
                yield
                tt(pw(2), w(SIN), w(TA), ALU.mult, [SIN, TA], [P_[2]], eng=PENG)
                yield
                tt(pw(3), w(COS), w(TB), ALU.mult, [COS, TB], [P_[3]], eng=PENG)
                yield
                for i in range(4):
                    mm(py, py.ap[:, 0:nt], clv(a, 0 if i < 2 else 1), pw(i), (a == 0 and i == 0), (a == 3 and i == 3), [CL, P_[i]])
                if is_s:
                    tt(HOUT.ap[:, pair:512:32], P_[0].ap[:, 0:nt], P_[1].ap[:, 0:nt], ALU.add, [P_[0], P_[1]], [HOUT])
                    yield
                    tt(HOUT.ap[:, 512 + pair:1024:32], P_[2].ap[:, 0:nt], P_[3].ap[:, 0:nt], ALU.add, [P_[2], P_[3]], [HOUT])
                    yield
                else:
                    tt(S5ST.ap[:, l * 64 + pair:l * 64 + pair + 1], P_[0].ap[:, nt - 1:nt], P_[1].ap[:, nt - 1:nt], ALU.add,
                       [P_[0], P_[1]], [S5ST])
                    yield
                    tt(S5ST.ap[:, l * 64 + 32 + pair:l * 64 + 32 + pair + 1], P_[2].ap[:, nt - 1:nt], P_[3].ap[:, nt - 1:nt], ALU.add,
                       [P_[2], P_[3]], [S5ST])
                    yield

            for a0_ in (0, 2):
                gens = [pair_ops(a0_), pair_ops(a0_ + 1)]
                alive = [True, True]
                while alive[0] or alive[1]:
                    for gi in range(2):
                        if alive[gi]:
                            try:
                                next(gens[gi])
                            except StopIteration:
                                alive[gi] = False
            YF = A.alloc(nt, "s5yf")
            YT = A.alloc(nt, "s5yt")
            dcol = VEC8.ap[:, (l * 3 + 2) * 8 + ch:(l * 3 + 2) * 8 + ch + 1]
            stt(YF.ap[:, 0:nt], suv[:, ch, :], dcol, py.ap[:, 0:nt], ALU.mult, ALU.add, [SU, VEC8, py], [YF])
            psum_unpin(py)
            act(YT.ap[:, 0:nt], YF.ap[:, 0:nt], AF.Square, [YF], [YT])
            ts(YT.ap[:, 0:nt], YT.ap[:, 0:nt], 0.044715, 1.0, ALU.mult, ALU.add, [YT], [YT])
            tt(YT.ap[:, 0:nt], YT.ap[:, 0:nt], YF.ap[:, 0:nt], ALU.mult, [YT, YF], [YT])
            act(YT.ap[:, 0:nt], YT.ap[:, 0:nt], AF.Sigmoid, [YT], [YT], scale=2.0 * math.sqrt(2.0 / math.pi))
            tt(z0v[:, ch, :], YT.ap[:, 0:nt], YF.ap[:, 0:nt], ALU.mult, [YT, YF], [Z0])
        if is_s:
            for ri, dstt in enumerate((o_re_s, o_im_s)):
                stg = A.alloc(512, "hostg")
                p = psum()
                for k in range(4):
                    transpose(p, p.ap[:, k * 128:(k + 1) * 128], HOUT.ap[:, ri * 512 + k * 128:ri * 512 + (k + 1) * 128], ident, [HOUT, CONST])
                cp(stg.ap[:, 0:512], p.ap, [p], [stg])
                T.dma("sp", dstt[l].rearrange("(k r) m -> r k m", r=128), stg.ap[:, 0:512].rearrange("r (k m) -> r k m", k=4), reads=[stg])
        elif ti == NTILE - 1:
            for ri, dstt in enumerate((o_re_p, o_im_p)):
                stg = A.alloc(128, "hostg")
                p = psum()
                transpose(p, p.ap[0:32, 0:128], S5ST.ap[:, l * 64 + ri * 32:l * 64 + (ri + 1) * 32], ident, [S5ST, CONST])
                cp(stg.ap[0:32, 0:128], p.ap[0:32, 0:128], [p], [stg])
                T.dma("sp", dstt[l], stg.ap[0:32, 0:128], reads=[stg])
        A.release(wm)
        gb = A.alloc(nt, "glusig")
        for cp_ in range(4):
            wg = WSR.get(slabK("glu_w", l, cp_ * 256, 256, 8))
            wgv = bf(wg)[:, 0:2048].rearrange("p (k n) -> p k n", k=8)
            for q in range(2):
                c = cp_ * 2 + q
                p = psum()
                for k in range(8):
                    mm(p, p.ap[:, 0:nt], wgv[:, k, q * 128:(q + 1) * 128], z0v[:, k, :], k == 0, k == 7, [wg, Z0])
                act(gb.ap[:, 0:nt], p.ap[:, 0:nt], AF.Sigmoid, [p, VEC8], [gb],
                    bias=VEC8.ap[:, (l * 3 + 1) * 8 + c:(l * 3 + 1) * 8 + c + 1])
                tt(zv[:, c, :], z0v[:, c, :], gb.ap[:, 0:nt], ALU.mult, [Z0, gb], [Z])
        A.release(m0)
        AR.release(mr0)

    def mixer(ti, nt, l):
        m = A.mark()
        rmsnorm(nt, l * 3 + 1, "xn")
        OALL = A.alloc(H * nt // 2, "OALL")
        mrope = A.mark()
        ROPE_H[0] = A.alloc(2 * NT, "rope")
        T.dma("sp", ROPE_H[0].ap[:, 0:2 * NT].rearrange("p (a n) -> p a n", a=2), rope[ti].rearrange("a p n -> p a n"),
              writes=[ROPE_H[0]])
        if ti < NTILE:
            retention_prompt(ti, l, OALL)
        else:
            retention_sample(l, OALL)
        A.release(mrope)
        Z = None
        if debug_stage >= 4:
            Z = A.alloc(8 * nt // 2, "Z")
            s5_branch(ti, nt, l, Z)
        merge_and_out(nt, l, OALL, Z, debug_stage >= 4)
        A.release(m)

    def tile_program(ti, nt, src, dst):
        load_x(src, nt)
        for l in range(DEPTH):
            if debug_stage >= 2:
                rmsnorm(nt, l * 3 + 0, "xn")
                ffn(nt, l, "ffn1")
            if debug_stage >= 3:
                mixer(ti, nt, l)
            if debug_stage >= 2:
                rmsnorm(nt, l * 3 + 2, "xn")
                ffn(nt, l, "ffn2")
        rmsnorm(nt, 12, "y")
        store_y(dst, nt)

    def program():
        WSR.reset()
        X.slots = list(X_FULL)
        XN.slots = list(XN_FULL)
        ps_rr[0] = 0
        A.release(top0)
        AR.release(topr0)
        T.dma("sp", CONST.ap, consts, writes=[CONST])
        T.dma("pool", fr(CONSTR), constsr, writes=[CONSTR])
        m = A.mark()
        stg = A.alloc(256, "gstage")
        for r0, nr in ((0, 128), (128, 80)):
            T.dma("sp", stg.ap[0:nr, 0:128], gains.rearrange("r c p -> (r c) p")[r0:r0 + nr, :], writes=[stg])
            p = psum()
            transpose(p, p.ap[:, 0:nr], stg.ap[0:nr, 0:128], ident[0:nr, 0:nr], [stg, CONST])
            cp(GAIN.ap[:, r0:r0 + nr], p.ap[:, 0:nr], [p], [GAIN])
        T.dma("sp", stg.ap[0:96, 128:256], vec8.rearrange("r c p -> (r c) p"), writes=[stg])
        p = psum()
        transpose(p, p.ap[:, 0:96], stg.ap[0:96, 128:256], ident[0:96, 0:96], [stg, CONST])
        cp(VEC8.ap[:, 0:96], p.ap[:, 0:96], [p], [VEC8])
        A.release(m)
        T.issue("dve", lambda e: e.memset(S5ST.ap, 0.0), writes=[S5ST])
        for ti in range(NTILE):
            tile_program(ti, NT, xp[ti * NT:(ti + 1) * NT, :], yp[ti * NT:(ti + 1) * NT, :])
        X.slots = X_FULL[:2]
        XN.slots = XN_FULL[:1]
        _WH.cur = WSR_SAMP
        tile_program(NTILE, NS, xs, ys)
        T.final_wait("sp")

    top0 = A.mark()
    topr0 = AR.mark()
    T.dry = True
    program()
    T.dry = False
    program()
    return nc, es, T


C_ID = 0
C_EPS6 = 128
C_EPS5 = 129
C_MASK = 256
C_QDEC = 384
C_KDI = 384 + 1024
C_MQ = 384 + 2048
C_TV = C_MQ + 512
C_ONE16 = C_TV + 512
C_SEL = C_ONE16 + 16
C_EVEN = C_SEL + 32
C_ODD = C_EVEN + 1
C_QUARTER = C_ODD + 1
CONST_W = (C_QUARTER + 1 + 127) // 128 * 128
R_PERM = 0
R_ONES = 128
R_ID = 256
R_ODIV = 384
CONSTR_W = 512


def make_consts():
    c = np.zeros((128, CONST_W), np.float32)
    c[:, C_ID:C_ID + 128] = np.eye(128, dtype=np.float32)
    c[:, C_EPS6] = 1e-6
    c[:, C_EPS5] = 1e-5
    i = np.arange(128)
    c[:, C_MASK:C_MASK + 128] = (i[None, :] >= i[:, None]).astype(np.float32)
    for h in range(H):
        g = 1.0 - 2.0 ** (-5.0 - h)
        c[:, C_QDEC + h * 128:C_QDEC + (h + 1) * 128] = (g ** (i + 1.0))[None, :]
        c[:, C_KDI + h * 128:C_KDI + (h + 1) * 128] = (g ** (-(i + 1.0)))[None, :]
    gl = i // 64
    g8 = i // 16
    for q in range(4):
        c[:, C_MQ + q * 128:C_MQ + (q + 1) * 128] = (g8[None, :] == (2 * q + gl[:, None])).astype(np.float32)
    c[:, C_TV:C_TV + 512] = (np.arange(512) + 1.0)[None, :]
    c[:, C_ONE16:C_ONE16 + 16] = 1.0
    gg = np.arange(64)
    c[0:64, C_SEL:C_SEL + 32] = (gg[:, None] // 2 == np.arange(32)[None, :]).astype(np.float32)
    c[0:64, C_EVEN] = (gg % 2 == 0)
    c[0:64, C_ODD] = (gg % 2 == 1)
    c[:, C_QUARTER] = 0.25
    return c


def make_constsr():
    c = np.zeros((128, CONSTR_W), np.float32)
    pm = np.zeros((128, 128), np.float32)
    for m_ in range(128):
        pm[(m_ + 64) % 128, m_] = 1.0
    c[:, R_PERM:R_PERM + 128] = pm
    c[:, R_ONES:R_ONES + 128] = 1.0
    c[:, R_ID:R_ID + 128] = np.eye(128, dtype=np.float32)
    c[:, R_ODIV:R_ODIV + 128] = 1.0 / 128.0
    return c


def make_rope():
    half = 64
    inv = (10000.0 ** (-np.arange(half, dtype=np.float32) / half)).astype(np.float32)
    out = np.zeros((NTILE + 1, 2, 128, NT), np.float32)
    for ti in range(NTILE + 1):
        if ti < NTILE:
            pos = np.arange(ti * NT, (ti + 1) * NT, dtype=np.float32)
        else:
            pos = np.full((NT,), float(PAST), np.float32)
        ang = (pos[None, :] * inv[:, None]).astype(np.float32)
        cs = np.cos(ang).astype(np.float32)
        sn = np.sin(ang).astype(np.float32)
        out[ti, 0, :64] = cs
        out[ti, 0, 64:] = cs
        out[ti, 1, :64] = -sn
        out[ti, 1, 64:] = sn
    return out


_CACHE = {}


def kernel(**inp):
    stage = inp.pop("_debug_stage", 99)
    ncores = inp.pop("_debug_cores", 8)
    if stage not in _CACHE:
        _CACHE[stage] = build(stage)
    nc, es, T = _CACHE[stage]
    f = lambda a: np.ascontiguousarray(np.asarray(a, dtype=np.float32))
    gains = np.zeros((13, 16, 128), np.float32)
    vec8 = np.zeros((12, 8, 128), np.float32)
    for l in range(DEPTH):
        gains[l * 3 + 0] = f(inp["ffn1_norm"][l]).reshape(16, 128)
        gains[l * 3 + 1] = f(inp["mix_norm"][l]).reshape(16, 128)
        gains[l * 3 + 2] = f(inp["ffn2_norm"][l]).reshape(16, 128)
        vec8[l * 3 + 0] = f(inp["ret_gn"][l]).reshape(8, 128)
        vec8[l * 3 + 1] = f(inp["glu_b"][l]).reshape(8, 128)
        vec8[l * 3 + 2] = f(inp["s5_d"][l]).reshape(8, 128)
    gains[12] = f(inp["final_norm"]).reshape(16, 128)
    shared = {k: f(inp[k]) for k in ("ffn1_w1", "ffn1_w3", "ffn1_w2", "w_in", "ret_proj", "glu_w", "s5_proj",
                                     "w_out", "ffn2_w1", "ffn2_w3", "ffn2_w2", "s5_lam_re", "s5_lam_im",
                                     "s5_b_re", "s5_b_im")}
    shared["s5_log_step"] = f(inp["s5_log_step"]).reshape(DEPTH, 64, 1)
    shared["s5_c_re"] = f(inp["s5_c_re"]).reshape(DEPTH, 1024, 64)
    shared["s5_c_im"] = f(inp["s5_c_im"]).reshape(DEPTH, 1024, 64)
    shared["gains"] = gains
    shared["vec8"] = vec8
    shared["consts"] = make_consts()
    shared["constsr"] = make_constsr()
    shared["rope"] = make_rope()
    xpv = f(inp["x_prompt"])
    xsv = f(inp["x_sample"]).reshape(128, D)
    sret = f(inp["state_ret"])
    sre = f(inp["state_s5_re"]).reshape(DEPTH, 128, 4096)
    sim = f(inp["state_s5_im"]).reshape(DEPTH, 128, 4096)
    in_maps = []
    for c in range(ncores):
        d = dict(shared)
        d["xp"] = xpv[c % 4]
        d["xs"] = np.ascontiguousarray(xsv[c * NS:(c + 1) * NS])
        d["st_ret"] = np.ascontiguousarray(sret[:, c * NS:(c + 1) * NS])
        d["st_re"] = np.ascontiguousarray(sre[:, c * NS:(c + 1) * NS])
        d["st_im"] = np.ascontiguousarray(sim[:, c * NS:(c + 1) * NS])
        in_maps.append(d)
    if ncores < 8:
        res = run_bass_kernel_spmd(nc, in_maps, core_ids=list(range(ncores)), trace=True)
        print("DEBUG exec_time_ns", res.exec_time_ns)
        return res.results
    res = run_bass_kernel_spmd(nc, in_maps, core_ids=list(range(ncores)))
    R = res.results
    y_p = np.stack([R[c]["yp"] for c in range(4)]).reshape(4, SEQ, D)
    y_s = np.concatenate([R[c]["ys"] for c in range(8)]).reshape(128, 1, D)
    ret_p = np.stack([R[c]["o_ret_p"] for c in range(4)], axis=1)
    re_p = np.stack([R[c]["o_re_p"].reshape(DEPTH, 64, 64) for c in range(4)], axis=1)
    im_p = np.stack([R[c]["o_im_p"].reshape(DEPTH, 64, 64) for c in range(4)], axis=1)
    ret_s = np.concatenate([R[c]["o_ret_s"] for c in range(8)], axis=1)
    re_s = np.concatenate([R[c]["o_re_s"].reshape(DEPTH, NS, 64, 64) for c in range(8)], axis=1)
    im_s = np.concatenate([R[c]["o_im_s"].reshape(DEPTH, NS, 64, 64) for c in range(8)], axis=1)
    return (y_p, y_s, ret_p, re_p, im_p, ret_s, re_s, im_s)
```
